# Optimizing a Trainium2 kernel written in Bass

```python
import math
import jax
import jax.numpy as jnp
from jax import lax
import numpy as np

D_MODEL = 1024
BATCH = 8
SEQ = 2048
DEPTH = 2

HEAD_DIM = 64
HEADS_PER_MIXER = 4
GROUP_W = HEADS_PER_MIXER * HEAD_DIM
DIFF_QK_DIM = HEAD_DIM // 2
IDX_HEADS = 8
IDX_DIM = 32
TOPK_MAX = 256
Q_BLOCK = 128
N_GROUPS = 4
EXPERTS_PER_GROUP = 8
EXPERT_FF = 256
EXPERT_TOPK = 2
N_ALIBI_HEADS = 2 * HEADS_PER_MIXER
RMS_EPS = 1e-6
NEG_INF = -1e30

IN_SPLITS = (GROUP_W, GROUP_W, GROUP_W, HEADS_PER_MIXER,
             GROUP_W, GROUP_W, GROUP_W,
             GROUP_W, GROUP_W, GROUP_W,
             GROUP_W, HEAD_DIM, HEAD_DIM,
             IDX_HEADS * IDX_DIM, IDX_DIM, IDX_HEADS)
P_IN = sum(IN_SPLITS)

kernel_name = 'hybrid_fox_stickbreak_diff_dsa_hmoe_adaln'


def rms_norm(x, g):
    xf = x.astype(jnp.float32)
    y = xf * lax.rsqrt(jnp.mean(xf * xf, axis=-1, keepdims=True) + RMS_EPS)
    return (y * g.astype(jnp.float32)).astype(x.dtype)


def sweep_query_blocks(fn, *q_side):
    B, S = q_side[0].shape[:2]
    nb = S // Q_BLOCK
    blocks = tuple(a.reshape(B, nb, Q_BLOCK, *a.shape[2:]).swapaxes(0, 1) for a in q_side)
    out = lax.map(lambda args: fn(args[0] * Q_BLOCK + jnp.arange(Q_BLOCK), *args[1:]),
                  (jnp.arange(nb),) + blocks)
    return out.swapaxes(0, 1).reshape(B, S, *out.shape[3:])


def forgetting_attention(q, k, v, log_f):
    S, d = q.shape[1], q.shape[-1]
    kpos = jnp.arange(S)
    kf = k.astype(jnp.float32)
    cum_bsh = jnp.cumsum(log_f, axis=1)
    cum = cum_bsh.transpose(0, 2, 1)

    def block(qpos, qb, cb):
        s = jnp.einsum('bqhd,bkhd->bhqk', qb.astype(jnp.float32), kf) * d ** -0.5
        s = s + cb.transpose(0, 2, 1)[..., None] - cum[:, :, None, :]
        s = jnp.where(qpos[:, None] >= kpos[None, :], s, NEG_INF)
        p = jax.nn.softmax(s, axis=-1)
        return jnp.einsum('bhqk,bkhd->bqhd', p.astype(v.dtype), v)

    return sweep_query_blocks(block, q, cum_bsh)


def stick_breaking_attention(q, k, v):
    S, d = q.shape[1], q.shape[-1]
    kpos = jnp.arange(S)
    kf = k.astype(jnp.float32)

    def block(qpos, qb):
        z = jnp.einsum('bqhd,bkhd->bhqk', qb.astype(jnp.float32), kf) * d ** -0.5
        strict = qpos[:, None] > kpos[None, :]
        log_beta = jax.nn.log_sigmoid(z)
        log_1m = jnp.where(strict, jax.nn.log_sigmoid(-z), 0.0)
        suffix = lax.cumsum(log_1m, axis=3, reverse=True) - log_1m
        w = jnp.where(strict, jnp.exp(log_beta + suffix), 0.0)
        return jnp.einsum('bhqk,bkhd->bqhd', w.astype(v.dtype), v)

    return sweep_query_blocks(block, q)


def differential_attention(q, k, v, slopes, lam, subln_g, lambda_init):
    S, dq = q.shape[1], q.shape[-1]
    kpos = jnp.arange(S)
    kf = k.astype(jnp.float32)

    def block(qpos, qb):
        dist = (qpos[:, None] - kpos[None, :]).astype(jnp.float32)
        s = jnp.einsum('bqhcd,bkhcd->bhcqk', qb.astype(jnp.float32), kf) * dq ** -0.5
        s = s - slopes[:, None, None, None] * dist
        s = jnp.where(dist >= 0, s, NEG_INF)
        p = jax.nn.softmax(s, axis=-1)
        a = p[:, :, 0] - lam * p[:, :, 1]
        return jnp.einsum('bhqk,bkhd->bqhd', a.astype(v.dtype), v)

    o = sweep_query_blocks(block, q)
    return rms_norm(o, subln_g) * (1.0 - lambda_init)


def indexed_sparse_attention(q, k, v, iq, ik, iw, slopes, topk):
    S, d = q.shape[1], q.shape[-1]
    kpos = jnp.arange(S)
    ikf = ik.astype(jnp.float32)
    gather = jax.vmap(lambda t, i: t[i])

    def block(qpos, qb, iqb, iwb):
        rel = jax.nn.relu(jnp.einsum('bqhe,bse->bqhs', iqb.astype(jnp.float32), ikf) * IDX_DIM ** -0.5)
        score = jnp.einsum('bqh,bqhs->bqs', iwb.astype(jnp.float32) * IDX_HEADS ** -0.5, rel)
        score = jnp.where(qpos[:, None] >= kpos[None, :], score, NEG_INF)
        _, idx = lax.top_k(score, topk)
        ks = gather(k, idx).astype(jnp.float32)
        vs = gather(v, idx)
        dist = (qpos[None, :, None] - idx).astype(jnp.float32)
        s = jnp.einsum('bqhd,bqkd->bhqk', qb.astype(jnp.float32), ks) * d ** -0.5
        s = s - slopes[None, :, None, None] * dist[:, None]
        s = jnp.where(dist[:, None] >= 0, s, NEG_INF)
        p = jax.nn.softmax(s, axis=-1)
        return jnp.einsum('bhqk,bqkd->bqhd', p.astype(vs.dtype), vs)

    return sweep_query_blocks(block, q, iq, iw)


def hierarchical_moe(h, w_group, b_group, w_expert, b_expert, w1, w3, w2):
    B, S, D = h.shape
    t = h.reshape(B * S, D)
    g_logits = jnp.einsum('nd,dg->ng', t, w_group).astype(jnp.float32) + b_group.astype(jnp.float32)
    g_onehot = jax.nn.one_hot(jnp.argmax(g_logits, axis=-1), N_GROUPS, dtype=jnp.float32)
    g_prob = jnp.sum(jax.nn.softmax(g_logits, axis=-1) * g_onehot, axis=-1, keepdims=True)
    e_logits = (jnp.einsum('nd,de->ne', t, w_expert).astype(jnp.float32)
                + b_expert.astype(jnp.float32)).reshape(-1, N_GROUPS, EXPERTS_PER_GROUP)
    e_sel = jnp.einsum('nge,ng->ne', e_logits, g_onehot)
    top_val, top_idx = lax.top_k(e_sel, EXPERT_TOPK)
    top_w = jax.nn.softmax(top_val, axis=-1) * g_prob
    in_group = jnp.sum(jax.nn.one_hot(top_idx, EXPERTS_PER_GROUP, dtype=jnp.float32) * top_w[..., None], axis=1)
    comb = (g_onehot[:, :, None] * in_group[:, None, :]).astype(t.dtype)
    out = jnp.zeros_like(t)
    for g in range(N_GROUPS):
        hid = jax.nn.silu(jnp.einsum('nd,edf->nef', t, w1[g])) * jnp.einsum('nd,edf->nef', t, w3[g])
        out = out + jnp.einsum('nef,efd->nd', hid * comb[:, g, :, None], w2[g])
    return out.reshape(B, S, D)


def setup_inputs(seed: int = 0) -> dict:
    key = jax.random.key(seed)
    ks = jax.random.split(key, 32)
    L, D = DEPTH, D_MODEL
    G, E, F = N_GROUPS, EXPERTS_PER_GROUP, EXPERT_FF

    def nrm(k, shape, s):
        return jax.random.normal(k, shape, jnp.float32) * s

    return {
        'x': nrm(ks[0], (BATCH, SEQ, D), 1.0),
        'c': nrm(ks[1], (BATCH, D), 1.0),
        'ada_w': nrm(ks[2], (L, D, 6 * D), 0.5 * D ** -0.5),
        'ada_b': nrm(ks[3], (L, 6 * D), 0.02),
        'norm1_g': 1.0 + nrm(ks[4], (L, D), 0.02),
        'norm2_g': 1.0 + nrm(ks[5], (L, D), 0.02),
        'w_in': nrm(ks[6], (L, D, P_IN), D ** -0.5),
        'b_f': 3.0 + nrm(ks[7], (L, HEADS_PER_MIXER), 0.5),
        'qn_a': 1.0 + nrm(ks[8], (L, HEAD_DIM), 0.02),
        'kn_a': 1.0 + nrm(ks[9], (L, HEAD_DIM), 0.02),
        'qn_c': 1.0 + nrm(ks[10], (L, DIFF_QK_DIM), 0.02),
        'kn_c': 1.0 + nrm(ks[11], (L, DIFF_QK_DIM), 0.02),
        'lam_q1': nrm(ks[12], (L, DIFF_QK_DIM), 0.1),
        'lam_k1': nrm(ks[13], (L, DIFF_QK_DIM), 0.1),
        'lam_q2': nrm(ks[14], (L, DIFF_QK_DIM), 0.1),
        'lam_k2': nrm(ks[15], (L, DIFF_QK_DIM), 0.1),
        'subln_g': 1.0 + nrm(ks[16], (L, HEAD_DIM), 0.02),
        'qn_d': 1.0 + nrm(ks[17], (L, HEAD_DIM), 0.02),
        'kn_d': 1.0 + nrm(ks[18], (L, HEAD_DIM), 0.02),
        'mix_beta': 1.0 + nrm(ks[19], (L, D), 0.02),
        'w_out': nrm(ks[20], (L, D, D), D ** -0.5),
        'w_group': nrm(ks[21], (L, D, G), D ** -0.5),
        'b_group': nrm(ks[22], (L, G), 0.01),
        'w_expert': nrm(ks[23], (L, D, G * E), D ** -0.5),
        'b_expert': nrm(ks[24], (L, G * E), 0.01),
        'w1': nrm(ks[25], (L, G, E, D, F), D ** -0.5),
        'w3': nrm(ks[26], (L, G, E, D, F), D ** -0.5),
        'w2': nrm(ks[27], (L, G, E, F, D), F ** -0.5),
    }


def reference(x, c, ada_w, ada_b, norm1_g, norm2_g, w_in, b_f, qn_a, kn_a, qn_c, kn_c,
              lam_q1, lam_k1, lam_q2, lam_k2, subln_g, qn_d, kn_d, mix_beta, w_out,
              w_group, b_group, w_expert, b_expert, w1, w3, w2):
    B, S, _ = x.shape
    H = HEADS_PER_MIXER
    topk = min(TOPK_MAX, S // 4)
    offsets = tuple(np.cumsum(IN_SPLITS)[:-1].tolist())
    slopes = jnp.exp2(-8.0 * jnp.arange(1, N_ALIBI_HEADS + 1, dtype=jnp.float32) / N_ALIBI_HEADS)
    slopes_c, slopes_d = slopes[0::2], slopes[1::2]
    c_act = jax.nn.silu(c)

    def heads(t):
        return t.reshape(B, S, H, -1)

    for l in range(DEPTH):
        mod = jnp.einsum('bd,de->be', c_act, ada_w[l]) + ada_b[l]
        shift1, scale1, gate1, shift2, scale2, gate2 = jnp.split(mod[:, None, :], 6, axis=-1)

        h = rms_norm(x, norm1_g[l]) * (1.0 + scale1) + shift1
        (a_q, a_k, a_v, a_f, b_q, b_k, b_v, c_q, c_k, c_v,
         d_q, d_k, d_v, i_q, i_k, i_w) = jnp.split(h @ w_in[l], offsets, axis=-1)

        log_f = jax.nn.log_sigmoid(a_f.astype(jnp.float32) + b_f[l].astype(jnp.float32))
        o_a = forgetting_attention(rms_norm(heads(a_q), qn_a[l]), rms_norm(heads(a_k), kn_a[l]),
                                   heads(a_v), log_f)

        o_b = stick_breaking_attention(heads(b_q), heads(b_k), heads(b_v))

        lambda_init = 0.8 - 0.6 * math.exp(-0.3 * l)
        lam = (jnp.exp(jnp.sum(lam_q1[l].astype(jnp.float32) * lam_k1[l].astype(jnp.float32)))
               - jnp.exp(jnp.sum(lam_q2[l].astype(jnp.float32) * lam_k2[l].astype(jnp.float32)))
               + lambda_init)
        cq = rms_norm(c_q.reshape(B, S, H, 2, DIFF_QK_DIM), qn_c[l])
        ck = rms_norm(c_k.reshape(B, S, H, 2, DIFF_QK_DIM), kn_c[l])
        o_c = differential_attention(cq, ck, heads(c_v), slopes_c, lam, subln_g[l], lambda_init)

        o_d = indexed_sparse_attention(rms_norm(heads(d_q), qn_d[l]), rms_norm(d_k, kn_d[l]), d_v,
                                       i_q.reshape(B, S, IDX_HEADS, IDX_DIM), i_k, i_w,
                                       slopes_d, topk)

        mix = jnp.concatenate([o.reshape(B, S, GROUP_W) for o in (o_a, o_b, o_c, o_d)],
                              axis=-1) * mix_beta[l]
        x = x + gate1 * (mix @ w_out[l])

        h = rms_norm(x, norm2_g[l]) * (1.0 + scale2) + shift2
        x = x + gate2 * hierarchical_moe(h, w_group[l], b_group[l], w_expert[l], b_expert[l],
                                         w1[l], w3[l], w2[l])
    return x
```

```python
import math
import numpy as np
import ml_dtypes
import concourse.bass as bass
import concourse.mybir as mybir
from concourse.bass_utils import run_bass_kernel_spmd

F32 = mybir.dt.float32
BF16 = mybir.dt.bfloat16
ALU = mybir.AluOpType
AF = mybir.ActivationFunctionType

D = 1024
S = 2048
NT = 16
L = 2
P_IN = 2988
EPS = 1e-6
NEG = -30000.0
SEM_LIMIT = 20000


class Ticket:
    __slots__ = ("sem", "val", "eng")

    def __init__(self, sem, val, eng):
        self.sem, self.val, self.eng = sem, val, eng


class Region:
    __slots__ = ("w", "r")

    def __init__(self):
        self.w = None
        self.r = []


class Emitter:
    ENGS = ("pe", "act", "dve", "pool", "sp")

    def __init__(self, nc):
        self.nc = nc
        self.prog = {e: [] for e in self.ENGS}
        self.cur_sem = {}
        self.cnt = {}
        self.nsem = 0
        for e in self.ENGS:
            self._new_eng_sem(e)
        self.waited = {}
        self.chan = {}
        self.regions = {}
        self.pending_dma = []

    def _alloc_sem(self, name):
        self.nsem += 1
        return self.nc.alloc_semaphore(name=f"{name}_{self.nsem}")

    def _new_eng_sem(self, e):
        self.cur_sem[e] = self._alloc_sem("e" + e)
        self.cnt[e] = 0

    def R(self, key):
        r = self.regions.get(key)
        if r is None:
            r = self.regions[key] = Region()
        return r

    def _regs(self, lst):
        return [x if isinstance(x, Region) else self.R(x) for x in (lst or [])]

    def _waits_for(self, eng, deps):
        best = {}
        for t in deps:
            if t.eng == "pe" and eng == "pe":
                continue
            k = id(t.sem)
            if k not in best or best[k].val < t.val:
                best[k] = t
        waits = []
        for k, t in best.items():
            if t.eng != "dma":
                wk = (eng, k)
                if self.waited.get(wk, 0) >= t.val:
                    continue
                self.waited[wk] = t.val
            waits.append(t)
        return waits

    def _collect(self, eng, reads, writes):
        deps = []
        for r in reads:
            if r.w is not None:
                deps.append(r.w)
        for w in writes:
            if w.w is not None:
                deps.append(w.w)
            deps.extend(w.r)
        return self._waits_for(eng, deps)

    def _update(self, ticket, reads, writes):
        for w in writes:
            w.w = ticket
            w.r = []
        for r in reads:
            if r not in writes:
                r.r.append(ticket)

    def op(self, eng, fn, reads=None, writes=None, inc=True):
        reads, writes = self._regs(reads), self._regs(writes)
        waits = self._collect(eng, reads, writes)
        if self.cnt[eng] >= SEM_LIMIT:
            self._new_eng_sem(eng)
        sem = self.cur_sem[eng]
        if inc:
            self.cnt[eng] += 1
            ticket = Ticket(sem, self.cnt[eng], eng)
        else:
            assert eng == "pe"
            ticket = Ticket(sem, self.cnt[eng] + 1, eng)

        def emit(e, waits=waits, fn=fn, sem=sem, inc=inc):
            for t in waits:
                e.wait_ge(t.sem, t.val)
            if inc:
                fn(e).then_inc(sem, 1)
            else:
                fn(e)

        self.prog[eng].append(emit)
        self._update(ticket, reads, writes)
        return ticket

    def dma(self, queue, chan, fn, reads=None, writes=None):
        reads, writes = self._regs(reads), self._regs(writes)
        waits = self._collect(queue, reads, writes)
        c = self.chan.get(chan)
        if c is None:
            c = self.chan[chan] = [self._alloc_sem("d"), 0]
        c[1] += 16
        ticket = Ticket(c[0], c[1], "dma")
        sem = c[0]

        def emit(e, waits=waits, fn=fn, sem=sem):
            for t in waits:
                e.wait_ge(t.sem, t.val)
            fn(e).then_inc(sem, 16)

        self.prog[queue].append(emit)
        self._update(ticket, reads, writes)
        self.pending_dma.append(ticket)
        return ticket

    def barrier(self):
        deps = [Ticket(self.cur_sem[e], self.cnt[e], e) for e in self.ENGS if self.cnt[e] > 0]
        last = {}
        for t in self.pending_dma:
            last[id(t.sem)] = t if (id(t.sem) not in last or last[id(t.sem)].val < t.val) else last[id(t.sem)]
        dmas = list(last.values())
        self.pending_dma = dmas
        for eng in self.ENGS:
            ws = [t for t in deps if t.eng != eng] + dmas

            def emit(e, ws=ws):
                for t in ws:
                    e.wait_ge(t.sem, t.val)

            self.prog[eng].append(emit)
            for t in deps:
                if t.eng != eng:
                    self.waited[(eng, id(t.sem))] = max(self.waited.get((eng, id(t.sem)), 0), t.val)

    def wait_all(self, eng, regions):
        regs = self._regs(regions)
        waits = self._collect(eng, regs, regs)

        def emit(e, waits=waits):
            for t in waits:
                e.wait_ge(t.sem, t.val)

        self.prog[eng].append(emit)

    def finish(self):
        nc = self.nc
        with nc.Block() as block:
            @block.tensor
            def _(e):
                for f in self.prog["pe"]:
                    f(e)

            @block.scalar
            def _(e):
                for f in self.prog["act"]:
                    f(e)

            @block.vector
            def _(e):
                for f in self.prog["dve"]:
                    f(e)

            @block.gpsimd
            def _(e):
                for f in self.prog["pool"]:
                    f(e)

            @block.sync
            def _(e):
                for f in self.prog["sp"]:
                    f(e)


CB_IDENT, CB_TRIADD, CB_TRIMUL, CB_B64, CB_B32, CB_ONES, CB_NSLOPE, CB_POS = (
    0, 128, 256, 384, 512, 640, 768, 1792)
NCB = 2304
CF_IDENT, CF_ALIBI, CF_NEGTRI, CF_GMASK = 0, 128, 288, 416
NCF = 420
AL_N, AL_O = 20, 16
SLOPES = [2.0 ** (-8.0 * i / 8) for i in range(1, 9)]
SLOPES_CD = SLOPES[0::2] + SLOPES[1::2]


def make_consts():
    cb = np.zeros((128, NCB), np.float32)
    p = np.arange(128)
    cb[:, CB_IDENT:CB_IDENT + 128] = np.eye(128)
    cb[:, CB_TRIADD:CB_TRIADD + 128] = np.where(p[:, None] <= p[None, :], 0.0, NEG)
    cb[:, CB_TRIMUL:CB_TRIMUL + 128] = (p[:, None] < p[None, :]).astype(np.float32)
    cb[:, CB_B64:CB_B64 + 128] = (p[:, None] // 64 == p[None, :] // 64)
    cb[:, CB_B32:CB_B32 + 128] = (p[:, None] // 32 == p[None, :] // 32)
    cb[:, CB_ONES:CB_ONES + 128] = 1.0
    for h in range(8):
        cb[0:2, CB_NSLOPE + h * 128:CB_NSLOPE + (h + 1) * 128] = -SLOPES_CD[h]
    t = np.arange(512)
    cb[0, CB_POS:CB_POS + 512] = 128 * (t // 128)
    cb[1, CB_POS:CB_POS + 512] = t % 128
    cf = np.zeros((128, NCF), np.float32)
    cf[:, CF_IDENT:CF_IDENT + 128] = np.eye(128)
    for h in range(8):
        for d in range(-16, 4):
            cf[:, CF_ALIBI + h * AL_N + d + AL_O] = SLOPES_CD[h] * (p + 128 * d)
    cf[:, CF_NEGTRI:CF_NEGTRI + 128] = np.where(p[None, :] <= p[:, None], 0.0, -1e30)
    for g in range(4):
        cf[:, CF_GMASK + g] = (p // 32 == g)
    return cb.astype(ml_dtypes.bfloat16), cf


RP_N1, RP_N2, RP_SUBLN, RP_BR, RP_LAM = 0, 1024, 2048, 2112, 2148
NRP = 2276
CP_QNA, CP_KNA, CP_QNC, CP_KNC, CP_QND, CP_KND, CP_BF, CP_BETA, CP_QNC1 = 0, 1, 2, 3, 4, 5, 6, 7, 15
NCP = 16


def build(nc, cfg):
    nl = cfg.get("n_layers", L)
    mixers = cfg.get("mixers", "ABCD")
    do_moe = cfg.get("moe", True)
    stopC = cfg.get("stopC", 99)
    stopD = cfg.get("stopD", 99)
    cdbg = cfg.get("cdbg", "")
    em = Emitter(nc)
    op, dma = em.op, em.dma

    def dram(name, shape, dt=F32, kind="ExternalInput"):
        return nc.dram_tensor(name, list(shape), dt, kind=kind).ap()

    x_d = dram("x", [S, D])
    out_d = dram("out", [S, D], kind="ExternalOutput")
    cT_d = dram("cT", [128, 8])
    ada_w_d = dram("ada_w", [L, D, 6 * D])
    ada_b_d = dram("ada_b", [L, 128, 6 * D])
    w_in_d = dram("w_in", [L, D, P_IN])
    w_out_d = dram("w_out", [L, D, D])
    w_r_d = dram("w_r", [L, D, 36])
    w1_d = dram("w1", [L, 32, D, 256])
    w3_d = dram("w3", [L, 32, D, 256])
    w2_d = dram("w2", [L, 32, 256, D])
    rowp_d = dram("rowp", [L, 128, NRP])
    colp_d = dram("colp", [L, 128, NCP])
    cb_d = dram("cb", [128, NCB], BF16)
    cf_d = dram("cf", [128, NCF])
    modb_d = dram("modb", [L, 128, 6 * D], kind="Internal")

    _sbc = {}

    def sb(name, shape, dt):
        if name not in _sbc:
            _sbc[name] = nc.alloc_sbuf_tensor("sb_" + name, list(shape), dt).ap()
        return _sbc[name]

    x = sb("x", [128, NT, D], F32)
    hT = sb("hT", [128, 8, S], BF16)
    cb = sb("cbs", [128, NCB], BF16)
    cf = sb("cfs", [128, NCF], F32)
    rowp = sb("rowps", [128, NRP - 2048], F32)
    colp = sb("colps", [128, NCP], F32)
    gate = sb("gate", [128, D], F32)
    small = sb("small", [128, 256], F32)
    arena = sb("arena", [128, 84224 // 2], BF16)
    ps = [nc.alloc_psum_tensor(f"ps{i}", [128, 512], F32).ap() for i in range(8)]
    PS = [f"ps{i}" for i in range(8)]

    ident_b = cb[:, CB_IDENT:CB_IDENT + 128]
    tri_add = cb[:, CB_TRIADD:CB_TRIADD + 128]
    tri_mul = cb[:, CB_TRIMUL:CB_TRIMUL + 128]
    bones64 = cb[:, CB_B64:CB_B64 + 128]
    bones32 = cb[:, CB_B32:CB_B32 + 128]
    ones_b = cb[:, CB_ONES:CB_ONES + 128]
    ident_f = cf[:, CF_IDENT:CF_IDENT + 128]

    def carve(off_bytes, shape, dt):
        n = int(np.prod(shape[1:]))
        esz = 4 if dt == F32 else 2
        a = arena[0:shape[0], off_bytes // 2: off_bytes // 2 + n * esz // 2]
        if dt == F32:
            a = a.bitcast(F32)
        if len(shape) == 3:
            a = a.rearrange("p (a b) -> p a b", a=shape[1])
        elif len(shape) == 4:
            a = a.rearrange("p (a b c) -> p a b c", a=shape[1], b=shape[2])
        return a

    def mm(out, lhsT, rhs, start, stop, reads, writes, inc=True):
        kw = {"skip_group_check": True}
        try:
            lhsT.base_partition()
        except BaseException:
            kw["tile_position"] = (96, 0)
        return op("pe", lambda e: e.matmul(out, lhsT=lhsT, rhs=rhs, start=start, stop=stop, **kw),
                  reads=reads, writes=writes, inc=inc)

    dma("sp", "c0", lambda e: e.dma_start(out=cb, in_=cb_d), writes=["cb"])
    dma("sp", "c1", lambda e: e.dma_start(out=cf, in_=cf_d), writes=["cf"])
    for t in range(NT):
        dma("sp", f"xin{t}", lambda e, t=t: e.dma_start(out=x[:, t, :], in_=x_d[t * 128:(t + 1) * 128, :]),
            writes=[("x", t)])
    cTs = small[:, 0:8]
    dma("sp", "c2", lambda e: e.dma_start(out=cTs, in_=cT_d), writes=["cT"])
    sc_e = small[:, 8:16]
    op("act", lambda e: e.activation(out=sc_e, in_=cTs, func=AF.Exp, scale=-1.0), reads=["cT"], writes=["sc_e"])
    op("dve", lambda e: e.tensor_scalar(out=sc_e, in0=sc_e, scalar1=1.0, scalar2=None, op0=ALU.add),
       reads=["sc_e"], writes=["sc_e"])
    op("dve", lambda e: e.reciprocal(out=sc_e, in_=sc_e), reads=["sc_e"], writes=["sc_e"])
    op("dve", lambda e: e.tensor_tensor(out=sc_e, in0=sc_e, in1=cTs, op=ALU.mult), reads=["sc_e", "cT"],
       writes=["sc_e"])
    eps_t = small[:, 16:17]
    op("dve", lambda e: e.memset(eps_t, EPS), writes=["eps"])
    lhs_rep = sb("lhs_rep", [128, 8, 128], BF16)
    gs = carve(20480, [128, D], F32)
    shf = carve(24576, [128, D], F32)
    for c in range(8):
        op("dve", lambda e, c=c: e.tensor_scalar(out=lhs_rep[:, c, :], in0=ones_b, scalar1=sc_e[:, c:c + 1],
                                                 scalar2=None, op0=ALU.mult),
           reads=["cb", "sc_e"], writes=["lhs_rep"])

    def phase_mod(l):
        aw = [carve(0, [128, 8, 512], BF16), carve(8192, [128, 8, 512], BF16)]
        ab = [carve(16384, [128, 512], F32), carve(18432, [128, 512], F32)]
        mo = [carve(20480, [128, 512], F32), carve(22528, [128, 512], F32)]
        awv = ada_w_d[l].rearrange("(c p) n -> p c n", p=128)
        for pc in range(12):
            b = pc % 2
            dma("pool", f"aw{b}", lambda e, b=b, pc=pc: e.dma_start(out=aw[b], in_=awv[:, :, pc * 512:(pc + 1) * 512]),
                writes=[("aw", b)])
            dma("sp", f"ab{b}", lambda e, b=b, pc=pc: e.dma_start(out=ab[b], in_=ada_b_d[l][:, pc * 512:(pc + 1) * 512]),
                writes=[("ab", b)])
            for c in range(8):
                mm(ps[b], lhs_rep[:, c, :], aw[b][:, c, :], c == 0, c == 7,
                   reads=["lhs_rep", ("aw", b)], writes=[PS[b]], inc=(c == 7))
            op("dve", lambda e, b=b: e.tensor_tensor(out=mo[b], in0=ps[b], in1=ab[b], op=ALU.add),
               reads=[PS[b], ("ab", b)], writes=[("mo", b)])
            dma("sp", f"mo{b}", lambda e, b=b, pc=pc: e.dma_start(out=modb_d[l][:, pc * 512:(pc + 1) * 512], in_=mo[b]),
                reads=[("mo", b)], writes=[("modb", l, pc // 2)])
        dma("sp", "rp", lambda e: e.dma_start(out=rowp, in_=rowp_d[l][:, 2048:NRP]), writes=["rowp"])
        dma("sp", "cp", lambda e: e.dma_start(out=colp, in_=colp_d[l]), writes=["colp"])

    def phase_norm(l, which):
        i_shift, i_scale = (0, 1) if which == 0 else (3, 4)
        tmpg = carve(0, [128, D], F32)
        junk = carve(4096, [128, D], BF16)
        tf = [carve(8192, [128, D], F32), carve(12288, [128, D], F32)]
        hb = [carve(16384, [128, D], BF16), carve(18432, [128, D], BF16)]
        ss = small[:, 32:48]
        rstd = small[:, 48:64]
        dma("sp", "gs", lambda e: e.dma_start(out=gs, in_=modb_d[l][:, i_scale * D:(i_scale + 1) * D]),
            reads=[("modb", l, i_scale)], writes=["gs"])
        dma("sp", "tg", lambda e: e.dma_start(out=tmpg, in_=rowp_d[l][:, which * D:(which + 1) * D]), writes=["tmpg"])
        dma("sp", "sh", lambda e: e.dma_start(out=shf, in_=modb_d[l][:, i_shift * D:(i_shift + 1) * D]),
            reads=[("modb", l, i_shift)], writes=["shf"])
        op("dve", lambda e: e.scalar_tensor_tensor(out=gs, in0=gs, scalar=1.0, in1=tmpg, op0=ALU.add, op1=ALU.mult),
           reads=["gs", "tmpg"], writes=["gs"])
        for t in range(NT):
            op("act", lambda e, t=t: e.activation(out=junk, in_=x[:, t, :], func=AF.Square, accum_out=ss[:, t:t + 1]),
               reads=[("x", t)], writes=["junk", "ss"])
        op("act", lambda e: e.activation(out=rstd, in_=ss, func=AF.Ln, bias=eps_t, scale=1.0 / D),
           reads=["ss", "eps"], writes=["rstd"])
        op("act", lambda e: e.activation(out=rstd, in_=rstd, func=AF.Exp, scale=-0.5), reads=["rstd"], writes=["rstd"])
        pst = ps[5].bitcast(BF16).rearrange("p (a b) -> p a b", a=8)
        for t in range(NT):
            b = t % 2
            op("dve", lambda e, t=t, b=b: e.scalar_tensor_tensor(out=tf[b], in0=x[:, t, :], scalar=rstd[:, t:t + 1], in1=gs,
                                                                 op0=ALU.mult, op1=ALU.mult),
               reads=[("x", t), "rstd", "gs"], writes=[("tf", b)])
            op("pool", lambda e, b=b: e.tensor_tensor(out=hb[b], in0=tf[b], in1=shf, op=ALU.add),
               reads=[("tf", b), "shf"], writes=[("hb", b)])
            for c in range(8):
                op("pe", lambda e, b=b, c=c: e.transpose(pst[:, c, :], hb[b][:, c * 128:(c + 1) * 128], ident_b),
                   reads=[("hb", b), "cb"], writes=[PS[5]])
            op("act", lambda e, t=t: e.activation(out=hT[:, :, t * 128:(t + 1) * 128], in_=pst, func=AF.Copy),
               reads=[PS[5]], writes=[("hT", t // 4)])

    A_BUFQ, A_BUFK, A_BUFIQ, A_BUFIK = 0, 8192, 16384, 24576
    A_V = 28672
    A_MIXTOK = 37120
    A_MIXT = 45312
    A_SCR = 53504
    A_W = 78080
    bufQ = carve(A_BUFQ, [128, 2, S], BF16)
    bufK = carve(A_BUFK, [128, 2, S], BF16)
    bufIQ = carve(A_BUFIQ, [128, 2, S], BF16)
    bufIK = carve(A_BUFIK, [128, S], BF16)
    Vaug = carve(A_V, [128, NT, 4, 65], BF16)
    mix_tok = carve(A_MIXTOK, [128, NT, 256], BF16)
    mixT = carve(A_MIXT, [128, 2, S], BF16)
    wst = [carve(A_W, [128, 8, 128], BF16), carve(A_W + 2048, [128, 8, 128], BF16),
           carve(A_W + 4096, [128, 8, 128], BF16)]
    wst_i = [0]
    sqb = sb("sqb", [128, 512], BF16)
    lnv = sb("lnv", [128, 512], F32)
    pT = [sb("pT0", [128, 512], BF16), sb("pT1", [128, 512], BF16)]
    recs = sb("recs", [128, 16], F32)

    def load_w(l, c0, ncols):
        i = wst_i[0] % 3
        wst_i[0] += 1
        wv = w_in_d[l].rearrange("(c p) n -> p c n", p=128)
        dst = wst[i][:, :, 0:ncols]
        dma("pool", f"wst{i}", lambda e: e.dma_start(out=dst, in_=wv[:, :, c0:c0 + ncols]), writes=[("wst", i)])
        return dst, ("wst", i)

    pj_i = [0]

    def projT(wt, wreg, M, tg):
        b = pj_i[0] % 2
        pj_i[0] += 1
        for c in range(8):
            mm(ps[b][0:M, :], wt[:, c, 0:M], hT[:, c, tg * 512:(tg + 1) * 512], c == 0, c == 7,
               reads=[wreg, ("hT", tg)], writes=[PS[b]], inc=(c == 7))
        return ps[b], PS[b]

    def qk_norm_store(pb, preg, M, bones, inv_d, gcol, lnscale, dst, dreg, out2=None):
        op("act", lambda e: e.activation(out=sqb[0:M, :], in_=pb[0:M, :], func=AF.Square), reads=[preg], writes=["sqb"])
        mm(ps[4][0:M, :], bones[0:M, 0:M], sqb[0:M, :], True, True, reads=["sqb", "cb"], writes=[PS[4]])
        op("act", lambda e: e.activation(out=lnv[0:M, :], in_=ps[4][0:M, :], func=AF.Ln, bias=eps_t[0:M, :], scale=inv_d),
           reads=[PS[4], "eps"], writes=["lnv"])
        lb = small[:, 17 + int(lnscale):18 + int(lnscale)]
        op("act", lambda e: e.activation(out=lnv[0:M, :], in_=lnv[0:M, :], func=AF.Exp, bias=lb[0:M, :], scale=-0.5),
           reads=["lnv", "lnb"], writes=["lnv"])
        op("dve", lambda e: e.scalar_tensor_tensor(out=dst, in0=pb[0:M, :], scalar=gcol[0:M, :], in1=lnv[0:M, :],
                                                   op0=ALU.mult, op1=ALU.mult),
           reads=[preg, "lnv", "colp"], writes=[dreg])
        if out2 is not None:
            gcol2, dst2, dreg2 = out2
            op("dve", lambda e: e.scalar_tensor_tensor(out=dst2, in0=pb[0:M, :], scalar=gcol2[0:M, :], in1=lnv[0:M, :],
                                                       op0=ALU.mult, op1=ALU.mult),
               reads=[preg, "lnv", "colp"], writes=[dreg2])

    def proj_v(l, c0, nheads, dstV, vreg):
        ncols = nheads * 64
        wparts = []
        for j in range(0, ncols, 128):
            wparts.append(load_w(l, c0 + j, min(128, ncols - j)))
        for t in range(NT):
            b = pj_i[0] % 2
            pj_i[0] += 1
            for j, (wt, wreg) in enumerate(wparts):
                n = wt.shape[2]
                for c in range(8):
                    mm(ps[b][:, j * 128:j * 128 + n], hT[:, c, t * 128:(t + 1) * 128], wt[:, c, :], c == 0 and j == 0, c == 7,
                       reads=[wreg, ("hT", t // 4)], writes=[PS[b]])
            src = ps[b][:, 0:ncols].rearrange("p (h d) -> p h d", h=nheads)
            op("act", lambda e, t=t, src=src: e.activation(out=dstV[:, t, 0:nheads, 0:64], in_=src, func=AF.Copy),
               reads=[PS[b]], writes=[vreg])

    po_i = [0]

    def run_pipelined(items):
        if items:
            items[0][0]()
        for i, (_, s2) in enumerate(items):
            if i + 1 < len(items):
                items[i + 1][0]()
            s2()

    def attn_core(nheads_spec, finish_fn, QG=512):
        nqb = QG // 128
        items = []
        for si, sp_ in enumerate(nheads_spec):
            for qg in range(S // QG):
                pb = 2 + (po_i[0] % 2)
                po_i[0] += 1
                po, poreg = ps[pb], PS[pb]
                first_po = [True]
                nkb = nqb * (qg + 1)
                for kb in range(nkb):
                    st = {}

                    def s1(sp_=sp_, qg=qg, kb=kb, st=st):
                        j = kb - nqb * qg
                        c0 = 0 if j < 0 else j * 128
                        sbk = pj_i[0] % 2
                        pj_i[0] += 1
                        pss, psreg = ps[sbk], PS[sbk]
                        has_extra = sp_.get("extra") is not None
                        dm = sp_.get("diag_mask", False) and j >= 0
                        mm(pss[:, c0:QG], sp_["kT"](kb), sp_["qT"](qg * QG + c0, (qg + 1) * QG), True,
                           not (has_extra or dm), reads=sp_["kq_regs"], writes=[psreg])
                        if has_extra:
                            sp_["extra"](pss, psreg, qg, kb, c0, not dm)
                        if dm:
                            mm(pss[:, c0:c0 + 128], ident_b, tri_add, False, True, reads=["cb"], writes=[psreg])
                        st.update(sbk=sbk, c0=c0)

                    def s2(sp_=sp_, si=si, qg=qg, kb=kb, st=st, po=po, poreg=poreg, first_po=first_po, last=(kb == nkb - 1)):
                        sbk, c0 = st["sbk"], st["c0"]
                        pss, psreg = ps[sbk], PS[sbk]
                        pt = pT[sbk]
                        bias_ap = sp_["bias"](kb, qg)
                        op("act", lambda e, pt=pt, pss=pss, c0=c0, bias_ap=bias_ap: e.activation(
                            out=pt[:, c0:QG], in_=pss[:, c0:QG], func=AF.Exp, bias=bias_ap),
                           reads=[psreg] + sp_["bias_regs"], writes=[("pT", sbk)])
                        for i in range(c0 // 128, nqb):
                            mm(po[:, i * 65:(i + 1) * 65], pt[:, i * 128:(i + 1) * 128], sp_["V"](kb), first_po[0], False,
                               reads=[("pT", sbk)] + sp_["v_regs"], writes=[poreg])
                            first_po[0] = False
                        if last:
                            finish_fn(si, qg, po, poreg)

                    items.append((s1, s2))
        run_pipelined(items)

    def finish_softmax(mcol0):
        def fin(si, qg, po, poreg):
            for i in range(4):
                t = qg * 4 + i
                op("dve", lambda e, i=i: e.reciprocal(out=recs[:, i:i + 1], in_=po[:, i * 65 + 64:i * 65 + 65]),
                   reads=[poreg], writes=["recs"])
                op("dve", lambda e, i=i, t=t: e.tensor_scalar(out=mix_tok[:, t, mcol0 + si * 64:mcol0 + (si + 1) * 64],
                                                              in0=po[:, i * 65:i * 65 + 64], scalar1=recs[:, i:i + 1],
                                                              scalar2=None, op0=ALU.mult),
                   reads=[poreg, "recs"], writes=[("mix_tok", t)])
        return fin

    def mixer_out(l, m):
        pst = ps[5].bitcast(BF16).rearrange("p (a b) -> p a b", a=8)
        for t in range(NT):
            for c in range(2):
                op("pe", lambda e, t=t, c=c: e.transpose(pst[:, c, :], mix_tok[:, t, c * 128:(c + 1) * 128], ident_b),
                   reads=[("mix_tok", t), "cb"], writes=[PS[5]])
            op("act", lambda e, t=t: e.activation(out=mixT[:, :, t * 128:(t + 1) * 128], in_=pst[:, 0:2, :], func=AF.Copy),
               reads=[PS[5]], writes=["mixT"])
        wo_f = carve(A_SCR, [128, 2, D], F32)
        wo_b = carve(A_SCR + 8192, [128, 2, D], BF16)
        wv = w_out_d[l][m * 256:(m + 1) * 256, :].rearrange("(c p) n -> p c n", p=128)
        dma("sp", "wo", lambda e: e.dma_start(out=wo_f, in_=wv), writes=["wo_f"])
        for c in range(2):
            bc = colp[:, CP_BETA + m * 2 + c:CP_BETA + m * 2 + c + 1]
            op("dve", lambda e, c=c, bc=bc: e.scalar_tensor_tensor(out=wo_b[:, c, :], in0=wo_f[:, c, :], scalar=bc, in1=gate,
                                                                   op0=ALU.mult, op1=ALU.mult),
               reads=["wo_f", "colp", "gate"], writes=["wo_b"])
        for t in range(NT):
            for hf in range(2):
                b = 6 + (t * 2 + hf) % 2
                for c in range(2):
                    mm(ps[b], mixT[:, c, t * 128:(t + 1) * 128], wo_b[:, c, hf * 512:(hf + 1) * 512], c == 0, c == 1,
                       reads=["mixT", "wo_b"], writes=[PS[b]])
                op("dve", lambda e, t=t, hf=hf, b=b: e.tensor_tensor(out=x[:, t, hf * 512:(hf + 1) * 512],
                                                                     in0=x[:, t, hf * 512:(hf + 1) * 512], in1=ps[b], op=ALU.add),
                   reads=[PS[b], ("x", t)], writes=[("x", t)])

    def set_v_ones():
        op("pool", lambda e: e.memset(Vaug[:, :, :, 64:65], 1.0), writes=["V"])

    def set_lnb():
        op("dve", lambda e: e.memset(small[:, 17:18], 0.0), writes=["lnb"])
        op("dve", lambda e: e.memset(small[:, 18:19], math.log(0.125)), writes=["lnb"])
        op("dve", lambda e: e.memset(small[:, 19:20], math.log(32 ** -0.5)), writes=["lnb"])

    def mixer_A(l):
        set_v_ones()
        spT = carve(A_SCR, [4, S], F32)
        cums = carve(A_SCR + 8192, [4, S], F32)
        chiT = carve(A_SCR + 16384, [4, S], BF16)
        ones4 = carve(A_SCR + 20480, [4, 512], F32)
        poscum = sb("poscum", [128, NT, 4], F32)
        nbf = small[:, 20:21]
        op("dve", lambda e: e.tensor_scalar(out=nbf, in0=colp[:, CP_BF:CP_BF + 1], scalar1=-1.0, scalar2=None, op0=ALU.mult),
           reads=["colp"], writes=["nbf"])
        op("dve", lambda e: e.memset(ones4, 1.0), writes=["ones4"])
        for (c0, dst, gc, lns, dreg) in ((0, bufQ, CP_QNA, 1.0, "bufQ"), (256, bufK, CP_KNA, 0.0, "bufK")):
            for ch in range(2):
                wt, wreg = load_w(l, c0 + ch * 128, 128)
                for tg in range(4):
                    pb, preg = projT(wt, wreg, 128, tg)
                    qk_norm_store(pb, preg, 128, bones64, 1.0 / 64, colp[:, gc:gc + 1], lns,
                                  dst[:, ch, tg * 512:(tg + 1) * 512], dreg)
        wt, wreg = load_w(l, 768, 4)
        for tg in range(4):
            pb, preg = projT(wt, wreg, 4, tg)
            op("act", lambda e, pb=pb, tg=tg: e.activation(out=spT[:, tg * 512:(tg + 1) * 512], in_=pb[0:4, :], func=AF.Exp,
                                                           bias=nbf[0:4, :], scale=-1.0),
               reads=[preg, "nbf"], writes=["spT"])
        op("act", lambda e: e.activation(out=spT, in_=spT, func=AF.Ln, bias=1.0), reads=["spT"], writes=["spT"])
        for tg in range(4):
            init = 0.0 if tg == 0 else cums[:, tg * 512 - 1:tg * 512]
            op("dve", lambda e, tg=tg, init=init: e.tensor_tensor_scan(out=cums[:, tg * 512:(tg + 1) * 512], data0=ones4,
                                                                       data1=spT[:, tg * 512:(tg + 1) * 512], initial=init,
                                                                       op0=ALU.mult, op1=ALU.add),
               reads=["spT", "ones4", "cums"], writes=["cums"])
        op("dve", lambda e: e.tensor_scalar(out=chiT, in0=cums, scalar1=-1.0, scalar2=None, op0=ALU.mult),
           reads=["cums"], writes=["chiT"])
        for t in range(NT):
            op("pe", lambda e, t=t: e.transpose(ps[4][:, t * 4:(t + 1) * 4], cums[0:4, t * 128:(t + 1) * 128], ident_f[0:4, 0:4]),
               reads=["cums", "cf"], writes=[PS[4]])
        op("dve", lambda e: e.tensor_copy(out=poscum, in_=ps[4][:, 0:64].rearrange("p (t h) -> p t h", t=NT)),
           reads=[PS[4]], writes=["poscum"])
        proj_v(l, 512, 4, Vaug, "V")
        sel4 = [sb(f"sel4_{h_}", [4, 128], BF16) for h_ in range(4)]
        for h_ in range(4):
            op("dve", lambda e, h_=h_: e.tensor_scalar(out=sel4[h_], in0=ones_b[0:4, :], scalar1=ident_f[0:4, h_:h_ + 1],
                                                       scalar2=None, op0=ALU.mult), reads=["cb", "cf"], writes=["sel4"])
        specs = []
        for h in range(4):
            hp, ch = (h % 2) * 64, h // 2

            def extra(pss, psreg, qg, kb, c0, last, h=h):
                mm(pss[:, c0:512], sel4[h],
                   chiT[0:4, qg * 512 + c0:(qg + 1) * 512], False, last, reads=["sel4", "chiT"], writes=[psreg])

            specs.append(dict(
                kT=lambda kb, hp=hp, ch=ch: bufK[hp:hp + 64, ch, kb * 128:(kb + 1) * 128],
                qT=lambda q0, q1, hp=hp, ch=ch: bufQ[hp:hp + 64, ch, q0:q1],
                kq_regs=["bufQ", "bufK"], extra=extra, diag_mask=True,
                bias=lambda kb, qg, h=h: poscum[:, kb, h:h + 1], bias_regs=["poscum"],
                V=lambda kb, h=h: Vaug[:, kb, h, :], v_regs=["V"]))
        attn_core(specs, finish_softmax(0))

    def mixer_B(l):
        QG = 256
        lbuf = carve(A_SCR, [128, NT, QG], F32)
        l1m = carve(A_SCR + 16384, [128, NT, QG], BF16)
        mixscr = sb("mixscr", [128, 512], F32)
        argb = [mixscr[:, 0:QG], mixscr[:, QG:2 * QG]]
        wTb = [sb("wTb0", [128, QG], BF16), sb("wTb1", [128, QG], BF16)]
        for (c0, dst, scl, dreg) in ((772, bufQ, 0.125, "bufQ"), (1028, bufK, 1.0, "bufK")):
            for ch in range(2):
                wt, wreg = load_w(l, c0 + ch * 128, 128)
                for tg in range(4):
                    pb, preg = projT(wt, wreg, 128, tg)
                    op("dve", lambda e, pb=pb, dst=dst, ch=ch, tg=tg, scl=scl: e.tensor_scalar(
                        out=dst[:, ch, tg * 512:(tg + 1) * 512], in0=pb, scalar1=scl, scalar2=None, op0=ALU.mult),
                       reads=[preg], writes=[dreg])
        proj_v(l, 1284, 4, Vaug, "V")
        for h in range(4):
            hp, ch = (h % 2) * 64, h // 2
            for qg in range(S // QG):
                nkb = 2 * (qg + 1)
                pb_ = 2 + (po_i[0] % 2)
                po_i[0] += 1
                po, poreg = ps[pb_], PS[pb_]
                for kb in range(nkb):
                    j = kb - 2 * qg
                    c0 = 0 if j < 0 else j * 128
                    sbk = pj_i[0] % 2
                    pj_i[0] += 1
                    pss, psreg = ps[sbk], PS[sbk]
                    mm(pss[:, c0:QG], bufK[hp:hp + 64, ch, kb * 128:(kb + 1) * 128],
                       bufQ[hp:hp + 64, ch, qg * QG + c0:(qg + 1) * QG], True, True, reads=["bufQ", "bufK"], writes=[psreg])
                    op("act", lambda e, kb=kb, c0=c0, pss=pss: e.activation(out=lbuf[:, kb, c0:QG], in_=pss[:, c0:QG],
                                                                            func=AF.Exp, scale=-1.0),
                       reads=[psreg], writes=[("lbuf", kb)])
                    op("act", lambda e, kb=kb, c0=c0: e.activation(out=lbuf[:, kb, c0:QG], in_=lbuf[:, kb, c0:QG],
                                                                   func=AF.Ln, bias=1.0),
                       reads=[("lbuf", kb)], writes=[("lbuf", kb)])
                    op("dve", lambda e, kb=kb, c0=c0, pss=pss: e.scalar_tensor_tensor(
                        out=l1m[:, kb, c0:QG], in0=pss[:, c0:QG], scalar=-1.0, in1=lbuf[:, kb, c0:QG],
                        op0=ALU.mult, op1=ALU.subtract),
                       reads=[psreg, ("lbuf", kb)], writes=[("l1m", kb)])
                    if j >= 0:
                        op("dve", lambda e, kb=kb, c0=c0: e.tensor_tensor(out=l1m[:, kb, c0:c0 + 128],
                                                                           in0=l1m[:, kb, c0:c0 + 128], in1=tri_mul, op=ALU.mult),
                           reads=[("l1m", kb), "cb"], writes=[("l1m", kb)])
                first_po = [True]
                items = []
                for kb in range(nkb):
                    st = {}

                    def s1(kb=kb, st=st, qg=qg, nkb=nkb):
                        j = kb - 2 * qg
                        c0 = 0 if j < 0 else j * 128
                        sbk = pj_i[0] % 2
                        pj_i[0] += 1
                        pss, psreg = ps[sbk], PS[sbk]
                        later = list(range(kb + 1, nkb))
                        mm(pss[:, c0:QG], triL, l1m[:, kb, c0:QG], True, len(later) == 0,
                           reads=[("l1m", kb), "triL"], writes=[psreg])
                        for n_, kb2 in enumerate(later):
                            j2 = kb2 - 2 * qg
                            c2 = 0 if j2 < 0 else j2 * 128
                            cc = max(c0, c2)
                            mm(pss[:, cc:QG], ones_b, l1m[:, kb2, cc:QG], False, n_ == len(later) - 1,
                               reads=[("l1m", kb2), "cb"], writes=[psreg])
                        st.update(sbk=sbk, c0=c0, j=j)

                    def s2(kb=kb, st=st, qg=qg, h=h, po=po, poreg=poreg, first_po=first_po, last=(kb == nkb - 1)):
                        sbk, c0, j = st["sbk"], st["c0"], st["j"]
                        pss, psreg = ps[sbk], PS[sbk]
                        ab_ = argb[sbk]
                        op("dve", lambda e, ab_=ab_, pss=pss, kb=kb, c0=c0: e.tensor_tensor(
                            out=ab_[:, c0:QG], in0=pss[:, c0:QG], in1=lbuf[:, kb, c0:QG], op=ALU.subtract),
                           reads=[psreg, ("lbuf", kb)], writes=[("argb", sbk)])
                        wt_ = wTb[sbk]
                        op("act", lambda e, ab_=ab_, wt_=wt_, c0=c0: e.activation(out=wt_[:, c0:QG], in_=ab_[:, c0:QG], func=AF.Exp),
                           reads=[("argb", sbk)], writes=[("wTb", sbk)])
                        if j >= 0:
                            op("dve", lambda e, wt_=wt_, c0=c0: e.tensor_tensor(out=wt_[:, c0:c0 + 128], in0=wt_[:, c0:c0 + 128],
                                                                                 in1=tri_mul, op=ALU.mult),
                               reads=[("wTb", sbk), "cb"], writes=[("wTb", sbk)])
                        for i in range(c0 // 128, 2):
                            mm(po[:, i * 65:i * 65 + 64], wt_[:, i * 128:(i + 1) * 128], Vaug[:, kb, h, 0:64], first_po[0], False,
                               reads=[("wTb", sbk), "V"], writes=[poreg])
                            first_po[0] = False
                        if not last:
                            return
                        for i in range(2):
                            t = qg * 2 + i
                            op("act", lambda e, i=i, t=t, po=po, h=h: e.activation(out=mix_tok[:, t, h * 64:(h + 1) * 64],
                                                                                   in_=po[:, i * 65:i * 65 + 64], func=AF.Copy),
                               reads=[poreg], writes=[("mix_tok", t)])

                    items.append((s1, s2))
                run_pipelined(items)

    triL = sb("triL", [128, 128], BF16)
    op("pe", lambda e: e.transpose(ps[5].bitcast(BF16)[:, 0:128], tri_mul, ident_b), reads=["cb"], writes=[PS[5]])
    op("dve", lambda e: e.tensor_copy(out=triL, in_=ps[5].bitcast(BF16)[:, 0:128]), reads=[PS[5]], writes=["triL"])

    def mixer_C(l):
        lam_init = 0.8 - 0.6 * math.exp(-0.3 * l)
        set_v_ones()
        lamrow = rowp[:, RP_LAM - 2048:RP_LAM - 2048 + 128]
        lt = small[:, 64:128]
        ls = small[:, 24:28]
        op("dve", lambda e: e.tensor_tensor(out=lt[:, 0:32], in0=lamrow[:, 0:32], in1=lamrow[:, 32:64], op=ALU.mult),
           reads=["rowp"], writes=["lt"])
        op("dve", lambda e: e.tensor_tensor(out=lt[:, 32:64], in0=lamrow[:, 64:96], in1=lamrow[:, 96:128], op=ALU.mult),
           reads=["rowp"], writes=["lt"])
        op("dve", lambda e: e.reduce_sum(out=ls[:, 0:1], in_=lt[:, 0:32], axis=mybir.AxisListType.X), reads=["lt"], writes=["ls"])
        op("dve", lambda e: e.reduce_sum(out=ls[:, 1:2], in_=lt[:, 32:64], axis=mybir.AxisListType.X), reads=["lt"], writes=["ls"])
        op("act", lambda e: e.activation(out=ls[:, 0:2], in_=ls[:, 0:2], func=AF.Exp), reads=["ls"], writes=["ls"])
        op("dve", lambda e: e.tensor_tensor(out=ls[:, 2:3], in0=ls[:, 1:2], in1=ls[:, 0:1], op=ALU.subtract),
           reads=["ls"], writes=["ls"])
        op("dve", lambda e: e.tensor_scalar(out=ls[:, 2:3], in0=ls[:, 2:3], scalar1=-lam_init, scalar2=None, op0=ALU.add),
           reads=["ls"], writes=["ls"])
        nlam = ls[:, 2:3]
        if stopC <= 1:
            return
        for (c0, dst, gc, lns, dreg) in ((1540, bufQ, CP_QNC, 2.0, "bufQ"), (1796, bufK, CP_KNC, 0.0, "bufK")):
            for ch in range(2):
                wt, wreg = load_w(l, c0 + ch * 128, 128)
                for tg in range(4):
                    pb, preg = projT(wt, wreg, 128, tg)
                    o2_ = None
                    if dst is bufQ:
                        o2_ = (colp[:, CP_QNC1:CP_QNC1 + 1], bufIQ[:, ch, tg * 512:(tg + 1) * 512], "bufIQ")
                    qk_norm_store(pb, preg, 128, bones32, 1.0 / 32, colp[:, gc:gc + 1], lns,
                                  dst[:, ch, tg * 512:(tg + 1) * 512], dreg, out2=o2_)
        if stopC <= 2:
            return
        proj_v(l, 2052, 4, Vaug, "V")
        if stopC <= 3:
            return
        o1 = sb("o1c", [128, 64], F32)
        o2 = sb("o2c", [128, 64], F32)
        keep = {}
        specs = []
        for h in range(4):
            for c in range(2):
                hp, ch = (h % 2) * 64, h // 2
                qsrc = bufQ if c == 0 else bufIQ

                def extra(pss, psreg, qg, kb, c0, last, h=h):
                    mm(pss[:, c0:512], cb[0:2, CB_NSLOPE + h * 128:CB_NSLOPE + (h + 1) * 128],
                       cb[0:2, CB_POS + c0:CB_POS + 512], False, last, reads=["cb"], writes=[psreg])

                if "noextra" in cdbg:
                    extra = None
                specs.append(dict(
                    kT=lambda kb, hp=hp, ch=ch: bufK[hp:hp + 64, ch, kb * 128:(kb + 1) * 128],
                    qT=lambda q0, q1, hp=hp, ch=ch, qsrc=qsrc: qsrc[hp:hp + 64, ch, q0:q1],
                    kq_regs=["bufQ", "bufIQ", "bufK"], extra=extra, diag_mask=("nodiag" not in cdbg),
                    bias=lambda kb, qg, h=h: cf[:, CF_ALIBI + h * AL_N + (kb - 4 * qg) + AL_O:CF_ALIBI + h * AL_N + (kb - 4 * qg) + AL_O + 1],
                    bias_regs=["cf"],
                    V=lambda kb, h=h: Vaug[:, kb, h, :], v_regs=["V"]))

        stash = carve(A_SCR, [128, NT, 64], F32)

        def fin(si, qg, po, poreg):
            h, c = si // 2, si % 2
            if "nofin" in cdbg:
                return
            for i in range(4):
                t = qg * 4 + i
                op("dve", lambda e, i=i: e.reciprocal(out=recs[:, i:i + 1], in_=po[:, i * 65 + 64:i * 65 + 65]),
                   reads=[poreg], writes=["recs"])
                if c == 0:
                    op("dve", lambda e, i=i, t=t: e.tensor_scalar(out=stash[:, t, :], in0=po[:, i * 65:i * 65 + 64],
                                                                  scalar1=recs[:, i:i + 1], scalar2=None, op0=ALU.mult),
                       reads=[poreg, "recs"], writes=[("stash", t)])
                else:
                    op("dve", lambda e, i=i: e.tensor_scalar(out=o1, in0=po[:, i * 65:i * 65 + 64], scalar1=recs[:, i:i + 1],
                                                             scalar2=nlam, op0=ALU.mult, op1=ALU.mult),
                       reads=[poreg, "recs", "ls"], writes=["o1"])
                    op("dve", lambda e, t=t: e.tensor_tensor(out=o1, in0=o1, in1=stash[:, t, :], op=ALU.add),
                       reads=["o1", ("stash", t)], writes=["o1"])
                    op("act", lambda e: e.activation(out=o2, in_=o1, func=AF.Square, accum_out=small[:, 28:29]),
                       reads=["o1"], writes=["o2", "cs"])
                    op("act", lambda e: e.activation(out=small[:, 28:29], in_=small[:, 28:29], func=AF.Ln, bias=eps_t, scale=1.0 / 64),
                       reads=["cs", "eps"], writes=["cs"])
                    op("act", lambda e: e.activation(out=small[:, 28:29], in_=small[:, 28:29], func=AF.Exp,
                                                     bias=small[:, 21:22], scale=-0.5),
                       reads=["cs", "lnb2"], writes=["cs"])
                    op("dve", lambda e, t=t, h=h: e.scalar_tensor_tensor(out=mix_tok[:, t, h * 64:(h + 1) * 64], in0=o1,
                                                                         scalar=small[:, 28:29],
                                                                         in1=rowp[:, RP_SUBLN - 2048:RP_SUBLN - 2048 + 64],
                                                                         op0=ALU.mult, op1=ALU.mult),
                       reads=["o1", "cs", "rowp"], writes=[("mix_tok", t)])

        op("dve", lambda e: e.memset(small[:, 21:22], math.log(1.0 - lam_init)), writes=["lnb2"])
        attn_core(specs, fin)

    def mixer_D(l):
        set_v_ones()
        rtmp = sb("mixscr", [128, 512], F32)
        mx8 = sb("mx8", [128, 8], F32)
        thr = small[:, 128:144]
        iw = sb("iw", [128, NT, 8], F32)
        iwa = sb("iwa", [128, NT, 8], F32)
        iws = sb("iws", [128, NT, 8], F32)
        iqm = [sb(f"iqm{k_}", [128, 128], BF16) for k_ in range(8)]
        for ch in range(2):
            wt, wreg = load_w(l, 2308 + ch * 128, 128)
            for tg in range(4):
                pb, preg = projT(wt, wreg, 128, tg)
                qk_norm_store(pb, preg, 128, bones64, 1.0 / 64, colp[:, CP_QND:CP_QND + 1], 1.0,
                              bufQ[:, ch, tg * 512:(tg + 1) * 512], "bufQ")
        wt, wreg = load_w(l, 2564, 64)
        ik2 = wst_i[0] % 3
        wst_i[0] += 1
        wk2 = wst[ik2]
        for r in range(2):
            op("dve", lambda e, r=r, wt=wt: e.tensor_copy(out=wk2[:, :, r * 64:(r + 1) * 64], in_=wt), reads=[wreg], writes=[("wst", ik2)])
        for tg in range(4):
            pb, preg = projT(wk2, ("wst", ik2), 128, tg)
            qk_norm_store(pb, preg, 128, bones64, 1.0 / 64, colp[:, CP_KND:CP_KND + 1], 0.0,
                          bufK[:, 0, tg * 512:(tg + 1) * 512], "bufK")
        if stopD <= 1:
            return
        for ch in range(2):
            wt, wreg = load_w(l, 2692 + ch * 128, 128)
            for tg in range(4):
                pb, preg = projT(wt, wreg, 128, tg)
                op("dve", lambda e, pb=pb, ch=ch, tg=tg: e.tensor_scalar(out=bufIQ[:, ch, tg * 512:(tg + 1) * 512], in0=pb,
                                                                         scalar1=32 ** -0.5, scalar2=None, op0=ALU.mult),
                   reads=[preg], writes=["bufIQ"])
        wt, wreg = load_w(l, 2948, 32)
        i4 = wst_i[0] % 3
        wst_i[0] += 1
        w4 = wst[i4]
        for r in range(4):
            op("dve", lambda e, r=r, wt=wt: e.tensor_copy(out=w4[:, :, r * 32:(r + 1) * 32], in_=wt), reads=[wreg], writes=[("wst", i4)])
        for tg in range(4):
            pb, preg = projT(w4, ("wst", i4), 128, tg)
            op("act", lambda e, pb=pb, tg=tg: e.activation(out=bufIK[:, tg * 512:(tg + 1) * 512], in_=pb, func=AF.Copy),
               reads=[preg], writes=["bufIK"])
        wt, wreg = load_w(l, 2980, 8)
        for t in range(NT):
            b = pj_i[0] % 2
            pj_i[0] += 1
            for c in range(8):
                mm(ps[b][:, 0:8], hT[:, c, t * 128:(t + 1) * 128], wt[:, c, :], c == 0, c == 7,
                   reads=[wreg, ("hT", t // 4)], writes=[PS[b]])
            op("dve", lambda e, t=t, b=b: e.tensor_scalar(out=iw[:, t, :], in0=ps[b][:, 0:8], scalar1=8 ** -0.5, scalar2=None, op0=ALU.mult),
               reads=[PS[b]], writes=["iw"])
        op("act", lambda e: e.activation(out=iwa, in_=iw, func=AF.Abs), reads=["iw"], writes=["iwa"])
        op("act", lambda e: e.activation(out=iws, in_=iw, func=AF.Sign), reads=["iw"], writes=["iws"])
        proj_v(l, 2628, 1, Vaug, "V")

        if stopD <= 2:
            return
        QG = 256
        scoreb = [carve(A_MIXT, [128, S], F32), carve(A_MIXT + 8192, [128, S], F32)]
        mbvb = [carve(A_MIXT + 16384, [128, 2, S], BF16), carve(A_MIXT + 24576, [128, 2, S], BF16)]
        rt = [rtmp, carve(A_BUFK + 4096, [128, 512], F32)]
        posb = carve(A_W, [128, 2, 132], F32)
        den = small[:, 144:148]
        rt_i = [0]

        def index_topk(qg):
            for i in range(2):
                t = qg * 2 + i
                nk = (t + 1) * 128
                score, sreg = scoreb[t % 2], ("score", t % 2)
                mbv = mbvb[qg % 2]
                for hh in range(8):
                    g, ch = hh % 4, hh // 4
                    op("pool", lambda e, hh=hh, g=g, ch=ch, t=t: e.tensor_scalar(
                        out=iqm[hh], in0=bufIQ[:, ch, t * 128:(t + 1) * 128], scalar1=cf[:, CF_GMASK + g:CF_GMASK + g + 1],
                        scalar2=None, op0=ALU.mult), reads=["bufIQ", "cf"], writes=[("iqm", hh)])
                for k0 in range(0, nk, 512):
                    kn = min(512, nk - k0)
                    for hh in range(8):
                        b = pj_i[0] % 2
                        pj_i[0] += 1
                        r = rt_i[0] % 2
                        rt_i[0] += 1
                        mm(ps[b][:, 0:kn], iqm[hh], bufIK[:, k0:k0 + kn], True, True,
                           reads=[("iqm", hh), "bufIK"], writes=[PS[b]])
                        op("act", lambda e, b=b, kn=kn, t=t, hh=hh, r=r: e.activation(out=rt[r][:, 0:kn], in_=ps[b][:, 0:kn], func=AF.Relu,
                                                                                      scale=iwa[:, t, hh:hh + 1]),
                           reads=[PS[b], "iwa"], writes=[("rtmp", r)])
                        if hh == 0:
                            op("act", lambda e, k0=k0, kn=kn, t=t, r=r, score=score: e.activation(
                                out=score[:, k0:k0 + kn], in_=rt[r][:, 0:kn], func=AF.Copy, scale=iws[:, t, 0:1]),
                               reads=[("rtmp", r), "iws"], writes=[sreg])
                        else:
                            op("act", lambda e, kn=kn, t=t, hh=hh, r=r: e.activation(
                                out=rt[r][:, 0:kn], in_=rt[r][:, 0:kn], func=AF.Copy, scale=iws[:, t, hh:hh + 1]),
                               reads=[("rtmp", r), "iws"], writes=[("rtmp", r)])
                            op("pool", lambda e, k0=k0, kn=kn, r=r, score=score: e.tensor_tensor(
                                out=score[:, k0:k0 + kn], in0=score[:, k0:k0 + kn], in1=rt[r][:, 0:kn], op=ALU.add),
                               reads=[("rtmp", r), sreg], writes=[sreg])
                op("pool", lambda e, t=t, score=score: e.tensor_tensor(out=score[:, t * 128:(t + 1) * 128], in0=score[:, t * 128:(t + 1) * 128],
                                                                       in1=cf[:, CF_NEGTRI:CF_NEGTRI + 128], op=ALU.add),
                   reads=[sreg, "cf"], writes=[sreg])
                if t < 2:
                    op("dve", lambda e, i=i, nk=nk, score=score, mbv=mbv: e.tensor_scalar(
                        out=mbv[:, i, 0:nk], in0=score[:, 0:nk], scalar1=-1e29, scalar2=NEG, op0=ALU.is_lt, op1=ALU.mult),
                       reads=[sreg], writes=[("mbv", qg % 2, i)])
                else:
                    for r_ in range(32):
                        op("dve", lambda e, score=score, nk=nk: e.max(out=mx8, in_=score[:, 0:nk]), reads=[sreg], writes=["mx8"])
                        op("dve", lambda e, score=score, nk=nk: e.match_replace(out=score[:, 0:nk], in_to_replace=mx8,
                                                                                in_values=score[:, 0:nk], imm_value=-3e38),
                           reads=[sreg, "mx8"], writes=[sreg])
                    op("dve", lambda e, i=i, nk=nk, score=score, mbv=mbv: e.tensor_scalar(
                        out=mbv[:, i, 0:nk], in0=score[:, 0:nk], scalar1=-1e35, scalar2=NEG, op0=ALU.is_ge, op1=ALU.mult),
                       reads=[sreg], writes=[("mbv", qg % 2, i)])

        def attention(qg):
            nkb = 2 * (qg + 1)
            mbv = mbvb[qg % 2]
            items = []
            for h in range(4):
                hp, ch = (h % 2) * 64, h // 2
                pb_ = 2 + (po_i[0] % 2)
                po_i[0] += 1
                po, poreg = ps[pb_], PS[pb_]
                first_po = [True]
                for kb in range(nkb):
                    st = {}

                    def s1(h=h, hp=hp, ch=ch, kb=kb, st=st):
                        j = kb - 2 * qg
                        c0 = 0 if j < 0 else j * 128
                        sbk = pj_i[0] % 2
                        pj_i[0] += 1
                        pss, psreg = ps[sbk], PS[sbk]
                        mm(pss[:, c0:QG], bufK[hp:hp + 64, 0, kb * 128:(kb + 1) * 128], bufQ[hp:hp + 64, ch, qg * QG + c0:(qg + 1) * QG],
                           True, False, reads=["bufQ", "bufK"], writes=[psreg])
                        mm(pss[:, c0:QG], cb[0:2, CB_NSLOPE + (4 + h) * 128:CB_NSLOPE + (5 + h) * 128],
                           cb[0:2, CB_POS + c0:CB_POS + QG], False, False, reads=["cb"], writes=[psreg])
                        for i in range(c0 // 128, 2):
                            mm(pss[:, i * 128:(i + 1) * 128], mbv[:, i, kb * 128:(kb + 1) * 128], ident_b, False, i == 1,
                               reads=[("mbv", qg % 2, i), "cb"], writes=[psreg])
                        st.update(sbk=sbk, c0=c0)

                    def s2(h=h, kb=kb, st=st, po=po, poreg=poreg, first_po=first_po, pi=pb_ - 2, last=(kb == nkb - 1)):
                        sbk, c0 = st["sbk"], st["c0"]
                        pss, psreg = ps[sbk], PS[sbk]
                        pt = pT[sbk]
                        dlt = kb - 2 * qg
                        bias_ap = cf[:, CF_ALIBI + (4 + h) * AL_N + dlt + AL_O:CF_ALIBI + (4 + h) * AL_N + dlt + AL_O + 1]
                        op("act", lambda e, pt=pt, pss=pss, c0=c0, bias_ap=bias_ap: e.activation(
                            out=pt[:, c0:QG], in_=pss[:, c0:QG], func=AF.Exp, bias=bias_ap),
                           reads=[psreg, "cf"], writes=[("pT", sbk)])
                        for i in range(c0 // 128, 2):
                            mm(po[:, i * 65:(i + 1) * 65], pt[:, i * 128:(i + 1) * 128], Vaug[:, kb, 0, :], first_po[0], False,
                               reads=[("pT", sbk), "V"], writes=[poreg])
                            first_po[0] = False
                        if not last:
                            return
                        dn = den[:, pi * 2:pi * 2 + 2]
                        dsrc = po[:, 0:130].rearrange("p (i c) -> p i c", c=65)[:, :, 64]
                        op("act", lambda e, dn=dn, dsrc=dsrc: e.activation(out=dn, in_=dsrc, func=AF.Ln, bias=1e-30),
                           reads=[poreg], writes=[("den", pi)])
                        op("act", lambda e, dn=dn: e.activation(out=dn, in_=dn, func=AF.Exp, scale=-1.0),
                           reads=[("den", pi)], writes=[("den", pi)])
                        for i in range(2):
                            t = qg * 2 + i
                            op("act", lambda e, i=i, t=t, h=h, po=po, dn=dn: e.activation(
                                out=mix_tok[:, t, h * 64:(h + 1) * 64], in_=po[:, i * 65:i * 65 + 64], func=AF.Copy, scale=dn[:, i:i + 1]),
                               reads=[poreg, ("den", pi)], writes=[("mix_tok", t)])

                    items.append((s1, s2))
            run_pipelined(items)

        nqg = S // QG
        for qg in range(nqg):
            index_topk(qg)
            if stopD > 3 and qg >= 1:
                attention(qg - 1)
        if stopD > 3:
            attention(nqg - 1)

    def phase_moe(l):
        NE = 2
        M_W = 0
        wb = [[dict(w1=carve(M_W + (s_ * NE + j) * 12288, [128, 8, 256], BF16),
                    w3=carve(M_W + (s_ * NE + j) * 12288 + 4096, [128, 8, 256], BF16),
                    w2f=carve(M_W + (s_ * NE + j) * 12288 + 8192, [128, 2, D], BF16)) for j in range(NE)] for s_ in range(2)]
        M_O = 2 * NE * 12288
        w2stage = [carve(M_O, [128, 2, D], F32)]
        hid = carve(M_O + 8192, [128, NE * 2, 512], BF16)
        t1 = [carve(M_O + 12288, [128, 512], F32), carve(M_O + 14336, [128, 512], F32)]
        t2 = [carve(M_O + 16384, [128, 512], F32), carve(M_O + 18432, [128, 512], F32)]
        chi_ = carve(M_O + 20480, [32, S], BF16)
        clo_ = carve(M_O + 24576, [32, S], BF16)
        comb = carve(M_O + 28672, [128, NT, 32], F32)
        lg = carve(M_O + 30720, [128, 64], F32)[:, 0:36]
        wr = carve(M_O + 30976, [128, 8, 36], BF16)
        rs = small[:, 160:200]
        ohb = [sb(f"ohb_{i_}", [32, 128], BF16) for i_ in range(2)]
        dma("sp", "gate", lambda e: e.dma_start(out=gate, in_=modb_d[l][:, 5 * D:6 * D]), reads=[("modb", l, 5)], writes=["gate"])
        dma("pool", "wr", lambda e: e.dma_start(out=wr, in_=w_r_d[l].rearrange("(c p) n -> p c n", p=128)), writes=["wr"])
        brow = rowp[:, RP_BR - 2048:RP_BR - 2048 + 36]
        for t in range(NT):
            b = pj_i[0] % 2
            pj_i[0] += 1
            for c in range(8):
                mm(ps[b][:, 0:36], hT[:, c, t * 128:(t + 1) * 128], wr[:, c, :], c == 0, c == 7,
                   reads=["wr", ("hT", t // 4)], writes=[PS[b]])
            op("dve", lambda e, b=b: e.tensor_tensor(out=lg, in0=ps[b][:, 0:36], in1=brow, op=ALU.add),
               reads=[PS[b], "rowp"], writes=["lg"])
            R_ = ["lg", "rs"]
            gmax, gsum, v1, v2, w1c, w2c = (rs[:, k:k + 1] for k in range(6))
            goh = rs[:, 8:12]
            gex = rs[:, 12:16]
            op("dve", lambda e: e.reduce_max(out=gmax, in_=lg[:, 0:4], axis=mybir.AxisListType.X), reads=R_, writes=["rs"])
            op("dve", lambda e: e.tensor_scalar(out=goh, in0=lg[:, 0:4], scalar1=gmax, scalar2=None, op0=ALU.is_ge), reads=R_, writes=["rs"])
            op("dve", lambda e: e.tensor_scalar(out=gex, in0=lg[:, 0:4], scalar1=gmax, scalar2=None, op0=ALU.subtract), reads=R_, writes=["rs"])
            op("act", lambda e: e.activation(out=gex, in_=gex, func=AF.Exp, accum_out=gsum), reads=R_, writes=["rs"])
            op("dve", lambda e: e.reciprocal(out=gsum, in_=gsum), reads=R_, writes=["rs"])
            op("dve", lambda e: e.tensor_scalar(out=gex, in0=goh, scalar1=-1.0, scalar2=1e9, op0=ALU.add, op1=ALU.mult), reads=R_, writes=["rs"])
            for g in range(4):
                op("dve", lambda e, g=g: e.tensor_scalar(out=lg[:, 4 + g * 8:12 + g * 8], in0=lg[:, 4 + g * 8:12 + g * 8],
                                                         scalar1=gex[:, g:g + 1], scalar2=None, op0=ALU.add), reads=R_, writes=["lg"])
            el = lg[:, 4:36]
            eq1 = lnv[:, 0:32]
            eq2 = lnv[:, 32:64]
            el2 = lnv[:, 64:96]
            R2 = ["lg", "rs", "lnv"]
            op("dve", lambda e: e.reduce_max(out=v1, in_=el, axis=mybir.AxisListType.X), reads=R2, writes=["rs"])
            op("dve", lambda e: e.tensor_scalar(out=eq1, in0=el, scalar1=v1, scalar2=None, op0=ALU.is_ge), reads=R2, writes=["lnv"])
            op("dve", lambda e: e.scalar_tensor_tensor(out=el2, in0=eq1, scalar=-1e9, in1=el, op0=ALU.mult, op1=ALU.add), reads=R2, writes=["lnv"])
            op("dve", lambda e: e.reduce_max(out=v2, in_=el2, axis=mybir.AxisListType.X), reads=R2, writes=["rs"])
            op("dve", lambda e: e.tensor_scalar(out=eq2, in0=el2, scalar1=v2, scalar2=None, op0=ALU.is_ge), reads=R2, writes=["lnv"])
            op("dve", lambda e: e.tensor_tensor(out=w2c, in0=v1, in1=v2, op=ALU.subtract), reads=R2, writes=["rs"])
            op("act", lambda e: e.activation(out=w2c, in_=w2c, func=AF.Exp), reads=R2, writes=["rs"])
            op("dve", lambda e: e.tensor_scalar(out=w2c, in0=w2c, scalar1=1.0, scalar2=None, op0=ALU.add), reads=R2, writes=["rs"])
            op("dve", lambda e: e.reciprocal(out=w2c, in_=w2c), reads=R2, writes=["rs"])
            op("dve", lambda e: e.tensor_scalar(out=w1c, in0=w2c, scalar1=-1.0, scalar2=1.0, op0=ALU.mult, op1=ALU.add), reads=R2, writes=["rs"])
            op("dve", lambda e: e.tensor_tensor(out=w1c, in0=w1c, in1=gsum, op=ALU.mult), reads=R2, writes=["rs"])
            op("dve", lambda e: e.tensor_tensor(out=w2c, in0=w2c, in1=gsum, op=ALU.mult), reads=R2, writes=["rs"])
            op("dve", lambda e: e.tensor_scalar(out=eq1, in0=eq1, scalar1=w1c, scalar2=None, op0=ALU.mult), reads=R2, writes=["lnv"])
            op("dve", lambda e, t=t: e.scalar_tensor_tensor(out=comb[:, t, :], in0=eq2, scalar=w2c, in1=eq1, op0=ALU.mult, op1=ALU.add),
               reads=R2, writes=["comb"])
        chl = carve(M_O + 12288, [128, NT, 64], BF16)
        ctmp = carve(M_O + 16384, [128, NT, 32], F32)
        op("dve", lambda e: e.tensor_copy(out=chl[:, :, 0:32], in_=comb), reads=["comb"], writes=["chl"])
        op("dve", lambda e: e.tensor_tensor(out=ctmp, in0=comb, in1=chl[:, :, 0:32], op=ALU.subtract), reads=["comb", "chl"], writes=["ctmp"])
        op("dve", lambda e: e.tensor_copy(out=chl[:, :, 32:64], in_=ctmp), reads=["ctmp"], writes=["chl"])
        pstb = ps[5].bitcast(BF16)
        for t in range(NT):
            op("pe", lambda e, t=t: e.transpose(pstb[0:64, (t % 8) * 128:(t % 8 + 1) * 128], chl[:, t, :], ident_b), reads=["chl", "cb"], writes=[PS[5]])
            if t % 8 == 7:
                t0_ = (t // 8) * 1024
                op("dve", lambda e, t0_=t0_: e.tensor_copy(out=chi_[:, t0_:t0_ + 1024], in_=pstb[0:32, :]), reads=[PS[5]], writes=["chi_"])
                op("dve", lambda e, t0_=t0_: e.tensor_copy(out=clo_[:, t0_:t0_ + 1024], in_=pstb[32:64, :]), reads=[PS[5]], writes=["clo_"])
        em.barrier()

        nsets = 32 // NE

        def load_set(s_):
            sl = s_ % 2
            for j in range(NE):
                e_ = s_ * NE + j
                w = wb[sl][j]
                dma("pool", f"mw{sl}{j}a", lambda e, w=w, e_=e_: e.dma_start(out=w["w1"], in_=w1_d[l][e_].rearrange("(c p) n -> p c n", p=128)),
                    writes=[("w13", sl, j)])
                dma("pool", f"mw{sl}{j}b", lambda e, w=w, e_=e_: e.dma_start(out=w["w3"], in_=w3_d[l][e_].rearrange("(c p) n -> p c n", p=128)),
                    writes=[("w13", sl, j)])
                st = w2stage[0]
                dma("sp", "mw2s", lambda e, st=st, e_=e_: e.dma_start(out=st, in_=w2_d[l][e_].rearrange("(c p) n -> p c n", p=128)),
                    writes=[("w2st", 0)])
                for c in range(2):
                    op("pool", lambda e, w=w, st=st, c=c: e.tensor_tensor(out=w["w2f"][:, c, :], in0=st[:, c, :], in1=gate, op=ALU.mult),
                       reads=[("w2st", 0), "gate"], writes=[("w2", sl, j)])

        hidb = [hid, carve(M_O + 28672, [128, NE * 2, 512], BF16)]
        steps = [(s_, tg) for s_ in range(nsets) for tg in range(4)]

        def stepA(i):
            s_, tg = steps[i]
            sl = s_ % 2
            hb = hidb[i % 2]
            for j in range(NE):
                e_ = s_ * NE + j
                w = wb[sl][j]
                n_ = i * NE + j
                cbk = 4 + n_ % 2
                oh = ohb[n_ % 2]
                op("dve", lambda e, oh=oh, e_=e_: e.tensor_scalar(out=oh, in0=ones_b[0:32, :], scalar1=ident_f[0:32, e_:e_ + 1],
                                                                  scalar2=None, op0=ALU.mult),
                   reads=["cb", "cf"], writes=[("ohb", n_ % 2)])
                mm(ps[cbk], oh, chi_[:, tg * 512:(tg + 1) * 512], True, False, reads=[("ohb", n_ % 2), "chi_"], writes=[PS[cbk]])
                mm(ps[cbk], oh, clo_[:, tg * 512:(tg + 1) * 512], False, True, reads=[("ohb", n_ % 2), "clo_"], writes=[PS[cbk]])
                for fc in range(2):
                    k = pj_i[0] % 2
                    pj_i[0] += 1
                    p1, p3 = ps[k], ps[2 + k]
                    for c in range(8):
                        mm(p1, w["w1"][:, c, fc * 128:(fc + 1) * 128], hT[:, c, tg * 512:(tg + 1) * 512], c == 0, c == 7,
                           reads=[("w13", sl, j), ("hT", tg)], writes=[PS[k]], inc=(c == 7))
                    for c in range(8):
                        mm(p3, w["w3"][:, c, fc * 128:(fc + 1) * 128], hT[:, c, tg * 512:(tg + 1) * 512], c == 0, c == 7,
                           reads=[("w13", sl, j), ("hT", tg)], writes=[PS[2 + k]], inc=(c == 7))
                    op("act", lambda e, k=k, p1=p1: e.activation(out=t1[k], in_=p1, func=AF.Silu), reads=[PS[k]], writes=[("t1", k)])
                    op("dve", lambda e, k=k, p3=p3: e.tensor_tensor(out=t2[k], in0=t1[k], in1=p3, op=ALU.mult),
                       reads=[("t1", k), PS[2 + k]], writes=[("t2", k)])
                    op("dve", lambda e, k=k, j=j, fc=fc, hb=hb, cbk=cbk: e.tensor_tensor(out=hb[:, j * 2 + fc, :], in0=t2[k], in1=ps[cbk], op=ALU.mult),
                       reads=[("t2", k), PS[cbk]], writes=[("hid", i % 2, j * 2 + fc)])

        def stepB(i):
            s_, tg = steps[i]
            sl = s_ % 2
            hb = hidb[i % 2]
            for ti in range(4):
                t = tg * 4 + ti
                for hf in range(2):
                    b = 6 + (ti * 2 + hf) % 2
                    n = 0
                    for j in range(NE):
                        for fc in range(2):
                            mm(ps[b], hb[:, j * 2 + fc, ti * 128:(ti + 1) * 128], wb[sl][j]["w2f"][:, fc, hf * 512:(hf + 1) * 512],
                               n == 0, n == NE * 2 - 1, reads=[("hid", i % 2, j * 2 + fc), ("w2", sl, j)], writes=[PS[b]],
                               inc=(n == NE * 2 - 1))
                            n += 1
                    op("dve", lambda e, t=t, hf=hf, b=b: e.tensor_tensor(out=x[:, t, hf * 512:(hf + 1) * 512],
                                                                         in0=x[:, t, hf * 512:(hf + 1) * 512], in1=ps[b], op=ALU.add),
                       reads=[PS[b], ("x", t)], writes=[("x", t)])

        load_set(0)
        load_set(1)
        stepA(0)
        for i, (s_, tg) in enumerate(steps):
            if tg == 0 and s_ >= 1 and s_ + 1 < nsets and "moe_nodma" not in cdbg:
                load_set(s_ + 1)
            if i + 1 < len(steps):
                stepA(i + 1)
            stepB(i)

    set_lnb()

    for l in range(nl):
        phase_mod(l)
        em.barrier()
        phase_norm(l, 0)
        dma("sp", "gate", lambda e, l=l: e.dma_start(out=gate, in_=modb_d[l][:, 2 * D:3 * D]), reads=[("modb", l, 2)], writes=["gate"])
        em.barrier()
        for m, name in enumerate("ABCD"):
            if name in mixers:
                {"A": mixer_A, "B": mixer_B, "C": mixer_C, "D": mixer_D}[name](l)
                em.barrier()
                mixer_out(l, m)
                em.barrier()
        phase_norm(l, 1)
        em.barrier()
        if do_moe:
            phase_moe(l)
            em.barrier()
    for t in range(NT):
        dma("sp", f"xo{t % 4}", lambda e, t=t: e.dma_start(out=out_d[t * 128:(t + 1) * 128, :], in_=x[:, t, :]),
            reads=[("x", t)], writes=[("out", t)])
    em.wait_all("sp", [("out", t) for t in range(NT)])
    em.finish()
    return nc


def prep_inputs(inp, n_cores=8):
    f = lambda a: np.ascontiguousarray(np.asarray(a, dtype=np.float32))
    cbv, cfv = make_consts()
    rowp = np.zeros((L, 128, NRP), np.float32)
    colp = np.zeros((L, 128, NCP), np.float32)
    for l in range(L):
        row = np.concatenate([f(inp["norm1_g"])[l], f(inp["norm2_g"])[l], f(inp["subln_g"])[l],
                              f(inp["b_group"])[l], f(inp["b_expert"])[l],
                              f(inp["lam_q1"])[l], f(inp["lam_k1"])[l], f(inp["lam_q2"])[l], f(inp["lam_k2"])[l]])
        rowp[l] = np.broadcast_to(row[None, :], (128, NRP))
        colp[l, :, CP_QNA] = np.tile(f(inp["qn_a"])[l], 2)
        colp[l, :, CP_KNA] = np.tile(f(inp["kn_a"])[l], 2)
        qc = np.tile(f(inp["qn_c"])[l], 4)
        m0 = (np.arange(128) % 64) // 32 == 0
        colp[l, m0, CP_QNC] = qc[m0]
        colp[l, ~m0, CP_QNC1] = qc[~m0]
        colp[l, :, CP_KNC] = np.tile(f(inp["kn_c"])[l], 4)
        colp[l, :, CP_QND] = np.tile(f(inp["qn_d"])[l], 2)
        colp[l, :, CP_KND] = np.tile(f(inp["kn_d"])[l], 2)
        colp[l, :, CP_BF] = np.tile(f(inp["b_f"])[l], 32)
        colp[l, :, CP_BETA:CP_BETA + 8] = f(inp["mix_beta"])[l].reshape(8, 128).T
    shared = dict(
        ada_w=f(inp["ada_w"]),
        ada_b=np.ascontiguousarray(np.broadcast_to(f(inp["ada_b"])[:, None, :], (L, 128, 6 * D))),
        w_in=f(inp["w_in"]), w_out=f(inp["w_out"]),
        w_r=np.ascontiguousarray(np.concatenate([f(inp["w_group"]), f(inp["w_expert"])], axis=-1)),
        w1=f(inp["w1"]).reshape(L, 32, D, 256), w3=f(inp["w3"]).reshape(L, 32, D, 256),
        w2=f(inp["w2"]).reshape(L, 32, 256, D),
        rowp=rowp, colp=colp, cb=cbv, cf=cfv)
    xs, cs = f(inp["x"]), f(inp["c"])
    maps = []
    for b in range(n_cores):
        m = dict(shared)
        m["x"] = xs[b]
        m["cT"] = np.ascontiguousarray(cs[b].reshape(8, 128).T)
        maps.append(m)
    return maps


_NC_CACHE = {}


def kernel(**inputs):
    if "nc" not in _NC_CACHE:
        nc = bass.Bass("TRN2", target_bir_lowering=False)
        _NC_CACHE["nc"] = build(nc, {})
    nc = _NC_CACHE["nc"]
    maps = prep_inputs(inputs, 8)
    res = run_bass_kernel_spmd(nc, maps, core_ids=list(range(8)))
    return np.stack([np.asarray(r["out"], dtype=np.float32) for r in res.results], axis=0)
```

```python
import math
import numpy as np
import ml_dtypes
import concourse.bass as bass
import concourse.mybir as mybir
from concourse.bass_utils import run_bass_kernel_spmd

F32 = mybir.dt.float32
BF16 = mybir.dt.bfloat16
ALU = mybir.AluOpType
AF = mybir.ActivationFunctionType

D = 1024
S = 2048
NT = 16
L = 2
P_IN = 2988
EPS = 1e-6
NEG = -30000.0
SEM_LIMIT = 20000


class Ticket:
    __slots__ = ("sem", "val", "eng")

    def __init__(self, sem, val, eng):
        self.sem, self.val, self.eng = sem, val, eng


class Region:
    __slots__ = ("w", "r")

    def __init__(self):
        self.w = None
        self.r = []


class Emitter:
    ENGS = ("pe", "act", "dve", "pool", "sp")

    def __init__(self, nc):
        self.nc = nc
        self.prog = {e: [] for e in self.ENGS}
        self.cur_sem = {}
        self.cnt = {}
        self.nsem = 0
        for e in self.ENGS:
            self._new_eng_sem(e)
        self.waited = {}
        self.chan = {}
        self.regions = {}
        self.pending_dma = []

    def _alloc_sem(self, name):
        self.nsem += 1
        return self.nc.alloc_semaphore(name=f"{name}_{self.nsem}")

    def _new_eng_sem(self, e):
        self.cur_sem[e] = self._alloc_sem("e" + e)
        self.cnt[e] = 0

    def R(self, key):
        r = self.regions.get(key)
        if r is None:
            r = self.regions[key] = Region()
        return r

    def _regs(self, lst):
        return [x if isinstance(x, Region) else self.R(x) for x in (lst or [])]

    def _waits_for(self, eng, deps):
        best = {}
        for t in deps:
            if t.eng == "pe" and eng == "pe":
                continue
            k = id(t.sem)
            if k not in best or best[k].val < t.val:
                best[k] = t
        waits = []
        for k, t in best.items():
            if t.eng != "dma":
                wk = (eng, k)
                if self.waited.get(wk, 0) >= t.val:
                    continue
                self.waited[wk] = t.val
            waits.append(t)
        return waits

    def _collect(self, eng, reads, writes):
        deps = []
        for r in reads:
            if r.w is not None:
                deps.append(r.w)
        for w in writes:
            if w.w is not None:
                deps.append(w.w)
            deps.extend(w.r)
        return self._waits_for(eng, deps)

    def _update(self, ticket, reads, writes):
        for w in writes:
            w.w = ticket
            w.r = []
        for r in reads:
            if r not in writes:
                r.r.append(ticket)

    def op(self, eng, fn, reads=None, writes=None, inc=True):
        reads, writes = self._regs(reads), self._regs(writes)
        waits = self._collect(eng, reads, writes)
        if self.cnt[eng] >= SEM_LIMIT:
            self._new_eng_sem(eng)
        sem = self.cur_sem[eng]
        if inc:
            self.cnt[eng] += 1
            ticket = Ticket(sem, self.cnt[eng], eng)
        else:
            assert eng == "pe"
            ticket = Ticket(sem, self.cnt[eng] + 1, eng)

        def emit(e, waits=waits, fn=fn, sem=sem, inc=inc):
            for t in waits:
                e.wait_ge(t.sem, t.val)
            if inc:
                fn(e).then_inc(sem, 1)
            else:
                fn(e)

        self.prog[eng].append(emit)
        self._update(ticket, reads, writes)
        return ticket

    def dma(self, queue, chan, fn, reads=None, writes=None):
        reads, writes = self._regs(reads), self._regs(writes)
        waits = self._collect(queue, reads, writes)
        c = self.chan.get(chan)
        if c is None:
            c = self.chan[chan] = [self._alloc_sem("d"), 0]
        c[1] += 16
        ticket = Ticket(c[0], c[1], "dma")
        sem = c[0]

        def emit(e, waits=waits, fn=fn, sem=sem):
            for t in waits:
                e.wait_ge(t.sem, t.val)
            fn(e).then_inc(sem, 16)

        self.prog[queue].append(emit)
        self._update(ticket, reads, writes)
        self.pending_dma.append(ticket)
        return ticket

    def barrier(self):
        deps = [Ticket(self.cur_sem[e], self.cnt[e], e) for e in self.ENGS if self.cnt[e] > 0]
        last = {}
        for t in self.pending_dma:
            last[id(t.sem)] = t if (id(t.sem) not in last or last[id(t.sem)].val < t.val) else last[id(t.sem)]
        dmas = list(last.values())
        self.pending_dma = dmas
        for eng in self.ENGS:
            ws = [t for t in deps if t.eng != eng] + dmas

            def emit(e, ws=ws):
                for t in ws:
                    e.wait_ge(t.sem, t.val)

            self.prog[eng].append(emit)
            for t in deps:
                if t.eng != eng:
                    self.waited[(eng, id(t.sem))] = max(self.waited.get((eng, id(t.sem)), 0), t.val)

    def wait_all(self, eng, regions):
        regs = self._regs(regions)
        waits = self._collect(eng, regs, regs)

        def emit(e, waits=waits):
            for t in waits:
                e.wait_ge(t.sem, t.val)

        self.prog[eng].append(emit)

    def finish(self):
        nc = self.nc
        with nc.Block() as block:
            @block.tensor
            def _(e):
                for f in self.prog["pe"]:
                    f(e)

            @block.scalar
            def _(e):
                for f in self.prog["act"]:
                    f(e)

            @block.vector
            def _(e):
                for f in self.prog["dve"]:
                    f(e)

            @block.gpsimd
            def _(e):
                for f in self.prog["pool"]:
                    f(e)

            @block.sync
            def _(e):
                for f in self.prog["sp"]:
                    f(e)


CB_IDENT, CB_TRIADD, CB_TRIMUL, CB_B64, CB_B32, CB_ONES, CB_NSLOPE, CB_POS = (
    0, 128, 256, 384, 512, 640, 768, 1792)
NCB = 2304
CF_IDENT, CF_ALIBI, CF_NEGTRI, CF_GMASK = 0, 128, 288, 416
NCF = 420
AL_N, AL_O = 20, 16
SLOPES = [2.0 ** (-8.0 * i / 8) for i in range(1, 9)]
SLOPES_CD = SLOPES[0::2] + SLOPES[1::2]


def make_consts():
    cb = np.zeros((128, NCB), np.float32)
    p = np.arange(128)
    cb[:, CB_IDENT:CB_IDENT + 128] = np.eye(128)
    cb[:, CB_TRIADD:CB_TRIADD + 128] = np.where(p[:, None] <= p[None, :], 0.0, NEG)
    cb[:, CB_TRIMUL:CB_TRIMUL + 128] = (p[:, None] < p[None, :]).astype(np.float32)
    cb[:, CB_B64:CB_B64 + 128] = (p[:, None] // 64 == p[None, :] // 64)
    cb[:, CB_B32:CB_B32 + 128] = (p[:, None] // 32 == p[None, :] // 32)
    cb[:, CB_ONES:CB_ONES + 128] = 1.0
    for h in range(8):
        cb[0:2, CB_NSLOPE + h * 128:CB_NSLOPE + (h + 1) * 128] = -SLOPES_CD[h]
    t = np.arange(512)
    cb[0, CB_POS:CB_POS + 512] = 128 * (t // 128)
    cb[1, CB_POS:CB_POS + 512] = t % 128
    cf = np.zeros((128, NCF), np.float32)
    cf[:, CF_IDENT:CF_IDENT + 128] = np.eye(128)
    for h in range(8):
        for d in range(-16, 4):
            cf[:, CF_ALIBI + h * AL_N + d + AL_O] = SLOPES_CD[h] * (p + 128 * d)
    cf[:, CF_NEGTRI:CF_NEGTRI + 128] = np.where(p[None, :] <= p[:, None], 0.0, -1e30)
    for g in range(4):
        cf[:, CF_GMASK + g] = (p // 32 == g)
    return cb.astype(ml_dtypes.bfloat16), cf


def make_alc():
    t = np.arange(S)
    a = np.zeros((4, 2, S), np.float32)
    for h in range(4):
        a[h, 0] = -SLOPES_CD[h] * 128 * ((t % 512) // 128)
        a[h, 1] = -SLOPES_CD[h] * (t % 128)
    return a.astype(ml_dtypes.bfloat16)


RP_N1, RP_N2, RP_SUBLN, RP_BR, RP_LAM = 0, 1024, 2048, 2112, 2148
NRP = 2276
CP_QNA, CP_KNA, CP_QNC, CP_KNC, CP_QND, CP_KND, CP_BF, CP_BETA, CP_QNC1 = 0, 1, 2, 3, 4, 5, 6, 7, 15
NCP = 16


def build(nc, cfg):
    nl = cfg.get("n_layers", L)
    mixers = cfg.get("mixers", "ABCD")
    do_moe = cfg.get("moe", True)
    stopC = cfg.get("stopC", 99)
    stopD = cfg.get("stopD", 99)
    cdbg = cfg.get("cdbg", "")
    em = Emitter(nc)
    op, dma = em.op, em.dma

    def dram(name, shape, dt=F32, kind="ExternalInput"):
        return nc.dram_tensor(name, list(shape), dt, kind=kind).ap()

    x_d = dram("x", [S, D])
    out_d = dram("out", [S, D], kind="ExternalOutput")
    cT_d = dram("cT", [128, 8])
    ada_w_d = dram("ada_w", [L, D, 6 * D])
    ada_b_d = dram("ada_b", [L, 128, 6 * D])
    w_in_d = dram("w_in", [L, D, P_IN])
    w_out_d = dram("w_out", [L, D, D])
    w_r_d = dram("w_r", [L, D, 36])
    w1_d = dram("w1", [L, 32, D, 256])
    w3_d = dram("w3", [L, 32, D, 256])
    w2_d = dram("w2", [L, 32, 256, D])
    rowp_d = dram("rowp", [L, 128, NRP])
    colp_d = dram("colp", [L, 128, NCP])
    cb_d = dram("cb", [128, NCB], BF16)
    cf_d = dram("cf", [128, NCF])
    alc_d = dram("alc", [4, 2, S], BF16)
    onesr_d = dram("onesr", [2, S], BF16)
    modb_d = dram("modb", [L, 128, 6 * D], kind="Internal")

    _sbc = {}

    def sb(name, shape, dt):
        if name not in _sbc:
            _sbc[name] = nc.alloc_sbuf_tensor("sb_" + name, list(shape), dt).ap()
        return _sbc[name]

    x = sb("x", [128, NT, D], F32)
    hT = sb("hT", [128, 8, S], BF16)
    cb = sb("cbs", [128, NCB], BF16)
    cf = sb("cfs", [128, NCF], F32)
    rowp = sb("rowps", [128, NRP - 2048], F32)
    colp = sb("colps", [128, NCP], F32)
    gate = sb("gate", [128, D], F32)
    small = sb("small", [128, 256], F32)
    arena = sb("arena", [128, 84224 // 2], BF16)
    ps = [nc.alloc_psum_tensor(f"ps{i}", [128, 512], F32).ap() for i in range(8)]
    PS = [f"ps{i}" for i in range(8)]

    ident_b = cb[:, CB_IDENT:CB_IDENT + 128]
    tri_add = cb[:, CB_TRIADD:CB_TRIADD + 128]
    tri_mul = cb[:, CB_TRIMUL:CB_TRIMUL + 128]
    bones64 = cb[:, CB_B64:CB_B64 + 128]
    bones32 = cb[:, CB_B32:CB_B32 + 128]
    ones_b = cb[:, CB_ONES:CB_ONES + 128]
    ident_f = cf[:, CF_IDENT:CF_IDENT + 128]

    def carve(off_bytes, shape, dt):
        n = int(np.prod(shape[1:]))
        esz = 4 if dt == F32 else 2
        a = arena[0:shape[0], off_bytes // 2: off_bytes // 2 + n * esz // 2]
        if dt == F32:
            a = a.bitcast(F32)
        if len(shape) == 3:
            a = a.rearrange("p (a b) -> p a b", a=shape[1])
        elif len(shape) == 4:
            a = a.rearrange("p (a b c) -> p a b c", a=shape[1], b=shape[2])
        return a

    def mm(out, lhsT, rhs, start, stop, reads, writes, inc=True):
        kw = {"skip_group_check": True}
        try:
            lhsT.base_partition()
        except BaseException:
            kw["tile_position"] = (96, 0)
        return op("pe", lambda e: e.matmul(out, lhsT=lhsT, rhs=rhs, start=start, stop=stop, **kw),
                  reads=reads, writes=writes, inc=inc)

    dma("sp", "c0", lambda e: e.dma_start(out=cb, in_=cb_d), writes=["cb"])
    dma("sp", "c1", lambda e: e.dma_start(out=cf, in_=cf_d), writes=["cf"])
    for t in range(NT):
        dma("sp", f"xin{t}", lambda e, t=t: e.dma_start(out=x[:, t, :], in_=x_d[t * 128:(t + 1) * 128, :]),
            writes=[("x", t)])
    cTs = small[:, 0:8]
    dma("sp", "c2", lambda e: e.dma_start(out=cTs, in_=cT_d), writes=["cT"])
    sc_e = small[:, 8:16]
    op("act", lambda e: e.activation(out=sc_e, in_=cTs, func=AF.Exp, scale=-1.0), reads=["cT"], writes=["sc_e"])
    op("dve", lambda e: e.tensor_scalar(out=sc_e, in0=sc_e, scalar1=1.0, scalar2=None, op0=ALU.add),
       reads=["sc_e"], writes=["sc_e"])
    op("dve", lambda e: e.reciprocal(out=sc_e, in_=sc_e), reads=["sc_e"], writes=["sc_e"])
    op("dve", lambda e: e.tensor_tensor(out=sc_e, in0=sc_e, in1=cTs, op=ALU.mult), reads=["sc_e", "cT"],
       writes=["sc_e"])
    eps_t = small[:, 16:17]
    op("dve", lambda e: e.memset(eps_t, EPS), writes=["eps"])
    lhs_rep = sb("lhs_rep", [128, 8, 128], BF16)
    gs = carve(20480, [128, D], F32)
    shf = carve(24576, [128, D], F32)
    for c in range(8):
        op("dve", lambda e, c=c: e.tensor_scalar(out=lhs_rep[:, c, :], in0=ones_b, scalar1=sc_e[:, c:c + 1],
                                                 scalar2=None, op0=ALU.mult),
           reads=["cb", "sc_e"], writes=["lhs_rep"])

    def phase_mod(l):
        aw = [carve(0, [128, 8, 512], BF16), carve(8192, [128, 8, 512], BF16)]
        ab = [carve(16384, [128, 512], F32), carve(18432, [128, 512], F32)]
        mo = [carve(20480, [128, 512], F32), carve(22528, [128, 512], F32)]
        awv = ada_w_d[l].rearrange("(c p) n -> p c n", p=128)
        for pc in range(12):
            b = pc % 2
            dma("pool", f"aw{b}", lambda e, b=b, pc=pc: e.dma_start(out=aw[b], in_=awv[:, :, pc * 512:(pc + 1) * 512]),
                writes=[("aw", b)])
            dma("sp", f"ab{b}", lambda e, b=b, pc=pc: e.dma_start(out=ab[b], in_=ada_b_d[l][:, pc * 512:(pc + 1) * 512]),
                writes=[("ab", b)])
            for c in range(8):
                mm(ps[b], lhs_rep[:, c, :], aw[b][:, c, :], c == 0, c == 7,
                   reads=["lhs_rep", ("aw", b)], writes=[PS[b]], inc=(c == 7))
            op("dve", lambda e, b=b: e.tensor_tensor(out=mo[b], in0=ps[b], in1=ab[b], op=ALU.add),
               reads=[PS[b], ("ab", b)], writes=[("mo", b)])
            dma("sp", f"mo{b}", lambda e, b=b, pc=pc: e.dma_start(out=modb_d[l][:, pc * 512:(pc + 1) * 512], in_=mo[b]),
                reads=[("mo", b)], writes=[("modb", l, pc // 2)])
        dma("sp", "rp", lambda e: e.dma_start(out=rowp, in_=rowp_d[l][:, 2048:NRP]), writes=["rowp"])
        dma("sp", "cp", lambda e: e.dma_start(out=colp, in_=colp_d[l]), writes=["colp"])

    def phase_norm(l, which):
        i_shift, i_scale = (0, 1) if which == 0 else (3, 4)
        tmpg = carve(0, [128, D], F32)
        junk = carve(4096, [128, D], BF16)
        tf = [carve(8192, [128, D], F32), carve(12288, [128, D], F32)]
        hb = [carve(16384, [128, D], BF16), carve(18432, [128, D], BF16)]
        ss = small[:, 32:48]
        rstd = small[:, 48:64]
        dma("sp", "gs", lambda e: e.dma_start(out=gs, in_=modb_d[l][:, i_scale * D:(i_scale + 1) * D]),
            reads=[("modb", l, i_scale)], writes=["gs"])
        dma("sp", "tg", lambda e: e.dma_start(out=tmpg, in_=rowp_d[l][:, which * D:(which + 1) * D]), writes=["tmpg"])
        dma("sp", "sh", lambda e: e.dma_start(out=shf, in_=modb_d[l][:, i_shift * D:(i_shift + 1) * D]),
            reads=[("modb", l, i_shift)], writes=["shf"])
        op("dve", lambda e: e.scalar_tensor_tensor(out=gs, in0=gs, scalar=1.0, in1=tmpg, op0=ALU.add, op1=ALU.mult),
           reads=["gs", "tmpg"], writes=["gs"])
        for t in range(NT):
            op("act", lambda e, t=t: e.activation(out=junk, in_=x[:, t, :], func=AF.Square, accum_out=ss[:, t:t + 1]),
               reads=[("x", t)], writes=["junk", "ss"])
        op("act", lambda e: e.activation(out=rstd, in_=ss, func=AF.Ln, bias=eps_t, scale=1.0 / D),
           reads=["ss", "eps"], writes=["rstd"])
        op("act", lambda e: e.activation(out=rstd, in_=rstd, func=AF.Exp, scale=-0.5), reads=["rstd"], writes=["rstd"])
        pst = ps[5].bitcast(BF16).rearrange("p (a b) -> p a b", a=8)
        for t in range(NT):
            b = t % 2
            op("dve", lambda e, t=t, b=b: e.scalar_tensor_tensor(out=tf[b], in0=x[:, t, :], scalar=rstd[:, t:t + 1], in1=gs,
                                                                 op0=ALU.mult, op1=ALU.mult),
               reads=[("x", t), "rstd", "gs"], writes=[("tf", b)])
            op("pool", lambda e, b=b: e.tensor_tensor(out=hb[b], in0=tf[b], in1=shf, op=ALU.add),
               reads=[("tf", b), "shf"], writes=[("hb", b)])
            for c in range(8):
                op("pe", lambda e, b=b, c=c: e.transpose(pst[:, c, :], hb[b][:, c * 128:(c + 1) * 128], ident_b),
                   reads=[("hb", b), "cb"], writes=[PS[5]])
            op("act", lambda e, t=t: e.activation(out=hT[:, :, t * 128:(t + 1) * 128], in_=pst, func=AF.Copy),
               reads=[PS[5]], writes=[("hT", t // 4)])

    A_BUFQ, A_BUFK, A_BUFIQ, A_BUFIK = 0, 8192, 16384, 24576
    A_V = 28672
    A_MIXTOK = 37120
    A_MIXT = 45312
    A_SCR = 53504
    A_W = 78080
    bufQ = carve(A_BUFQ, [128, 2, S], BF16)
    bufK = carve(A_BUFK, [128, 2, S], BF16)
    bufIQ = carve(A_BUFIQ, [128, 2, S], BF16)
    bufIK = carve(A_BUFIK, [128, S], BF16)
    Vaug = carve(A_V, [128, NT, 4, 65], BF16)
    mix_tok = carve(A_MIXTOK, [128, NT, 256], BF16)
    mixT = carve(A_MIXT, [128, 2, S], BF16)
    wst = [carve(A_W, [128, 8, 128], BF16), carve(A_W + 2048, [128, 8, 128], BF16),
           carve(A_W + 4096, [128, 8, 128], BF16)]
    wst_i = [0]
    sqb = sb("sqb", [128, 512], BF16)
    lnv = sb("lnv", [128, 512], F32)
    pT = [sb("pT0", [128, 512], BF16), sb("pT1", [128, 512], BF16)]
    recs = sb("recs", [128, 16], F32)

    def load_w(l, c0, ncols):
        i = wst_i[0] % 3
        wst_i[0] += 1
        wv = w_in_d[l].rearrange("(c p) n -> p c n", p=128)
        dst = wst[i][:, :, 0:ncols]
        dma("pool", f"wst{i}", lambda e: e.dma_start(out=dst, in_=wv[:, :, c0:c0 + ncols]), writes=[("wst", i)])
        return dst, ("wst", i)

    pj_i = [0]

    def projT(wt, wreg, M, tg):
        b = pj_i[0] % 2
        pj_i[0] += 1
        for c in range(8):
            mm(ps[b][0:M, :], wt[:, c, 0:M], hT[:, c, tg * 512:(tg + 1) * 512], c == 0, c == 7,
               reads=[wreg, ("hT", tg)], writes=[PS[b]], inc=(c == 7))
        return ps[b], PS[b]

    def qk_norm_store(pb, preg, M, bones, inv_d, gcol, lnscale, dst, dreg, out2=None):
        op("act", lambda e: e.activation(out=sqb[0:M, :], in_=pb[0:M, :], func=AF.Square), reads=[preg], writes=["sqb"])
        mm(ps[4][0:M, :], bones[0:M, 0:M], sqb[0:M, :], True, True, reads=["sqb", "cb"], writes=[PS[4]])
        op("act", lambda e: e.activation(out=lnv[0:M, :], in_=ps[4][0:M, :], func=AF.Ln, bias=eps_t[0:M, :], scale=inv_d),
           reads=[PS[4], "eps"], writes=["lnv"])
        lb = small[:, 17 + int(lnscale):18 + int(lnscale)]
        op("act", lambda e: e.activation(out=lnv[0:M, :], in_=lnv[0:M, :], func=AF.Exp, bias=lb[0:M, :], scale=-0.5),
           reads=["lnv", "lnb"], writes=["lnv"])
        op("dve", lambda e: e.scalar_tensor_tensor(out=dst, in0=pb[0:M, :], scalar=gcol[0:M, :], in1=lnv[0:M, :],
                                                   op0=ALU.mult, op1=ALU.mult),
           reads=[preg, "lnv", "colp"], writes=[dreg])
        if out2 is not None:
            gcol2, dst2, dreg2 = out2
            op("dve", lambda e: e.scalar_tensor_tensor(out=dst2, in0=pb[0:M, :], scalar=gcol2[0:M, :], in1=lnv[0:M, :],
                                                       op0=ALU.mult, op1=ALU.mult),
               reads=[preg, "lnv", "colp"], writes=[dreg2])

    def proj_v(l, c0, nheads, dstV, vreg):
        ncols = nheads * 64
        wparts = []
        for j in range(0, ncols, 128):
            wparts.append(load_w(l, c0 + j, min(128, ncols - j)))
        for t in range(NT):
            b = pj_i[0] % 2
            pj_i[0] += 1
            for j, (wt, wreg) in enumerate(wparts):
                n = wt.shape[2]
                for c in range(8):
                    mm(ps[b][:, j * 128:j * 128 + n], hT[:, c, t * 128:(t + 1) * 128], wt[:, c, :], c == 0 and j == 0, c == 7,
                       reads=[wreg, ("hT", t // 4)], writes=[PS[b]])
            src = ps[b][:, 0:ncols].rearrange("p (h d) -> p h d", h=nheads)
            op("act", lambda e, t=t, src=src: e.activation(out=dstV[:, t, 0:nheads, 0:64], in_=src, func=AF.Copy),
               reads=[PS[b]], writes=[vreg])

    po_i = [0]

    def run_pipelined(items):
        if items:
            items[0][0]()
        for i, (_, s2) in enumerate(items):
            if i + 1 < len(items):
                items[i + 1][0]()
            s2()

    def attn_core(nheads_spec, finish_fn, QG=512):
        nqb = QG // 128
        items = []
        for si, sp_ in enumerate(nheads_spec):
            for qg in range(S // QG):
                pb = 2 + (po_i[0] % 2)
                po_i[0] += 1
                po, poreg = ps[pb], PS[pb]
                first_po = [True]
                nkb = nqb * (qg + 1)
                for kb in range(nkb):
                    st = {}

                    def s1(sp_=sp_, qg=qg, kb=kb, st=st):
                        j = kb - nqb * qg
                        c0 = 0 if j < 0 else j * 128
                        sbk = pj_i[0] % 2
                        pj_i[0] += 1
                        pss, psreg = ps[sbk], PS[sbk]
                        has_extra = sp_.get("extra") is not None
                        dm = sp_.get("diag_mask", False) and j >= 0
                        mm(pss[:, c0:QG], sp_["kT"](kb), sp_["qT"](qg * QG + c0, (qg + 1) * QG), True,
                           not (has_extra or dm), reads=sp_["kq_regs"], writes=[psreg])
                        if has_extra:
                            sp_["extra"](pss, psreg, qg, kb, c0, not dm)
                        if dm:
                            mm(pss[:, c0:c0 + 128], ident_b, tri_add, False, True, reads=["cb"], writes=[psreg])
                        st.update(sbk=sbk, c0=c0)

                    def s2(sp_=sp_, si=si, qg=qg, kb=kb, st=st, po=po, poreg=poreg, first_po=first_po, last=(kb == nkb - 1)):
                        sbk, c0 = st["sbk"], st["c0"]
                        pss, psreg = ps[sbk], PS[sbk]
                        pt = pT[sbk]
                        bias_ap = sp_["bias"](kb, qg)
                        op("act", lambda e, pt=pt, pss=pss, c0=c0, bias_ap=bias_ap: e.activation(
                            out=pt[:, c0:QG], in_=pss[:, c0:QG], func=AF.Exp, bias=bias_ap),
                           reads=[psreg] + sp_["bias_regs"], writes=[("pT", sbk)])
                        for i in range(c0 // 128, nqb):
                            mm(po[:, i * 65:(i + 1) * 65], pt[:, i * 128:(i + 1) * 128], sp_["V"](kb), first_po[0], False,
                               reads=[("pT", sbk)] + sp_["v_regs"], writes=[poreg])
                            first_po[0] = False
                        if last:
                            finish_fn(si, qg, po, poreg)

                    items.append((s1, s2))
        run_pipelined(items)

    def finish_softmax(mcol0):
        def fin(si, qg, po, poreg):
            for i in range(4):
                t = qg * 4 + i
                op("dve", lambda e, i=i: e.reciprocal(out=recs[:, i:i + 1], in_=po[:, i * 65 + 64:i * 65 + 65]),
                   reads=[poreg], writes=["recs"])
                op("dve", lambda e, i=i, t=t: e.tensor_scalar(out=mix_tok[:, t, mcol0 + si * 64:mcol0 + (si + 1) * 64],
                                                              in0=po[:, i * 65:i * 65 + 64], scalar1=recs[:, i:i + 1],
                                                              scalar2=None, op0=ALU.mult),
                   reads=[poreg, "recs"], writes=[("mix_tok", t)])
        return fin

    def mixer_out(l, m):
        pst = ps[5].bitcast(BF16).rearrange("p (a b) -> p a b", a=8)
        for t in range(NT):
            for c in range(2):
                op("pe", lambda e, t=t, c=c: e.transpose(pst[:, c, :], mix_tok[:, t, c * 128:(c + 1) * 128], ident_b),
                   reads=[("mix_tok", t), "cb"], writes=[PS[5]])
            op("act", lambda e, t=t: e.activation(out=mixT[:, :, t * 128:(t + 1) * 128], in_=pst[:, 0:2, :], func=AF.Copy),
               reads=[PS[5]], writes=["mixT"])
        wo_f = carve(A_SCR, [128, 2, D], F32)
        wo_b = carve(A_SCR + 8192, [128, 2, D], BF16)
        wv = w_out_d[l][m * 256:(m + 1) * 256, :].rearrange("(c p) n -> p c n", p=128)
        dma("sp", "wo", lambda e: e.dma_start(out=wo_f, in_=wv), writes=["wo_f"])
        for c in range(2):
            bc = colp[:, CP_BETA + m * 2 + c:CP_BETA + m * 2 + c + 1]
            op("dve", lambda e, c=c, bc=bc: e.scalar_tensor_tensor(out=wo_b[:, c, :], in0=wo_f[:, c, :], scalar=bc, in1=gate,
                                                                   op0=ALU.mult, op1=ALU.mult),
               reads=["wo_f", "colp", "gate"], writes=["wo_b"])
        for t in range(NT):
            for hf in range(2):
                b = 6 + (t * 2 + hf) % 2
                for c in range(2):
                    mm(ps[b], mixT[:, c, t * 128:(t + 1) * 128], wo_b[:, c, hf * 512:(hf + 1) * 512], c == 0, c == 1,
                       reads=["mixT", "wo_b"], writes=[PS[b]])
                op("dve", lambda e, t=t, hf=hf, b=b: e.tensor_tensor(out=x[:, t, hf * 512:(hf + 1) * 512],
                                                                     in0=x[:, t, hf * 512:(hf + 1) * 512], in1=ps[b], op=ALU.add),
                   reads=[PS[b], ("x", t)], writes=[("x", t)])

    def set_v_ones():
        op("pool", lambda e: e.memset(Vaug[:, :, :, 64:65], 1.0), writes=["V"])

    def set_lnb():
        op("dve", lambda e: e.memset(small[:, 17:18], 0.0), writes=["lnb"])
        op("dve", lambda e: e.memset(small[:, 18:19], math.log(0.125)), writes=["lnb"])
        op("dve", lambda e: e.memset(small[:, 19:20], math.log(32 ** -0.5)), writes=["lnb"])

    def mixer_A(l):
        set_v_ones()
        spT = carve(A_SCR, [4, S], F32)
        cums = carve(A_SCR + 8192, [4, S], F32)
        chiT = carve(A_SCR + 16384, [4, S], BF16)
        ones4 = carve(A_SCR + 20480, [4, 512], F32)
        poscum = sb("poscum", [128, NT, 4], F32)
        nbf = small[:, 20:21]
        op("dve", lambda e: e.tensor_scalar(out=nbf, in0=colp[:, CP_BF:CP_BF + 1], scalar1=-1.0, scalar2=None, op0=ALU.mult),
           reads=["colp"], writes=["nbf"])
        op("dve", lambda e: e.memset(ones4, 1.0), writes=["ones4"])
        for (c0, dst, gc, lns, dreg) in ((0, bufQ, CP_QNA, 1.0, "bufQ"), (256, bufK, CP_KNA, 0.0, "bufK")):
            for ch in range(2):
                wt, wreg = load_w(l, c0 + ch * 128, 128)
                for tg in range(4):
                    pb, preg = projT(wt, wreg, 128, tg)
                    qk_norm_store(pb, preg, 128, bones64, 1.0 / 64, colp[:, gc:gc + 1], lns,
                                  dst[:, ch, tg * 512:(tg + 1) * 512], dreg)
        wt, wreg = load_w(l, 768, 4)
        for tg in range(4):
            pb, preg = projT(wt, wreg, 4, tg)
            op("act", lambda e, pb=pb, tg=tg: e.activation(out=spT[:, tg * 512:(tg + 1) * 512], in_=pb[0:4, :], func=AF.Exp,
                                                           bias=nbf[0:4, :], scale=-1.0),
               reads=[preg, "nbf"], writes=["spT"])
        op("act", lambda e: e.activation(out=spT, in_=spT, func=AF.Ln, bias=1.0), reads=["spT"], writes=["spT"])
        for tg in range(4):
            init = 0.0 if tg == 0 else cums[:, tg * 512 - 1:tg * 512]
            op("dve", lambda e, tg=tg, init=init: e.tensor_tensor_scan(out=cums[:, tg * 512:(tg + 1) * 512], data0=ones4,
                                                                       data1=spT[:, tg * 512:(tg + 1) * 512], initial=init,
                                                                       op0=ALU.mult, op1=ALU.add),
               reads=["spT", "ones4", "cums"], writes=["cums"])
        op("dve", lambda e: e.tensor_scalar(out=chiT, in0=cums, scalar1=-1.0, scalar2=None, op0=ALU.mult),
           reads=["cums"], writes=["chiT"])
        for t in range(NT):
            op("pe", lambda e, t=t: e.transpose(ps[4][:, t * 4:(t + 1) * 4], cums[0:4, t * 128:(t + 1) * 128], ident_f[0:4, 0:4]),
               reads=["cums", "cf"], writes=[PS[4]])
        op("dve", lambda e: e.tensor_copy(out=poscum, in_=ps[4][:, 0:64].rearrange("p (t h) -> p t h", t=NT)),
           reads=[PS[4]], writes=["poscum"])
        proj_v(l, 512, 4, Vaug, "V")
        sel4 = [sb(f"sel4_{h_}", [4, 128], BF16) for h_ in range(4)]
        for h_ in range(4):
            op("dve", lambda e, h_=h_: e.tensor_scalar(out=sel4[h_], in0=ones_b[0:4, :], scalar1=ident_f[0:4, h_:h_ + 1],
                                                       scalar2=None, op0=ALU.mult), reads=["cb", "cf"], writes=["sel4"])
        specs = []
        for h in range(4):
            hp, ch = (h % 2) * 64, h // 2

            def extra(pss, psreg, qg, kb, c0, last, h=h):
                mm(pss[:, c0:512], sel4[h],
                   chiT[0:4, qg * 512 + c0:(qg + 1) * 512], False, last, reads=["sel4", "chiT"], writes=[psreg])

            specs.append(dict(
                kT=lambda kb, hp=hp, ch=ch: bufK[hp:hp + 64, ch, kb * 128:(kb + 1) * 128],
                qT=lambda q0, q1, hp=hp, ch=ch: bufQ[hp:hp + 64, ch, q0:q1],
                kq_regs=["bufQ", "bufK"], extra=extra, diag_mask=True,
                bias=lambda kb, qg, h=h: poscum[:, kb, h:h + 1], bias_regs=["poscum"],
                V=lambda kb, h=h: Vaug[:, kb, h, :], v_regs=["V"]))
        attn_core(specs, finish_softmax(0))

    def mixer_B(l):
        QG = 256
        lbuf = carve(A_SCR, [128, NT, QG], F32)
        l1m = carve(A_SCR + 16384, [128, NT, QG], BF16)
        mixscr = sb("mixscr", [128, 512], F32)
        argb = [mixscr[:, 0:QG], mixscr[:, QG:2 * QG]]
        wTb = [sb("wTb0", [128, QG], BF16), sb("wTb1", [128, QG], BF16)]
        for (c0, dst, scl, dreg) in ((772, bufQ, 0.125, "bufQ"), (1028, bufK, 1.0, "bufK")):
            for ch in range(2):
                wt, wreg = load_w(l, c0 + ch * 128, 128)
                for tg in range(4):
                    pb, preg = projT(wt, wreg, 128, tg)
                    op("dve", lambda e, pb=pb, dst=dst, ch=ch, tg=tg, scl=scl: e.tensor_scalar(
                        out=dst[:, ch, tg * 512:(tg + 1) * 512], in0=pb, scalar1=scl, scalar2=None, op0=ALU.mult),
                       reads=[preg], writes=[dreg])
        proj_v(l, 1284, 4, Vaug, "V")
        for h in range(4):
            hp, ch = (h % 2) * 64, h // 2
            for qg in range(S // QG):
                nkb = 2 * (qg + 1)
                pb_ = 2 + (po_i[0] % 2)
                po_i[0] += 1
                po, poreg = ps[pb_], PS[pb_]
                for kb in range(nkb):
                    j = kb - 2 * qg
                    c0 = 0 if j < 0 else j * 128
                    sbk = pj_i[0] % 2
                    pj_i[0] += 1
                    pss, psreg = ps[sbk], PS[sbk]
                    mm(pss[:, c0:QG], bufK[hp:hp + 64, ch, kb * 128:(kb + 1) * 128],
                       bufQ[hp:hp + 64, ch, qg * QG + c0:(qg + 1) * QG], True, True, reads=["bufQ", "bufK"], writes=[psreg])
                    op("act", lambda e, kb=kb, c0=c0, pss=pss: e.activation(out=lbuf[:, kb, c0:QG], in_=pss[:, c0:QG],
                                                                            func=AF.Exp, scale=-1.0),
                       reads=[psreg], writes=[("lbuf", kb)])
                    op("act", lambda e, kb=kb, c0=c0: e.activation(out=lbuf[:, kb, c0:QG], in_=lbuf[:, kb, c0:QG],
                                                                   func=AF.Ln, bias=1.0),
                       reads=[("lbuf", kb)], writes=[("lbuf", kb)])
                    op("dve", lambda e, kb=kb, c0=c0, pss=pss: e.scalar_tensor_tensor(
                        out=l1m[:, kb, c0:QG], in0=pss[:, c0:QG], scalar=-1.0, in1=lbuf[:, kb, c0:QG],
                        op0=ALU.mult, op1=ALU.subtract),
                       reads=[psreg, ("lbuf", kb)], writes=[("l1m", kb)])
                    if j >= 0:
                        op("dve", lambda e, kb=kb, c0=c0: e.tensor_tensor(out=l1m[:, kb, c0:c0 + 128],
                                                                           in0=l1m[:, kb, c0:c0 + 128], in1=tri_mul, op=ALU.mult),
                           reads=[("l1m", kb), "cb"], writes=[("l1m", kb)])
                first_po = [True]
                items = []
                for kb in range(nkb):
                    st = {}

                    def s1(kb=kb, st=st, qg=qg, nkb=nkb):
                        j = kb - 2 * qg
                        c0 = 0 if j < 0 else j * 128
                        sbk = pj_i[0] % 2
                        pj_i[0] += 1
                        pss, psreg = ps[sbk], PS[sbk]
                        later = list(range(kb + 1, nkb))
                        mm(pss[:, c0:QG], triL, l1m[:, kb, c0:QG], True, len(later) == 0,
                           reads=[("l1m", kb), "triL"], writes=[psreg])
                        for n_, kb2 in enumerate(later):
                            j2 = kb2 - 2 * qg
                            c2 = 0 if j2 < 0 else j2 * 128
                            cc = max(c0, c2)
                            mm(pss[:, cc:QG], ones_b, l1m[:, kb2, cc:QG], False, n_ == len(later) - 1,
                               reads=[("l1m", kb2), "cb"], writes=[psreg])
                        st.update(sbk=sbk, c0=c0, j=j)

                    def s2(kb=kb, st=st, qg=qg, h=h, po=po, poreg=poreg, first_po=first_po, last=(kb == nkb - 1)):
                        sbk, c0, j = st["sbk"], st["c0"], st["j"]
                        pss, psreg = ps[sbk], PS[sbk]
                        ab_ = argb[sbk]
                        op("dve", lambda e, ab_=ab_, pss=pss, kb=kb, c0=c0: e.tensor_tensor(
                            out=ab_[:, c0:QG], in0=pss[:, c0:QG], in1=lbuf[:, kb, c0:QG], op=ALU.subtract),
                           reads=[psreg, ("lbuf", kb)], writes=[("argb", sbk)])
                        wt_ = wTb[sbk]
                        op("act", lambda e, ab_=ab_, wt_=wt_, c0=c0: e.activation(out=wt_[:, c0:QG], in_=ab_[:, c0:QG], func=AF.Exp),
                           reads=[("argb", sbk)], writes=[("wTb", sbk)])
                        if j >= 0:
                            op("dve", lambda e, wt_=wt_, c0=c0: e.tensor_tensor(out=wt_[:, c0:c0 + 128], in0=wt_[:, c0:c0 + 128],
                                                                                 in1=tri_mul, op=ALU.mult),
                               reads=[("wTb", sbk), "cb"], writes=[("wTb", sbk)])
                        for i in range(c0 // 128, 2):
                            mm(po[:, i * 65:i * 65 + 64], wt_[:, i * 128:(i + 1) * 128], Vaug[:, kb, h, 0:64], first_po[0], False,
                               reads=[("wTb", sbk), "V"], writes=[poreg])
                            first_po[0] = False
                        if not last:
                            return
                        for i in range(2):
                            t = qg * 2 + i
                            op("act", lambda e, i=i, t=t, po=po, h=h: e.activation(out=mix_tok[:, t, h * 64:(h + 1) * 64],
                                                                                   in_=po[:, i * 65:i * 65 + 64], func=AF.Copy),
                               reads=[poreg], writes=[("mix_tok", t)])

                    items.append((s1, s2))
                run_pipelined(items)

    triL = sb("triL", [128, 128], BF16)
    op("pe", lambda e: e.transpose(ps[5].bitcast(BF16)[:, 0:128], tri_mul, ident_b), reads=["cb"], writes=[PS[5]])
    op("dve", lambda e: e.tensor_copy(out=triL, in_=ps[5].bitcast(BF16)[:, 0:128]), reads=[PS[5]], writes=["triL"])

    def mixer_C(l):
        lam_init = 0.8 - 0.6 * math.exp(-0.3 * l)
        set_v_ones()
        lamrow = rowp[:, RP_LAM - 2048:RP_LAM - 2048 + 128]
        lt = small[:, 64:128]
        ls = small[:, 24:28]
        op("dve", lambda e: e.tensor_tensor(out=lt[:, 0:32], in0=lamrow[:, 0:32], in1=lamrow[:, 32:64], op=ALU.mult),
           reads=["rowp"], writes=["lt"])
        op("dve", lambda e: e.tensor_tensor(out=lt[:, 32:64], in0=lamrow[:, 64:96], in1=lamrow[:, 96:128], op=ALU.mult),
           reads=["rowp"], writes=["lt"])
        op("dve", lambda e: e.reduce_sum(out=ls[:, 0:1], in_=lt[:, 0:32], axis=mybir.AxisListType.X), reads=["lt"], writes=["ls"])
        op("dve", lambda e: e.reduce_sum(out=ls[:, 1:2], in_=lt[:, 32:64], axis=mybir.AxisListType.X), reads=["lt"], writes=["ls"])
        op("act", lambda e: e.activation(out=ls[:, 0:2], in_=ls[:, 0:2], func=AF.Exp), reads=["ls"], writes=["ls"])
        op("dve", lambda e: e.tensor_tensor(out=ls[:, 2:3], in0=ls[:, 1:2], in1=ls[:, 0:1], op=ALU.subtract),
           reads=["ls"], writes=["ls"])
        op("dve", lambda e: e.tensor_scalar(out=ls[:, 2:3], in0=ls[:, 2:3], scalar1=-lam_init, scalar2=None, op0=ALU.add),
           reads=["ls"], writes=["ls"])
        nlam = ls[:, 2:3]
        if stopC <= 1:
            return
        bufK1 = carve(A_SCR + 4096, [128, 2, S], BF16)
        for (c0, dst, gc, lns, dreg) in ((1540, bufQ, CP_QNC, 2.0, "bufQ"), (1796, bufK, CP_KNC, 0.0, "bufK")):
            for ch in range(2):
                wt, wreg = load_w(l, c0 + ch * 128, 128)
                for tg in range(4):
                    pb, preg = projT(wt, wreg, 128, tg)
                    if dst is bufQ:
                        o2_ = (colp[:, CP_QNC1:CP_QNC1 + 1], bufIQ[:, ch, tg * 512:(tg + 1) * 512], "bufIQ")
                    else:
                        o2_ = (colp[:, gc:gc + 1], bufK1[:, ch, tg * 512:(tg + 1) * 512], "bufK1")
                    qk_norm_store(pb, preg, 128, bones32, 1.0 / 32, colp[:, gc:gc + 1], lns,
                                  dst[:, ch, tg * 512:(tg + 1) * 512], dreg, out2=o2_)
        if stopC <= 2:
            return
        for h in range(4):
            hp, ch = (h % 2) * 64, h // 2
            for c, (qc, qreg, kc, kreg) in enumerate(((bufQ, "bufQ", bufK, "bufK"), (bufIQ, "bufIQ", bufK1, "bufK1"))):
                r0 = hp + 32 * (1 - c)
                dma("sp", f"alq{c}", lambda e, qc=qc, r0=r0, ch=ch, h=h: e.dma_start(out=qc[r0:r0 + 2, ch, :], in_=alc_d[h]),
                    writes=[qreg])
                dma("sp", f"alk{c}", lambda e, kc=kc, r0=r0, ch=ch: e.dma_start(out=kc[r0:r0 + 2, ch, :], in_=onesr_d),
                    writes=[kreg])
        proj_v(l, 2052, 4, Vaug, "V")
        if stopC <= 3:
            return
        o1 = sb("o1c", [128, 64], F32)
        o2 = sb("o2c", [128, 64], F32)
        keep = {}
        specs = []
        for h in range(4):
            for c in range(2):
                hp, ch = (h % 2) * 64, h // 2
                qsrc = bufQ if c == 0 else bufIQ
                ksrc = bufK if c == 0 else bufK1
                specs.append(dict(
                    kT=lambda kb, hp=hp, ch=ch, ksrc=ksrc: ksrc[hp:hp + 64, ch, kb * 128:(kb + 1) * 128],
                    qT=lambda q0, q1, hp=hp, ch=ch, qsrc=qsrc: qsrc[hp:hp + 64, ch, q0:q1],
                    kq_regs=["bufQ", "bufIQ", "bufK", "bufK1"], extra=None, diag_mask=("nodiag" not in cdbg),
                    bias=lambda kb, qg, h=h: cf[:, CF_ALIBI + h * AL_N + (kb - 4 * qg) + AL_O:CF_ALIBI + h * AL_N + (kb - 4 * qg) + AL_O + 1],
                    bias_regs=["cf"],
                    V=lambda kb, h=h: Vaug[:, kb, h, :], v_regs=["V"]))

        stash = carve(A_SCR, [128, NT, 64], F32)

        def fin(si, qg, po, poreg):
            h, c = si // 2, si % 2
            if "nofin" in cdbg:
                return
            for i in range(4):
                t = qg * 4 + i
                op("dve", lambda e, i=i: e.reciprocal(out=recs[:, i:i + 1], in_=po[:, i * 65 + 64:i * 65 + 65]),
                   reads=[poreg], writes=["recs"])
                if c == 0:
                    op("dve", lambda e, i=i, t=t: e.tensor_scalar(out=stash[:, t, :], in0=po[:, i * 65:i * 65 + 64],
                                                                  scalar1=recs[:, i:i + 1], scalar2=None, op0=ALU.mult),
                       reads=[poreg, "recs"], writes=[("stash", t)])
                else:
                    op("dve", lambda e, i=i: e.tensor_scalar(out=o1, in0=po[:, i * 65:i * 65 + 64], scalar1=recs[:, i:i + 1],
                                                             scalar2=nlam, op0=ALU.mult, op1=ALU.mult),
                       reads=[poreg, "recs", "ls"], writes=["o1"])
                    op("dve", lambda e, t=t: e.tensor_tensor(out=o1, in0=o1, in1=stash[:, t, :], op=ALU.add),
                       reads=["o1", ("stash", t)], writes=["o1"])
                    op("act", lambda e: e.activation(out=o2, in_=o1, func=AF.Square, accum_out=small[:, 28:29]),
                       reads=["o1"], writes=["o2", "cs"])
                    op("act", lambda e: e.activation(out=small[:, 28:29], in_=small[:, 28:29], func=AF.Ln, bias=eps_t, scale=1.0 / 64),
                       reads=["cs", "eps"], writes=["cs"])
                    op("act", lambda e: e.activation(out=small[:, 28:29], in_=small[:, 28:29], func=AF.Exp,
                                                     bias=small[:, 21:22], scale=-0.5),
                       reads=["cs", "lnb2"], writes=["cs"])
                    op("dve", lambda e, t=t, h=h: e.scalar_tensor_tensor(out=mix_tok[:, t, h * 64:(h + 1) * 64], in0=o1,
                                                                         scalar=small[:, 28:29],
                                                                         in1=rowp[:, RP_SUBLN - 2048:RP_SUBLN - 2048 + 64],
                                                                         op0=ALU.mult, op1=ALU.mult),
                       reads=["o1", "cs", "rowp"], writes=[("mix_tok", t)])

        op("dve", lambda e: e.memset(small[:, 21:22], math.log(1.0 - lam_init)), writes=["lnb2"])
        attn_core(specs, fin)

    def mixer_D(l):
        set_v_ones()
        rtmp = sb("mixscr", [128, 512], F32)
        mx8 = sb("mx8", [128, 8], F32)
        thr = small[:, 128:144]
        iw = sb("iw", [128, NT, 8], F32)
        iwa = sb("iwa", [128, NT, 8], F32)
        iws = sb("iws", [128, NT, 8], F32)
        iqm = [sb(f"iqm{k_}", [128, 128], BF16) for k_ in range(8)]
        for ch in range(2):
            wt, wreg = load_w(l, 2308 + ch * 128, 128)
            for tg in range(4):
                pb, preg = projT(wt, wreg, 128, tg)
                qk_norm_store(pb, preg, 128, bones64, 1.0 / 64, colp[:, CP_QND:CP_QND + 1], 1.0,
                              bufQ[:, ch, tg * 512:(tg + 1) * 512], "bufQ")
        wt, wreg = load_w(l, 2564, 64)
        ik2 = wst_i[0] % 3
        wst_i[0] += 1
        wk2 = wst[ik2]
        for r in range(2):
            op("dve", lambda e, r=r, wt=wt: e.tensor_copy(out=wk2[:, :, r * 64:(r + 1) * 64], in_=wt), reads=[wreg], writes=[("wst", ik2)])
        for tg in range(4):
            pb, preg = projT(wk2, ("wst", ik2), 128, tg)
            qk_norm_store(pb, preg, 128, bones64, 1.0 / 64, colp[:, CP_KND:CP_KND + 1], 0.0,
                          bufK[:, 0, tg * 512:(tg + 1) * 512], "bufK")
        if stopD <= 1:
            return
        for ch in range(2):
            wt, wreg = load_w(l, 2692 + ch * 128, 128)
            for tg in range(4):
                pb, preg = projT(wt, wreg, 128, tg)
                op("dve", lambda e, pb=pb, ch=ch, tg=tg: e.tensor_scalar(out=bufIQ[:, ch, tg * 512:(tg + 1) * 512], in0=pb,
                                                                         scalar1=32 ** -0.5, scalar2=None, op0=ALU.mult),
                   reads=[preg], writes=["bufIQ"])
        wt, wreg = load_w(l, 2948, 32)
        i4 = wst_i[0] % 3
        wst_i[0] += 1
        w4 = wst[i4]
        for r in range(4):
            op("dve", lambda e, r=r, wt=wt: e.tensor_copy(out=w4[:, :, r * 32:(r + 1) * 32], in_=wt), reads=[wreg], writes=[("wst", i4)])
        for tg in range(4):
            pb, preg = projT(w4, ("wst", i4), 128, tg)
            op("act", lambda e, pb=pb, tg=tg: e.activation(out=bufIK[:, tg * 512:(tg + 1) * 512], in_=pb, func=AF.Copy),
               reads=[preg], writes=["bufIK"])
        wt, wreg = load_w(l, 2980, 8)
        for t in range(NT):
            b = pj_i[0] % 2
            pj_i[0] += 1
            for c in range(8):
                mm(ps[b][:, 0:8], hT[:, c, t * 128:(t + 1) * 128], wt[:, c, :], c == 0, c == 7,
                   reads=[wreg, ("hT", t // 4)], writes=[PS[b]])
            op("dve", lambda e, t=t, b=b: e.tensor_scalar(out=iw[:, t, :], in0=ps[b][:, 0:8], scalar1=8 ** -0.5, scalar2=None, op0=ALU.mult),
               reads=[PS[b]], writes=["iw"])
        op("act", lambda e: e.activation(out=iwa, in_=iw, func=AF.Abs), reads=["iw"], writes=["iwa"])
        op("act", lambda e: e.activation(out=iws, in_=iw, func=AF.Sign), reads=["iw"], writes=["iws"])
        proj_v(l, 2628, 1, Vaug, "V")

        if stopD <= 2:
            return
        QG = 256
        scoreb = [carve(A_MIXT, [128, S], F32), carve(A_MIXT + 8192, [128, S], F32)]
        mbvb = [carve(A_MIXT + 16384, [128, 2, S], BF16), carve(A_MIXT + 24576, [128, 2, S], BF16)]
        rt = [rtmp, carve(A_BUFK + 4096, [128, 512], F32)]
        posb = carve(A_W, [128, 2, 132], F32)
        den = small[:, 144:148]
        rt_i = [0]

        def index_topk(qg):
            for i in range(2):
                t = qg * 2 + i
                nk = (t + 1) * 128
                score, sreg = scoreb[t % 2], ("score", t % 2)
                mbv = mbvb[qg % 2]
                for hh in range(8):
                    g, ch = hh % 4, hh // 4
                    op("pool", lambda e, hh=hh, g=g, ch=ch, t=t: e.tensor_scalar(
                        out=iqm[hh], in0=bufIQ[:, ch, t * 128:(t + 1) * 128], scalar1=cf[:, CF_GMASK + g:CF_GMASK + g + 1],
                        scalar2=None, op0=ALU.mult), reads=["bufIQ", "cf"], writes=[("iqm", hh)])
                for k0 in range(0, nk, 512):
                    kn = min(512, nk - k0)
                    for hh in range(8):
                        b = pj_i[0] % 2
                        pj_i[0] += 1
                        r = rt_i[0] % 2
                        rt_i[0] += 1
                        mm(ps[b][:, 0:kn], iqm[hh], bufIK[:, k0:k0 + kn], True, True,
                           reads=[("iqm", hh), "bufIK"], writes=[PS[b]])
                        op("act", lambda e, b=b, kn=kn, t=t, hh=hh, r=r: e.activation(out=rt[r][:, 0:kn], in_=ps[b][:, 0:kn], func=AF.Relu,
                                                                                      scale=iwa[:, t, hh:hh + 1]),
                           reads=[PS[b], "iwa"], writes=[("rtmp", r)])
                        if hh == 0:
                            op("act", lambda e, k0=k0, kn=kn, t=t, r=r, score=score: e.activation(
                                out=score[:, k0:k0 + kn], in_=rt[r][:, 0:kn], func=AF.Copy, scale=iws[:, t, 0:1]),
                               reads=[("rtmp", r), "iws"], writes=[sreg])
                        else:
                            op("act", lambda e, kn=kn, t=t, hh=hh, r=r: e.activation(
                                out=rt[r][:, 0:kn], in_=rt[r][:, 0:kn], func=AF.Copy, scale=iws[:, t, hh:hh + 1]),
                               reads=[("rtmp", r), "iws"], writes=[("rtmp", r)])
                            op("pool", lambda e, k0=k0, kn=kn, r=r, score=score: e.tensor_tensor(
                                out=score[:, k0:k0 + kn], in0=score[:, k0:k0 + kn], in1=rt[r][:, 0:kn], op=ALU.add),
                               reads=[("rtmp", r), sreg], writes=[sreg])
                op("pool", lambda e, t=t, score=score: e.tensor_tensor(out=score[:, t * 128:(t + 1) * 128], in0=score[:, t * 128:(t + 1) * 128],
                                                                       in1=cf[:, CF_NEGTRI:CF_NEGTRI + 128], op=ALU.add),
                   reads=[sreg, "cf"], writes=[sreg])
                if t < 2:
                    op("dve", lambda e, i=i, nk=nk, score=score, mbv=mbv: e.tensor_scalar(
                        out=mbv[:, i, 0:nk], in0=score[:, 0:nk], scalar1=-1e29, scalar2=NEG, op0=ALU.is_lt, op1=ALU.mult),
                       reads=[sreg], writes=[("mbv", qg % 2, i)])
                else:
                    for r_ in range(32):
                        op("dve", lambda e, score=score, nk=nk: e.max(out=mx8, in_=score[:, 0:nk]), reads=[sreg], writes=["mx8"])
                        op("dve", lambda e, score=score, nk=nk: e.match_replace(out=score[:, 0:nk], in_to_replace=mx8,
                                                                                in_values=score[:, 0:nk], imm_value=-3e38),
                           reads=[sreg, "mx8"], writes=[sreg])
                    op("dve", lambda e, i=i, nk=nk, score=score, mbv=mbv: e.tensor_scalar(
                        out=mbv[:, i, 0:nk], in0=score[:, 0:nk], scalar1=-1e35, scalar2=NEG, op0=ALU.is_ge, op1=ALU.mult),
                       reads=[sreg], writes=[("mbv", qg % 2, i)])

        def attention(qg):
            nkb = 2 * (qg + 1)
            mbv = mbvb[qg % 2]
            items = []
            for h in range(4):
                hp, ch = (h % 2) * 64, h // 2
                pb_ = 2 + (po_i[0] % 2)
                po_i[0] += 1
                po, poreg = ps[pb_], PS[pb_]
                first_po = [True]
                for kb in range(nkb):
                    st = {}

                    def s1(h=h, hp=hp, ch=ch, kb=kb, st=st):
                        j = kb - 2 * qg
                        c0 = 0 if j < 0 else j * 128
                        sbk = pj_i[0] % 2
                        pj_i[0] += 1
                        pss, psreg = ps[sbk], PS[sbk]
                        mm(pss[:, c0:QG], bufK[hp:hp + 64, 0, kb * 128:(kb + 1) * 128], bufQ[hp:hp + 64, ch, qg * QG + c0:(qg + 1) * QG],
                           True, False, reads=["bufQ", "bufK"], writes=[psreg])
                        mm(pss[:, c0:QG], cb[0:2, CB_NSLOPE + (4 + h) * 128:CB_NSLOPE + (5 + h) * 128],
                           cb[0:2, CB_POS + c0:CB_POS + QG], False, False, reads=["cb"], writes=[psreg])
                        for i in range(c0 // 128, 2):
                            mm(pss[:, i * 128:(i + 1) * 128], mbv[:, i, kb * 128:(kb + 1) * 128], ident_b, False, i == 1,
                               reads=[("mbv", qg % 2, i), "cb"], writes=[psreg])
                        st.update(sbk=sbk, c0=c0)

                    def s2(h=h, kb=kb, st=st, po=po, poreg=poreg, first_po=first_po, pi=pb_ - 2, last=(kb == nkb - 1)):
                        sbk, c0 = st["sbk"], st["c0"]
                        pss, psreg = ps[sbk], PS[sbk]
                        pt = pT[sbk]
                        dlt = kb - 2 * qg
                        bias_ap = cf[:, CF_ALIBI + (4 + h) * AL_N + dlt + AL_O:CF_ALIBI + (4 + h) * AL_N + dlt + AL_O + 1]
                        op("act", lambda e, pt=pt, pss=pss, c0=c0, bias_ap=bias_ap: e.activation(
                            out=pt[:, c0:QG], in_=pss[:, c0:QG], func=AF.Exp, bias=bias_ap),
                           reads=[psreg, "cf"], writes=[("pT", sbk)])
                        for i in range(c0 // 128, 2):
                            mm(po[:, i * 65:(i + 1) * 65], pt[:, i * 128:(i + 1) * 128], Vaug[:, kb, 0, :], first_po[0], False,
                               reads=[("pT", sbk), "V"], writes=[poreg])
                            first_po[0] = False
                        if not last:
                            return
                        dn = den[:, pi * 2:pi * 2 + 2]
                        dsrc = po[:, 0:130].rearrange("p (i c) -> p i c", c=65)[:, :, 64]
                        op("act", lambda e, dn=dn, dsrc=dsrc: e.activation(out=dn, in_=dsrc, func=AF.Ln, bias=1e-30),
                           reads=[poreg], writes=[("den", pi)])
                        op("act", lambda e, dn=dn: e.activation(out=dn, in_=dn, func=AF.Exp, scale=-1.0),
                           reads=[("den", pi)], writes=[("den", pi)])
                        for i in range(2):
                            t = qg * 2 + i
                            op("act", lambda e, i=i, t=t, h=h, po=po, dn=dn: e.activation(
                                out=mix_tok[:, t, h * 64:(h + 1) * 64], in_=po[:, i * 65:i * 65 + 64], func=AF.Copy, scale=dn[:, i:i + 1]),
                               reads=[poreg, ("den", pi)], writes=[("mix_tok", t)])

                    items.append((s1, s2))
            run_pipelined(items)

        nqg = S // QG
        for qg in range(nqg):
            index_topk(qg)
            if stopD > 3 and qg >= 1:
                attention(qg - 1)
        if stopD > 3:
            attention(nqg - 1)

    def phase_moe(l):
        NE = 2
        M_W = 0
        wb = [[dict(w1=carve(M_W + (s_ * NE + j) * 12288, [128, 8, 256], BF16),
                    w3=carve(M_W + (s_ * NE + j) * 12288 + 4096, [128, 8, 256], BF16),
                    w2f=carve(M_W + (s_ * NE + j) * 12288 + 8192, [128, 2, D], BF16)) for j in range(NE)] for s_ in range(2)]
        M_O = 2 * NE * 12288
        w2stage = [carve(M_O, [128, 2, D], F32)]
        hid = carve(M_O + 8192, [128, NE * 2, 512], BF16)
        t1 = [carve(M_O + 12288, [128, 512], F32), carve(M_O + 14336, [128, 512], F32)]
        t2 = [carve(M_O + 16384, [128, 512], F32), carve(M_O + 18432, [128, 512], F32)]
        chi_ = carve(M_O + 20480, [32, S], BF16)
        clo_ = carve(M_O + 24576, [32, S], BF16)
        comb = carve(M_O + 28672, [128, NT, 32], F32)
        lg = carve(M_O + 30720, [128, 64], F32)[:, 0:36]
        wr = carve(M_O + 30976, [128, 8, 36], BF16)
        rs = small[:, 160:200]
        ohb = [sb(f"ohb_{i_}", [32, 128], BF16) for i_ in range(2)]
        dma("sp", "gate", lambda e: e.dma_start(out=gate, in_=modb_d[l][:, 5 * D:6 * D]), reads=[("modb", l, 5)], writes=["gate"])
        dma("pool", "wr", lambda e: e.dma_start(out=wr, in_=w_r_d[l].rearrange("(c p) n -> p c n", p=128)), writes=["wr"])
        brow = rowp[:, RP_BR - 2048:RP_BR - 2048 + 36]
        for t in range(NT):
            b = pj_i[0] % 2
            pj_i[0] += 1
            for c in range(8):
                mm(ps[b][:, 0:36], hT[:, c, t * 128:(t + 1) * 128], wr[:, c, :], c == 0, c == 7,
                   reads=["wr", ("hT", t // 4)], writes=[PS[b]])
            op("dve", lambda e, b=b: e.tensor_tensor(out=lg, in0=ps[b][:, 0:36], in1=brow, op=ALU.add),
               reads=[PS[b], "rowp"], writes=["lg"])
            R_ = ["lg", "rs"]
            gmax, gsum, v1, v2, w1c, w2c = (rs[:, k:k + 1] for k in range(6))
            goh = rs[:, 8:12]
            gex = rs[:, 12:16]
            op("dve", lambda e: e.reduce_max(out=gmax, in_=lg[:, 0:4], axis=mybir.AxisListType.X), reads=R_, writes=["rs"])
            op("dve", lambda e: e.tensor_scalar(out=goh, in0=lg[:, 0:4], scalar1=gmax, scalar2=None, op0=ALU.is_ge), reads=R_, writes=["rs"])
            op("dve", lambda e: e.tensor_scalar(out=gex, in0=lg[:, 0:4], scalar1=gmax, scalar2=None, op0=ALU.subtract), reads=R_, writes=["rs"])
            op("act", lambda e: e.activation(out=gex, in_=gex, func=AF.Exp, accum_out=gsum), reads=R_, writes=["rs"])
            op("dve", lambda e: e.reciprocal(out=gsum, in_=gsum), reads=R_, writes=["rs"])
            op("dve", lambda e: e.tensor_scalar(out=gex, in0=goh, scalar1=-1.0, scalar2=1e9, op0=ALU.add, op1=ALU.mult), reads=R_, writes=["rs"])
            for g in range(4):
                op("dve", lambda e, g=g: e.tensor_scalar(out=lg[:, 4 + g * 8:12 + g * 8], in0=lg[:, 4 + g * 8:12 + g * 8],
                                                         scalar1=gex[:, g:g + 1], scalar2=None, op0=ALU.add), reads=R_, writes=["lg"])
            el = lg[:, 4:36]
            eq1 = lnv[:, 0:32]
            eq2 = lnv[:, 32:64]
            el2 = lnv[:, 64:96]
            R2 = ["lg", "rs", "lnv"]
            op("dve", lambda e: e.reduce_max(out=v1, in_=el, axis=mybir.AxisListType.X), reads=R2, writes=["rs"])
            op("dve", lambda e: e.tensor_scalar(out=eq1, in0=el, scalar1=v1, scalar2=None, op0=ALU.is_ge), reads=R2, writes=["lnv"])
            op("dve", lambda e: e.scalar_tensor_tensor(out=el2, in0=eq1, scalar=-1e9, in1=el, op0=ALU.mult, op1=ALU.add), reads=R2, writes=["lnv"])
            op("dve", lambda e: e.reduce_max(out=v2, in_=el2, axis=mybir.AxisListType.X), reads=R2, writes=["rs"])
            op("dve", lambda e: e.tensor_scalar(out=eq2, in0=el2, scalar1=v2, scalar2=None, op0=ALU.is_ge), reads=R2, writes=["lnv"])
            op("dve", lambda e: e.tensor_tensor(out=w2c, in0=v1, in1=v2, op=ALU.subtract), reads=R2, writes=["rs"])
            op("act", lambda e: e.activation(out=w2c, in_=w2c, func=AF.Exp), reads=R2, writes=["rs"])
            op("dve", lambda e: e.tensor_scalar(out=w2c, in0=w2c, scalar1=1.0, scalar2=None, op0=ALU.add), reads=R2, writes=["rs"])
            op("dve", lambda e: e.reciprocal(out=w2c, in_=w2c), reads=R2, writes=["rs"])
            op("dve", lambda e: e.tensor_scalar(out=w1c, in0=w2c, scalar1=-1.0, scalar2=1.0, op0=ALU.mult, op1=ALU.add), reads=R2, writes=["rs"])
            op("dve", lambda e: e.tensor_tensor(out=w1c, in0=w1c, in1=gsum, op=ALU.mult), reads=R2, writes=["rs"])
            op("dve", lambda e: e.tensor_tensor(out=w2c, in0=w2c, in1=gsum, op=ALU.mult), reads=R2, writes=["rs"])
            op("dve", lambda e: e.tensor_scalar(out=eq1, in0=eq1, scalar1=w1c, scalar2=None, op0=ALU.mult), reads=R2, writes=["lnv"])
            op("dve", lambda e, t=t: e.scalar_tensor_tensor(out=comb[:, t, :], in0=eq2, scalar=w2c, in1=eq1, op0=ALU.mult, op1=ALU.add),
               reads=R2, writes=["comb"])
        chl = carve(M_O + 12288, [128, NT, 64], BF16)
        ctmp = carve(M_O + 16384, [128, NT, 32], F32)
        op("dve", lambda e: e.tensor_copy(out=chl[:, :, 0:32], in_=comb), reads=["comb"], writes=["chl"])
        op("dve", lambda e: e.tensor_tensor(out=ctmp, in0=comb, in1=chl[:, :, 0:32], op=ALU.subtract), reads=["comb", "chl"], writes=["ctmp"])
        op("dve", lambda e: e.tensor_copy(out=chl[:, :, 32:64], in_=ctmp), reads=["ctmp"], writes=["chl"])
        pstb = ps[5].bitcast(BF16)
        for t in range(NT):
            op("pe", lambda e, t=t: e.transpose(pstb[0:64, (t % 8) * 128:(t % 8 + 1) * 128], chl[:, t, :], ident_b), reads=["chl", "cb"], writes=[PS[5]])
            if t % 8 == 7:
                t0_ = (t // 8) * 1024
                op("dve", lambda e, t0_=t0_: e.tensor_copy(out=chi_[:, t0_:t0_ + 1024], in_=pstb[0:32, :]), reads=[PS[5]], writes=["chi_"])
                op("dve", lambda e, t0_=t0_: e.tensor_copy(out=clo_[:, t0_:t0_ + 1024], in_=pstb[32:64, :]), reads=[PS[5]], writes=["clo_"])
        em.barrier()

        nsets = 32 // NE

        def load_set(s_):
            sl = s_ % 2
            for j in range(NE):
                e_ = s_ * NE + j
                w = wb[sl][j]
                dma("pool", f"mw{sl}{j}a", lambda e, w=w, e_=e_: e.dma_start(out=w["w1"], in_=w1_d[l][e_].rearrange("(c p) n -> p c n", p=128)),
                    writes=[("w13", sl, j)])
                dma("pool", f"mw{sl}{j}b", lambda e, w=w, e_=e_: e.dma_start(out=w["w3"], in_=w3_d[l][e_].rearrange("(c p) n -> p c n", p=128)),
                    writes=[("w13", sl, j)])
                st = w2stage[0]
                dma("sp", "mw2s", lambda e, st=st, e_=e_: e.dma_start(out=st, in_=w2_d[l][e_].rearrange("(c p) n -> p c n", p=128)),
                    writes=[("w2st", 0)])
                for c in range(2):
                    op("pool", lambda e, w=w, st=st, c=c: e.tensor_tensor(out=w["w2f"][:, c, :], in0=st[:, c, :], in1=gate, op=ALU.mult),
                       reads=[("w2st", 0), "gate"], writes=[("w2", sl, j)])

        hidb = [hid, carve(M_O + 28672, [128, NE * 2, 512], BF16)]
        steps = [(s_, tg) for s_ in range(nsets) for tg in range(4)]

        def stepA(i):
            s_, tg = steps[i]
            sl = s_ % 2
            hb = hidb[i % 2]
            for j in range(NE):
                e_ = s_ * NE + j
                w = wb[sl][j]
                n_ = i * NE + j
                cbk = 4 + n_ % 2
                oh = ohb[n_ % 2]
                op("dve", lambda e, oh=oh, e_=e_: e.tensor_scalar(out=oh, in0=ones_b[0:32, :], scalar1=ident_f[0:32, e_:e_ + 1],
                                                                  scalar2=None, op0=ALU.mult),
                   reads=["cb", "cf"], writes=[("ohb", n_ % 2)])
                mm(ps[cbk], oh, chi_[:, tg * 512:(tg + 1) * 512], True, False, reads=[("ohb", n_ % 2), "chi_"], writes=[PS[cbk]])
                mm(ps[cbk], oh, clo_[:, tg * 512:(tg + 1) * 512], False, True, reads=[("ohb", n_ % 2), "clo_"], writes=[PS[cbk]])
                for fc in range(2):
                    k = pj_i[0] % 2
                    pj_i[0] += 1
                    p1, p3 = ps[k], ps[2 + k]
                    for c in range(8):
                        mm(p1, w["w1"][:, c, fc * 128:(fc + 1) * 128], hT[:, c, tg * 512:(tg + 1) * 512], c == 0, c == 7,
                           reads=[("w13", sl, j), ("hT", tg)], writes=[PS[k]], inc=(c == 7))
                    for c in range(8):
                        mm(p3, w["w3"][:, c, fc * 128:(fc + 1) * 128], hT[:, c, tg * 512:(tg + 1) * 512], c == 0, c == 7,
                           reads=[("w13", sl, j), ("hT", tg)], writes=[PS[2 + k]], inc=(c == 7))
                    op("act", lambda e, k=k, p1=p1: e.activation(out=t1[k], in_=p1, func=AF.Silu), reads=[PS[k]], writes=[("t1", k)])
                    op("dve", lambda e, k=k, p3=p3: e.tensor_tensor(out=t2[k], in0=t1[k], in1=p3, op=ALU.mult),
                       reads=[("t1", k), PS[2 + k]], writes=[("t2", k)])
                    op("dve", lambda e, k=k, j=j, fc=fc, hb=hb, cbk=cbk: e.tensor_tensor(out=hb[:, j * 2 + fc, :], in0=t2[k], in1=ps[cbk], op=ALU.mult),
                       reads=[("t2", k), PS[cbk]], writes=[("hid", i % 2, j * 2 + fc)])

        def stepB(i):
            s_, tg = steps[i]
            sl = s_ % 2
            hb = hidb[i % 2]
            for ti in range(4):
                t = tg * 4 + ti
                for hf in range(2):
                    b = 6 + (ti * 2 + hf) % 2
                    n = 0
                    for j in range(NE):
                        for fc in range(2):
                            mm(ps[b], hb[:, j * 2 + fc, ti * 128:(ti + 1) * 128], wb[sl][j]["w2f"][:, fc, hf * 512:(hf + 1) * 512],
                               n == 0, n == NE * 2 - 1, reads=[("hid", i % 2, j * 2 + fc), ("w2", sl, j)], writes=[PS[b]],
                               inc=(n == NE * 2 - 1))
                            n += 1
                    op("dve", lambda e, t=t, hf=hf, b=b: e.tensor_tensor(out=x[:, t, hf * 512:(hf + 1) * 512],
                                                                         in0=x[:, t, hf * 512:(hf + 1) * 512], in1=ps[b], op=ALU.add),
                       reads=[PS[b], ("x", t)], writes=[("x", t)])

        load_set(0)
        load_set(1)
        stepA(0)
        for i, (s_, tg) in enumerate(steps):
            if tg == 0 and s_ >= 1 and s_ + 1 < nsets and "moe_nodma" not in cdbg:
                load_set(s_ + 1)
            if i + 1 < len(steps):
                stepA(i + 1)
            stepB(i)

    set_lnb()

    for l in range(nl):
        phase_mod(l)
        em.barrier()
        phase_norm(l, 0)
        dma("sp", "gate", lambda e, l=l: e.dma_start(out=gate, in_=modb_d[l][:, 2 * D:3 * D]), reads=[("modb", l, 2)], writes=["gate"])
        em.barrier()
        for m, name in enumerate("ABCD"):
            if name in mixers:
                {"A": mixer_A, "B": mixer_B, "C": mixer_C, "D": mixer_D}[name](l)
                em.barrier()
                mixer_out(l, m)
                em.barrier()
        phase_norm(l, 1)
        em.barrier()
        if do_moe:
            phase_moe(l)
            em.barrier()
    for t in range(NT):
        dma("sp", f"xo{t % 4}", lambda e, t=t: e.dma_start(out=out_d[t * 128:(t + 1) * 128, :], in_=x[:, t, :]),
            reads=[("x", t)], writes=[("out", t)])
    em.wait_all("sp", [("out", t) for t in range(NT)])
    em.finish()
    return nc


def prep_inputs(inp, n_cores=8):
    f = lambda a: np.ascontiguousarray(np.asarray(a, dtype=np.float32))
    cbv, cfv = make_consts()
    rowp = np.zeros((L, 128, NRP), np.float32)
    colp = np.zeros((L, 128, NCP), np.float32)
    for l in range(L):
        row = np.concatenate([f(inp["norm1_g"])[l], f(inp["norm2_g"])[l], f(inp["subln_g"])[l],
                              f(inp["b_group"])[l], f(inp["b_expert"])[l],
                              f(inp["lam_q1"])[l], f(inp["lam_k1"])[l], f(inp["lam_q2"])[l], f(inp["lam_k2"])[l]])
        rowp[l] = np.broadcast_to(row[None, :], (128, NRP))
        colp[l, :, CP_QNA] = np.tile(f(inp["qn_a"])[l], 2)
        colp[l, :, CP_KNA] = np.tile(f(inp["kn_a"])[l], 2)
        qc = np.tile(f(inp["qn_c"])[l], 4)
        m0 = (np.arange(128) % 64) // 32 == 0
        colp[l, m0, CP_QNC] = qc[m0]
        colp[l, ~m0, CP_QNC1] = qc[~m0]
        colp[l, :, CP_KNC] = np.tile(f(inp["kn_c"])[l], 4)
        colp[l, :, CP_QND] = np.tile(f(inp["qn_d"])[l], 2)
        colp[l, :, CP_KND] = np.tile(f(inp["kn_d"])[l], 2)
        colp[l, :, CP_BF] = np.tile(f(inp["b_f"])[l], 32)
        colp[l, :, CP_BETA:CP_BETA + 8] = f(inp["mix_beta"])[l].reshape(8, 128).T
    shared = dict(
        ada_w=f(inp["ada_w"]),
        ada_b=np.ascontiguousarray(np.broadcast_to(f(inp["ada_b"])[:, None, :], (L, 128, 6 * D))),
        w_in=f(inp["w_in"]), w_out=f(inp["w_out"]),
        w_r=np.ascontiguousarray(np.concatenate([f(inp["w_group"]), f(inp["w_expert"])], axis=-1)),
        w1=f(inp["w1"]).reshape(L, 32, D, 256), w3=f(inp["w3"]).reshape(L, 32, D, 256),
        w2=f(inp["w2"]).reshape(L, 32, 256, D),
        rowp=rowp, colp=colp, cb=cbv, cf=cfv, alc=make_alc(), onesr=np.ones((2, S), ml_dtypes.bfloat16))
    xs, cs = f(inp["x"]), f(inp["c"])
    maps = []
    for b in range(n_cores):
        m = dict(shared)
        m["x"] = xs[b]
        m["cT"] = np.ascontiguousarray(cs[b].reshape(8, 128).T)
        maps.append(m)
    return maps


_NC_CACHE = {}


def kernel(**inputs):
    if "nc" not in _NC_CACHE:
        nc = bass.Bass("TRN2", target_bir_lowering=False)
        _NC_CACHE["nc"] = build(nc, {})
    nc = _NC_CACHE["nc"]
    maps = prep_inputs(inputs, 8)
    res = run_bass_kernel_spmd(nc, maps, core_ids=list(range(8)))
    return np.stack([np.asarray(r["out"], dtype=np.float32) for r in res.results], axis=0)
```

```python
import math
import numpy as np
import ml_dtypes
import concourse.bass as bass
import concourse.mybir as mybir
from concourse.bass_utils import run_bass_kernel_spmd

F32 = mybir.dt.float32
BF16 = mybir.dt.bfloat16
ALU = mybir.AluOpType
AF = mybir.ActivationFunctionType

D = 1024
S = 2048
NT = 16
L = 2
P_IN = 2988
EPS = 1e-6
NEG = -30000.0
SEM_LIMIT = 20000


class Ticket:
    __slots__ = ("sem", "val", "eng")

    def __init__(self, sem, val, eng):
        self.sem, self.val, self.eng = sem, val, eng


class Region:
    __slots__ = ("w", "r")

    def __init__(self):
        self.w = None
        self.r = []


class Emitter:
    ENGS = ("pe", "act", "dve", "pool", "sp")

    def __init__(self, nc):
        self.nc = nc
        self.prog = {e: [] for e in self.ENGS}
        self.cur_sem = {}
        self.cnt = {}
        self.nsem = 0
        for e in self.ENGS:
            self._new_eng_sem(e)
        self.waited = {}
        self.chan = {}
        self.regions = {}
        self.pending_dma = []

    def _alloc_sem(self, name):
        self.nsem += 1
        return self.nc.alloc_semaphore(name=f"{name}_{self.nsem}")

    def _new_eng_sem(self, e):
        self.cur_sem[e] = self._alloc_sem("e" + e)
        self.cnt[e] = 0

    def R(self, key):
        r = self.regions.get(key)
        if r is None:
            r = self.regions[key] = Region()
        return r

    def _regs(self, lst):
        return [x if isinstance(x, Region) else self.R(x) for x in (lst or [])]

    def _waits_for(self, eng, deps):
        best = {}
        for t in deps:
            if t.eng == "pe" and eng == "pe":
                continue
            k = id(t.sem)
            if k not in best or best[k].val < t.val:
                best[k] = t
        waits = []
        for k, t in best.items():
            if t.eng != "dma":
                wk = (eng, k)
                if self.waited.get(wk, 0) >= t.val:
                    continue
                self.waited[wk] = t.val
            waits.append(t)
        return waits

    def _collect(self, eng, reads, writes):
        deps = []
        for r in reads:
            if r.w is not None:
                deps.append(r.w)
        for w in writes:
            if w.w is not None:
                deps.append(w.w)
            deps.extend(w.r)
        return self._waits_for(eng, deps)

    def _update(self, ticket, reads, writes):
        for w in writes:
            w.w = ticket
            w.r = []
        for r in reads:
            if r not in writes:
                r.r.append(ticket)

    def op(self, eng, fn, reads=None, writes=None, inc=True):
        reads, writes = self._regs(reads), self._regs(writes)
        waits = self._collect(eng, reads, writes)
        if self.cnt[eng] >= SEM_LIMIT:
            self._new_eng_sem(eng)
        sem = self.cur_sem[eng]
        if inc:
            self.cnt[eng] += 1
            ticket = Ticket(sem, self.cnt[eng], eng)
        else:
            assert eng == "pe"
            ticket = Ticket(sem, self.cnt[eng] + 1, eng)

        def emit(e, waits=waits, fn=fn, sem=sem, inc=inc):
            for t in waits:
                e.wait_ge(t.sem, t.val)
            if inc:
                fn(e).then_inc(sem, 1)
            else:
                fn(e)

        self.prog[eng].append(emit)
        self._update(ticket, reads, writes)
        return ticket

    def dma(self, queue, chan, fn, reads=None, writes=None):
        reads, writes = self._regs(reads), self._regs(writes)
        waits = self._collect(queue, reads, writes)
        c = self.chan.get(chan)
        if c is None:
            c = self.chan[chan] = [self._alloc_sem("d"), 0]
        c[1] += 16
        ticket = Ticket(c[0], c[1], "dma")
        sem = c[0]

        def emit(e, waits=waits, fn=fn, sem=sem):
            for t in waits:
                e.wait_ge(t.sem, t.val)
            fn(e).then_inc(sem, 16)

        self.prog[queue].append(emit)
        self._update(ticket, reads, writes)
        self.pending_dma.append(ticket)
        return ticket

    def barrier(self):
        deps = [Ticket(self.cur_sem[e], self.cnt[e], e) for e in self.ENGS if self.cnt[e] > 0]
        last = {}
        for t in self.pending_dma:
            last[id(t.sem)] = t if (id(t.sem) not in last or last[id(t.sem)].val < t.val) else last[id(t.sem)]
        dmas = list(last.values())
        self.pending_dma = dmas
        for eng in self.ENGS:
            ws = [t for t in deps if t.eng != eng] + dmas

            def emit(e, ws=ws):
                for t in ws:
                    e.wait_ge(t.sem, t.val)

            self.prog[eng].append(emit)
            for t in deps:
                if t.eng != eng:
                    self.waited[(eng, id(t.sem))] = max(self.waited.get((eng, id(t.sem)), 0), t.val)

    def wait_all(self, eng, regions):
        regs = self._regs(regions)
        waits = self._collect(eng, regs, regs)

        def emit(e, waits=waits):
            for t in waits:
                e.wait_ge(t.sem, t.val)

        self.prog[eng].append(emit)

    def finish(self):
        nc = self.nc
        with nc.Block() as block:
            @block.tensor
            def _(e):
                for f in self.prog["pe"]:
                    f(e)

            @block.scalar
            def _(e):
                for f in self.prog["act"]:
                    f(e)

            @block.vector
            def _(e):
                for f in self.prog["dve"]:
                    f(e)

            @block.gpsimd
            def _(e):
                for f in self.prog["pool"]:
                    f(e)

            @block.sync
            def _(e):
                for f in self.prog["sp"]:
                    f(e)


CB_IDENT, CB_TRIADD, CB_TRIMUL, CB_B64, CB_B32, CB_ONES, CB_NSLOPE, CB_POS = (
    0, 128, 256, 384, 512, 640, 768, 1792)
NCB = 2304
CF_IDENT, CF_ALIBI, CF_NEGTRI, CF_GMASK = 0, 128, 288, 416
NCF = 420
AL_N, AL_O = 20, 16
SLOPES = [2.0 ** (-8.0 * i / 8) for i in range(1, 9)]
SLOPES_CD = SLOPES[0::2] + SLOPES[1::2]


def make_consts():
    cb = np.zeros((128, NCB), np.float32)
    p = np.arange(128)
    cb[:, CB_IDENT:CB_IDENT + 128] = np.eye(128)
    cb[:, CB_TRIADD:CB_TRIADD + 128] = np.where(p[:, None] <= p[None, :], 0.0, NEG)
    cb[:, CB_TRIMUL:CB_TRIMUL + 128] = (p[:, None] < p[None, :]).astype(np.float32)
    cb[:, CB_B64:CB_B64 + 128] = (p[:, None] // 64 == p[None, :] // 64)
    cb[:, CB_B32:CB_B32 + 128] = (p[:, None] // 32 == p[None, :] // 32)
    cb[:, CB_ONES:CB_ONES + 128] = 1.0
    for h in range(8):
        cb[0:2, CB_NSLOPE + h * 128:CB_NSLOPE + (h + 1) * 128] = -SLOPES_CD[h]
    t = np.arange(512)
    cb[0, CB_POS:CB_POS + 512] = 128 * (t // 128)
    cb[1, CB_POS:CB_POS + 512] = t % 128
    cf = np.zeros((128, NCF), np.float32)
    cf[:, CF_IDENT:CF_IDENT + 128] = np.eye(128)
    for h in range(8):
        for d in range(-16, 4):
            cf[:, CF_ALIBI + h * AL_N + d + AL_O] = SLOPES_CD[h] * (p + 128 * d)
    cf[:, CF_NEGTRI:CF_NEGTRI + 128] = np.where(p[None, :] <= p[:, None], 0.0, -1e30)
    for g in range(4):
        cf[:, CF_GMASK + g] = (p // 32 == g)
    return cb.astype(ml_dtypes.bfloat16), cf


def make_alc():
    t = np.arange(S)
    a = np.zeros((4, 2, S), np.float32)
    for h in range(4):
        a[h, 0] = -SLOPES_CD[h] * 128 * ((t % 512) // 128)
        a[h, 1] = -SLOPES_CD[h] * (t % 128)
    return a.astype(ml_dtypes.bfloat16)


RP_N1, RP_N2, RP_SUBLN, RP_BR, RP_LAM = 0, 1024, 2048, 2112, 2148
NRP = 2276
CP_QNA, CP_KNA, CP_QNC, CP_KNC, CP_QND, CP_KND, CP_BF, CP_BETA, CP_QNC1 = 0, 1, 2, 3, 4, 5, 6, 7, 15
NCP = 16


def build(nc, cfg):
    nl = cfg.get("n_layers", L)
    mixers = cfg.get("mixers", "ABCD")
    do_moe = cfg.get("moe", True)
    stopC = cfg.get("stopC", 99)
    stopD = cfg.get("stopD", 99)
    cdbg = cfg.get("cdbg", "")
    em = Emitter(nc)
    op, dma = em.op, em.dma

    def dram(name, shape, dt=F32, kind="ExternalInput"):
        return nc.dram_tensor(name, list(shape), dt, kind=kind).ap()

    x_d = dram("x", [S, D])
    out_d = dram("out", [S, D], kind="ExternalOutput")
    cT_d = dram("cT", [128, 8])
    ada_w_d = dram("ada_w", [L, D, 6 * D])
    ada_b_d = dram("ada_b", [L, 128, 6 * D])
    w_in_d = dram("w_in", [L, D, P_IN])
    w_out_d = dram("w_out", [L, D, D])
    w_r_d = dram("w_r", [L, D, 36])
    w1_d = dram("w1", [L, 32, D, 256])
    w3_d = dram("w3", [L, 32, D, 256])
    w2_d = dram("w2", [L, 32, 256, D])
    rowp_d = dram("rowp", [L, 128, NRP])
    colp_d = dram("colp", [L, 128, NCP])
    cb_d = dram("cb", [128, NCB], BF16)
    cf_d = dram("cf", [128, NCF])
    alc_d = dram("alc", [4, 2, S], BF16)
    onesr_d = dram("onesr", [2, S], BF16)
    modb_d = dram("modb", [L, 128, 6 * D], kind="Internal")

    _sbc = {}

    def sb(name, shape, dt):
        if name not in _sbc:
            _sbc[name] = nc.alloc_sbuf_tensor("sb_" + name, list(shape), dt).ap()
        return _sbc[name]

    x = sb("x", [128, NT, D], F32)
    hT = sb("hT", [128, 8, S], BF16)
    cb = sb("cbs", [128, NCB], BF16)
    cf = sb("cfs", [128, NCF], F32)
    rowp = sb("rowps", [128, NRP - 2048], F32)
    colp = sb("colps", [128, NCP], F32)
    gate = sb("gate", [128, D], F32)
    small = sb("small", [128, 256], F32)
    arena = sb("arena", [128, 84224 // 2], BF16)
    ps = [nc.alloc_psum_tensor(f"ps{i}", [128, 512], F32).ap() for i in range(8)]
    PS = [f"ps{i}" for i in range(8)]

    ident_b = cb[:, CB_IDENT:CB_IDENT + 128]
    tri_add = cb[:, CB_TRIADD:CB_TRIADD + 128]
    tri_mul = cb[:, CB_TRIMUL:CB_TRIMUL + 128]
    bones64 = cb[:, CB_B64:CB_B64 + 128]
    bones32 = cb[:, CB_B32:CB_B32 + 128]
    ones_b = cb[:, CB_ONES:CB_ONES + 128]
    ident_f = cf[:, CF_IDENT:CF_IDENT + 128]

    def carve(off_bytes, shape, dt):
        n = int(np.prod(shape[1:]))
        esz = 4 if dt == F32 else 2
        a = arena[0:shape[0], off_bytes // 2: off_bytes // 2 + n * esz // 2]
        if dt == F32:
            a = a.bitcast(F32)
        if len(shape) == 3:
            a = a.rearrange("p (a b) -> p a b", a=shape[1])
        elif len(shape) == 4:
            a = a.rearrange("p (a b c) -> p a b c", a=shape[1], b=shape[2])
        return a

    def mm(out, lhsT, rhs, start, stop, reads, writes, inc=True):
        kw = {"skip_group_check": True}
        try:
            lhsT.base_partition()
        except BaseException:
            kw["tile_position"] = (96, 0)
        return op("pe", lambda e: e.matmul(out, lhsT=lhsT, rhs=rhs, start=start, stop=stop, **kw),
                  reads=reads, writes=writes, inc=inc)

    dma("sp", "c0", lambda e: e.dma_start(out=cb, in_=cb_d), writes=["cb"])
    dma("sp", "c1", lambda e: e.dma_start(out=cf, in_=cf_d), writes=["cf"])
    for t in range(NT):
        dma("sp", f"xin{t}", lambda e, t=t: e.dma_start(out=x[:, t, :], in_=x_d[t * 128:(t + 1) * 128, :]),
            writes=[("x", t)])
    cTs = small[:, 0:8]
    dma("sp", "c2", lambda e: e.dma_start(out=cTs, in_=cT_d), writes=["cT"])
    sc_e = small[:, 8:16]
    op("act", lambda e: e.activation(out=sc_e, in_=cTs, func=AF.Exp, scale=-1.0), reads=["cT"], writes=["sc_e"])
    op("dve", lambda e: e.tensor_scalar(out=sc_e, in0=sc_e, scalar1=1.0, scalar2=None, op0=ALU.add),
       reads=["sc_e"], writes=["sc_e"])
    op("dve", lambda e: e.reciprocal(out=sc_e, in_=sc_e), reads=["sc_e"], writes=["sc_e"])
    op("dve", lambda e: e.tensor_tensor(out=sc_e, in0=sc_e, in1=cTs, op=ALU.mult), reads=["sc_e", "cT"],
       writes=["sc_e"])
    eps_t = small[:, 16:17]
    op("dve", lambda e: e.memset(eps_t, EPS), writes=["eps"])
    lhs_rep = sb("lhs_rep", [128, 8, 128], BF16)
    gs = carve(20480, [128, D], F32)
    shf = carve(24576, [128, D], F32)
    for c in range(8):
        op("dve", lambda e, c=c: e.tensor_scalar(out=lhs_rep[:, c, :], in0=ones_b, scalar1=sc_e[:, c:c + 1],
                                                 scalar2=None, op0=ALU.mult),
           reads=["cb", "sc_e"], writes=["lhs_rep"])

    def phase_mod(l):
        aw = [carve(0, [128, 8, 512], BF16), carve(8192, [128, 8, 512], BF16)]
        ab = [carve(16384, [128, 512], F32), carve(18432, [128, 512], F32)]
        mo = [carve(20480, [128, 512], F32), carve(22528, [128, 512], F32)]
        awv = ada_w_d[l].rearrange("(c p) n -> p c n", p=128)
        for pc in range(12):
            b = pc % 2
            dma("pool", f"aw{b}", lambda e, b=b, pc=pc: e.dma_start(out=aw[b], in_=awv[:, :, pc * 512:(pc + 1) * 512]),
                writes=[("aw", b)])
            dma("sp", f"ab{b}", lambda e, b=b, pc=pc: e.dma_start(out=ab[b], in_=ada_b_d[l][:, pc * 512:(pc + 1) * 512]),
                writes=[("ab", b)])
            for c in range(8):
                mm(ps[b], lhs_rep[:, c, :], aw[b][:, c, :], c == 0, c == 7,
                   reads=["lhs_rep", ("aw", b)], writes=[PS[b]], inc=(c == 7))
            op("dve", lambda e, b=b: e.tensor_tensor(out=mo[b], in0=ps[b], in1=ab[b], op=ALU.add),
               reads=[PS[b], ("ab", b)], writes=[("mo", b)])
            dma("sp", f"mo{b}", lambda e, b=b, pc=pc: e.dma_start(out=modb_d[l][:, pc * 512:(pc + 1) * 512], in_=mo[b]),
                reads=[("mo", b)], writes=[("modb", l, pc // 2)])
        dma("sp", "rp", lambda e: e.dma_start(out=rowp, in_=rowp_d[l][:, 2048:NRP]), writes=["rowp"])
        dma("sp", "cp", lambda e: e.dma_start(out=colp, in_=colp_d[l]), writes=["colp"])

    def phase_norm(l, which):
        i_shift, i_scale = (0, 1) if which == 0 else (3, 4)
        tmpg = carve(0, [128, D], F32)
        junk = carve(4096, [128, D], BF16)
        tf = [carve(8192, [128, D], F32), carve(12288, [128, D], F32)]
        hb = [carve(16384, [128, D], BF16), carve(18432, [128, D], BF16)]
        ss = small[:, 32:48]
        rstd = small[:, 48:64]
        dma("sp", "gs", lambda e: e.dma_start(out=gs, in_=modb_d[l][:, i_scale * D:(i_scale + 1) * D]),
            reads=[("modb", l, i_scale)], writes=["gs"])
        dma("sp", "tg", lambda e: e.dma_start(out=tmpg, in_=rowp_d[l][:, which * D:(which + 1) * D]), writes=["tmpg"])
        dma("sp", "sh", lambda e: e.dma_start(out=shf, in_=modb_d[l][:, i_shift * D:(i_shift + 1) * D]),
            reads=[("modb", l, i_shift)], writes=["shf"])
        op("dve", lambda e: e.scalar_tensor_tensor(out=gs, in0=gs, scalar=1.0, in1=tmpg, op0=ALU.add, op1=ALU.mult),
           reads=["gs", "tmpg"], writes=["gs"])
        for t in range(NT):
            op("act", lambda e, t=t: e.activation(out=junk, in_=x[:, t, :], func=AF.Square, accum_out=ss[:, t:t + 1]),
               reads=[("x", t)], writes=["junk", "ss"])
        op("act", lambda e: e.activation(out=rstd, in_=ss, func=AF.Ln, bias=eps_t, scale=1.0 / D),
           reads=["ss", "eps"], writes=["rstd"])
        op("act", lambda e: e.activation(out=rstd, in_=rstd, func=AF.Exp, scale=-0.5), reads=["rstd"], writes=["rstd"])
        pst = ps[5].bitcast(BF16).rearrange("p (a b) -> p a b", a=8)
        for t in range(NT):
            b = t % 2
            op("dve", lambda e, t=t, b=b: e.scalar_tensor_tensor(out=tf[b], in0=x[:, t, :], scalar=rstd[:, t:t + 1], in1=gs,
                                                                 op0=ALU.mult, op1=ALU.mult),
               reads=[("x", t), "rstd", "gs"], writes=[("tf", b)])
            op("pool", lambda e, b=b: e.tensor_tensor(out=hb[b], in0=tf[b], in1=shf, op=ALU.add),
               reads=[("tf", b), "shf"], writes=[("hb", b)])
            for c in range(8):
                op("pe", lambda e, b=b, c=c: e.transpose(pst[:, c, :], hb[b][:, c * 128:(c + 1) * 128], ident_b),
                   reads=[("hb", b), "cb"], writes=[PS[5]])
            op("act", lambda e, t=t: e.activation(out=hT[:, :, t * 128:(t + 1) * 128], in_=pst, func=AF.Copy),
               reads=[PS[5]], writes=[("hT", t // 4)])

    A_BUFQ, A_BUFK, A_BUFIQ, A_BUFIK = 0, 8192, 16384, 24576
    A_V = 28672
    A_MIXTOK = 37120
    A_MIXT = 45312
    A_SCR = 53504
    A_W = 78080
    bufQ = carve(A_BUFQ, [128, 2, S], BF16)
    bufK = carve(A_BUFK, [128, 2, S], BF16)
    bufIQ = carve(A_BUFIQ, [128, 2, S], BF16)
    bufIK = carve(A_BUFIK, [128, S], BF16)
    Vaug = carve(A_V, [128, NT, 4, 65], BF16)
    mix_tok = carve(A_MIXTOK, [128, NT, 256], BF16)
    mixT = carve(A_MIXT, [128, 2, S], BF16)
    wst = [carve(A_W, [128, 8, 128], BF16), carve(A_W + 2048, [128, 8, 128], BF16),
           carve(A_W + 4096, [128, 8, 128], BF16)]
    wst_i = [0]
    sqb = sb("sqb", [128, 512], BF16)
    lnv = sb("lnv", [128, 512], F32)
    pT = [sb("pT0", [128, 512], BF16), sb("pT1", [128, 512], BF16), sb("pT2", [128, 512], BF16)]
    SBANK = [0, 1, 6]
    ac_i = [0]
    recs = sb("recs", [128, 16], F32)

    def load_w(l, c0, ncols):
        i = wst_i[0] % 3
        wst_i[0] += 1
        wv = w_in_d[l].rearrange("(c p) n -> p c n", p=128)
        dst = wst[i][:, :, 0:ncols]
        dma("pool", f"wst{i}", lambda e: e.dma_start(out=dst, in_=wv[:, :, c0:c0 + ncols]), writes=[("wst", i)])
        return dst, ("wst", i)

    pj_i = [0]

    def projT(wt, wreg, M, tg):
        b = pj_i[0] % 2
        pj_i[0] += 1
        for c in range(8):
            mm(ps[b][0:M, :], wt[:, c, 0:M], hT[:, c, tg * 512:(tg + 1) * 512], c == 0, c == 7,
               reads=[wreg, ("hT", tg)], writes=[PS[b]], inc=(c == 7))
        return ps[b], PS[b]

    def qk_norm_store(pb, preg, M, bones, inv_d, gcol, lnscale, dst, dreg, out2=None):
        op("act", lambda e: e.activation(out=sqb[0:M, :], in_=pb[0:M, :], func=AF.Square), reads=[preg], writes=["sqb"])
        mm(ps[4][0:M, :], bones[0:M, 0:M], sqb[0:M, :], True, True, reads=["sqb", "cb"], writes=[PS[4]])
        op("act", lambda e: e.activation(out=lnv[0:M, :], in_=ps[4][0:M, :], func=AF.Ln, bias=eps_t[0:M, :], scale=inv_d),
           reads=[PS[4], "eps"], writes=["lnv"])
        lb = small[:, 17 + int(lnscale):18 + int(lnscale)]
        op("act", lambda e: e.activation(out=lnv[0:M, :], in_=lnv[0:M, :], func=AF.Exp, bias=lb[0:M, :], scale=-0.5),
           reads=["lnv", "lnb"], writes=["lnv"])
        op("dve", lambda e: e.scalar_tensor_tensor(out=dst, in0=pb[0:M, :], scalar=gcol[0:M, :], in1=lnv[0:M, :],
                                                   op0=ALU.mult, op1=ALU.mult),
           reads=[preg, "lnv", "colp"], writes=[dreg])
        if out2 is not None:
            gcol2, dst2, dreg2 = out2
            op("dve", lambda e: e.scalar_tensor_tensor(out=dst2, in0=pb[0:M, :], scalar=gcol2[0:M, :], in1=lnv[0:M, :],
                                                       op0=ALU.mult, op1=ALU.mult),
               reads=[preg, "lnv", "colp"], writes=[dreg2])

    def proj_v(l, c0, nheads, dstV, vreg):
        ncols = nheads * 64
        wparts = []
        for j in range(0, ncols, 128):
            wparts.append(load_w(l, c0 + j, min(128, ncols - j)))
        for t in range(NT):
            b = pj_i[0] % 2
            pj_i[0] += 1
            for j, (wt, wreg) in enumerate(wparts):
                n = wt.shape[2]
                for c in range(8):
                    mm(ps[b][:, j * 128:j * 128 + n], hT[:, c, t * 128:(t + 1) * 128], wt[:, c, :], c == 0 and j == 0, c == 7,
                       reads=[wreg, ("hT", t // 4)], writes=[PS[b]])
            src = ps[b][:, 0:ncols].rearrange("p (h d) -> p h d", h=nheads)
            op("act", lambda e, t=t, src=src: e.activation(out=dstV[:, t, 0:nheads, 0:64], in_=src, func=AF.Copy),
               reads=[PS[b]], writes=[vreg])

    po_i = [0]

    def run_pipelined(items, depth=1):
        for k in range(min(depth, len(items))):
            items[k][0]()
        for i, (_, s2) in enumerate(items):
            if i + depth < len(items):
                items[i + depth][0]()
            s2()

    def attn_core(nheads_spec, finish_fn, QG=512):
        nqb = QG // 128
        items = []
        for si, sp_ in enumerate(nheads_spec):
            for qg in range(S // QG):
                pb = 2 + (po_i[0] % 2)
                po_i[0] += 1
                po, poreg = ps[pb], PS[pb]
                first_po = [True]
                nkb = nqb * (qg + 1)
                for kb in range(nkb):
                    st = {}

                    def s1(sp_=sp_, qg=qg, kb=kb, st=st):
                        j = kb - nqb * qg
                        c0 = 0 if j < 0 else j * 128
                        sbk = ac_i[0] % 3
                        ac_i[0] += 1
                        pss, psreg = ps[SBANK[sbk]], PS[SBANK[sbk]]
                        has_extra = sp_.get("extra") is not None
                        dm = sp_.get("diag_mask", False) and j >= 0
                        mm(pss[:, c0:QG], sp_["kT"](kb), sp_["qT"](qg * QG + c0, (qg + 1) * QG), True,
                           not (has_extra or dm), reads=sp_["kq_regs"], writes=[psreg])
                        if has_extra:
                            sp_["extra"](pss, psreg, qg, kb, c0, not dm)
                        if dm:
                            mm(pss[:, c0:c0 + 128], ident_b, tri_add, False, True, reads=["cb"], writes=[psreg])
                        st.update(sbk=sbk, c0=c0)

                    def s2(sp_=sp_, si=si, qg=qg, kb=kb, st=st, po=po, poreg=poreg, first_po=first_po, last=(kb == nkb - 1)):
                        sbk, c0 = st["sbk"], st["c0"]
                        pss, psreg = ps[SBANK[sbk]], PS[SBANK[sbk]]
                        pt = pT[sbk]
                        bias_ap = sp_["bias"](kb, qg)
                        op("act", lambda e, pt=pt, pss=pss, c0=c0, bias_ap=bias_ap: e.activation(
                            out=pt[:, c0:QG], in_=pss[:, c0:QG], func=AF.Exp, bias=bias_ap),
                           reads=[psreg] + sp_["bias_regs"], writes=[("pT", sbk)])
                        for i in range(c0 // 128, nqb):
                            mm(po[:, i * 65:(i + 1) * 65], pt[:, i * 128:(i + 1) * 128], sp_["V"](kb), first_po[0], False,
                               reads=[("pT", sbk)] + sp_["v_regs"], writes=[poreg])
                            first_po[0] = False
                        if last:
                            finish_fn(si, qg, po, poreg)

                    items.append((s1, s2))
        run_pipelined(items, depth=2)

    def finish_softmax(mcol0):
        def fin(si, qg, po, poreg):
            for i in range(4):
                t = qg * 4 + i
                op("dve", lambda e, i=i: e.reciprocal(out=recs[:, i:i + 1], in_=po[:, i * 65 + 64:i * 65 + 65]),
                   reads=[poreg], writes=["recs"])
                op("dve", lambda e, i=i, t=t: e.tensor_scalar(out=mix_tok[:, t, mcol0 + si * 64:mcol0 + (si + 1) * 64],
                                                              in0=po[:, i * 65:i * 65 + 64], scalar1=recs[:, i:i + 1],
                                                              scalar2=None, op0=ALU.mult),
                   reads=[poreg, "recs"], writes=[("mix_tok", t)])
        return fin

    def mixer_out(l, m):
        pst = ps[5].bitcast(BF16).rearrange("p (a b) -> p a b", a=8)
        for t in range(NT):
            for c in range(2):
                op("pe", lambda e, t=t, c=c: e.transpose(pst[:, c, :], mix_tok[:, t, c * 128:(c + 1) * 128], ident_b),
                   reads=[("mix_tok", t), "cb"], writes=[PS[5]])
            op("act", lambda e, t=t: e.activation(out=mixT[:, :, t * 128:(t + 1) * 128], in_=pst[:, 0:2, :], func=AF.Copy),
               reads=[PS[5]], writes=["mixT"])
        wo_f = carve(A_SCR, [128, 2, D], F32)
        wo_b = carve(A_SCR + 8192, [128, 2, D], BF16)
        wv = w_out_d[l][m * 256:(m + 1) * 256, :].rearrange("(c p) n -> p c n", p=128)
        dma("sp", "wo", lambda e: e.dma_start(out=wo_f, in_=wv), writes=["wo_f"])
        for c in range(2):
            bc = colp[:, CP_BETA + m * 2 + c:CP_BETA + m * 2 + c + 1]
            op("dve", lambda e, c=c, bc=bc: e.scalar_tensor_tensor(out=wo_b[:, c, :], in0=wo_f[:, c, :], scalar=bc, in1=gate,
                                                                   op0=ALU.mult, op1=ALU.mult),
               reads=["wo_f", "colp", "gate"], writes=["wo_b"])
        for t in range(NT):
            for hf in range(2):
                b = 6 + (t * 2 + hf) % 2
                for c in range(2):
                    mm(ps[b], mixT[:, c, t * 128:(t + 1) * 128], wo_b[:, c, hf * 512:(hf + 1) * 512], c == 0, c == 1,
                       reads=["mixT", "wo_b"], writes=[PS[b]])
                op("dve", lambda e, t=t, hf=hf, b=b: e.tensor_tensor(out=x[:, t, hf * 512:(hf + 1) * 512],
                                                                     in0=x[:, t, hf * 512:(hf + 1) * 512], in1=ps[b], op=ALU.add),
                   reads=[PS[b], ("x", t)], writes=[("x", t)])

    def set_v_ones():
        op("pool", lambda e: e.memset(Vaug[:, :, :, 64:65], 1.0), writes=["V"])

    def set_lnb():
        op("dve", lambda e: e.memset(small[:, 17:18], 0.0), writes=["lnb"])
        op("dve", lambda e: e.memset(small[:, 18:19], math.log(0.125)), writes=["lnb"])
        op("dve", lambda e: e.memset(small[:, 19:20], math.log(32 ** -0.5)), writes=["lnb"])

    def mixer_A(l):
        set_v_ones()
        spT = carve(A_SCR, [4, S], F32)
        cums = carve(A_SCR + 8192, [4, S], F32)
        chiT = carve(A_SCR + 16384, [4, S], BF16)
        ones4 = carve(A_SCR + 20480, [4, 512], F32)
        poscum = sb("poscum", [128, NT, 4], F32)
        nbf = small[:, 20:21]
        op("dve", lambda e: e.tensor_scalar(out=nbf, in0=colp[:, CP_BF:CP_BF + 1], scalar1=-1.0, scalar2=None, op0=ALU.mult),
           reads=["colp"], writes=["nbf"])
        op("dve", lambda e: e.memset(ones4, 1.0), writes=["ones4"])
        for (c0, dst, gc, lns, dreg) in ((0, bufQ, CP_QNA, 1.0, "bufQ"), (256, bufK, CP_KNA, 0.0, "bufK")):
            for ch in range(2):
                wt, wreg = load_w(l, c0 + ch * 128, 128)
                for tg in range(4):
                    pb, preg = projT(wt, wreg, 128, tg)
                    qk_norm_store(pb, preg, 128, bones64, 1.0 / 64, colp[:, gc:gc + 1], lns,
                                  dst[:, ch, tg * 512:(tg + 1) * 512], dreg)
        wt, wreg = load_w(l, 768, 4)
        for tg in range(4):
            pb, preg = projT(wt, wreg, 4, tg)
            op("act", lambda e, pb=pb, tg=tg: e.activation(out=spT[:, tg * 512:(tg + 1) * 512], in_=pb[0:4, :], func=AF.Exp,
                                                           bias=nbf[0:4, :], scale=-1.0),
               reads=[preg, "nbf"], writes=["spT"])
        op("act", lambda e: e.activation(out=spT, in_=spT, func=AF.Ln, bias=1.0), reads=["spT"], writes=["spT"])
        for tg in range(4):
            init = 0.0 if tg == 0 else cums[:, tg * 512 - 1:tg * 512]
            op("dve", lambda e, tg=tg, init=init: e.tensor_tensor_scan(out=cums[:, tg * 512:(tg + 1) * 512], data0=ones4,
                                                                       data1=spT[:, tg * 512:(tg + 1) * 512], initial=init,
                                                                       op0=ALU.mult, op1=ALU.add),
               reads=["spT", "ones4", "cums"], writes=["cums"])
        op("dve", lambda e: e.tensor_scalar(out=chiT, in0=cums, scalar1=-1.0, scalar2=None, op0=ALU.mult),
           reads=["cums"], writes=["chiT"])
        for t in range(NT):
            op("pe", lambda e, t=t: e.transpose(ps[4][:, t * 4:(t + 1) * 4], cums[0:4, t * 128:(t + 1) * 128], ident_f[0:4, 0:4]),
               reads=["cums", "cf"], writes=[PS[4]])
        op("dve", lambda e: e.tensor_copy(out=poscum, in_=ps[4][:, 0:64].rearrange("p (t h) -> p t h", t=NT)),
           reads=[PS[4]], writes=["poscum"])
        proj_v(l, 512, 4, Vaug, "V")
        sel4 = [sb(f"sel4_{h_}", [4, 128], BF16) for h_ in range(4)]
        for h_ in range(4):
            op("dve", lambda e, h_=h_: e.tensor_scalar(out=sel4[h_], in0=ones_b[0:4, :], scalar1=ident_f[0:4, h_:h_ + 1],
                                                       scalar2=None, op0=ALU.mult), reads=["cb", "cf"], writes=["sel4"])
        specs = []
        for h in range(4):
            hp, ch = (h % 2) * 64, h // 2

            def extra(pss, psreg, qg, kb, c0, last, h=h):
                mm(pss[:, c0:512], sel4[h],
                   chiT[0:4, qg * 512 + c0:(qg + 1) * 512], False, last, reads=["sel4", "chiT"], writes=[psreg])

            specs.append(dict(
                kT=lambda kb, hp=hp, ch=ch: bufK[hp:hp + 64, ch, kb * 128:(kb + 1) * 128],
                qT=lambda q0, q1, hp=hp, ch=ch: bufQ[hp:hp + 64, ch, q0:q1],
                kq_regs=["bufQ", "bufK"], extra=extra, diag_mask=True,
                bias=lambda kb, qg, h=h: poscum[:, kb, h:h + 1], bias_regs=["poscum"],
                V=lambda kb, h=h: Vaug[:, kb, h, :], v_regs=["V"]))
        attn_core(specs, finish_softmax(0))

    def mixer_B(l):
        QG = 256
        lbuf = carve(A_SCR, [128, NT, QG], F32)
        l1m = carve(A_SCR + 16384, [128, NT, QG], BF16)
        mixscr = sb("mixscr", [128, 512], F32)
        argb = [mixscr[:, 0:QG], mixscr[:, QG:2 * QG]]
        wTb = [sb("wTb0", [128, QG], BF16), sb("wTb1", [128, QG], BF16)]
        for (c0, dst, scl, dreg) in ((772, bufQ, 0.125, "bufQ"), (1028, bufK, 1.0, "bufK")):
            for ch in range(2):
                wt, wreg = load_w(l, c0 + ch * 128, 128)
                for tg in range(4):
                    pb, preg = projT(wt, wreg, 128, tg)
                    op("dve", lambda e, pb=pb, dst=dst, ch=ch, tg=tg, scl=scl: e.tensor_scalar(
                        out=dst[:, ch, tg * 512:(tg + 1) * 512], in0=pb, scalar1=scl, scalar2=None, op0=ALU.mult),
                       reads=[preg], writes=[dreg])
        proj_v(l, 1284, 4, Vaug, "V")
        for h in range(4):
            hp, ch = (h % 2) * 64, h // 2
            for qg in range(S // QG):
                nkb = 2 * (qg + 1)
                pb_ = 2 + (po_i[0] % 2)
                po_i[0] += 1
                po, poreg = ps[pb_], PS[pb_]
                for kb in range(nkb):
                    j = kb - 2 * qg
                    c0 = 0 if j < 0 else j * 128
                    sbk = pj_i[0] % 2
                    pj_i[0] += 1
                    pss, psreg = ps[sbk], PS[sbk]
                    mm(pss[:, c0:QG], bufK[hp:hp + 64, ch, kb * 128:(kb + 1) * 128],
                       bufQ[hp:hp + 64, ch, qg * QG + c0:(qg + 1) * QG], True, True, reads=["bufQ", "bufK"], writes=[psreg])
                    op("act", lambda e, kb=kb, c0=c0, pss=pss: e.activation(out=lbuf[:, kb, c0:QG], in_=pss[:, c0:QG],
                                                                            func=AF.Exp, scale=-1.0),
                       reads=[psreg], writes=[("lbuf", kb)])
                    op("act", lambda e, kb=kb, c0=c0: e.activation(out=lbuf[:, kb, c0:QG], in_=lbuf[:, kb, c0:QG],
                                                                   func=AF.Ln, bias=1.0),
                       reads=[("lbuf", kb)], writes=[("lbuf", kb)])
                    op("dve", lambda e, kb=kb, c0=c0, pss=pss: e.scalar_tensor_tensor(
                        out=l1m[:, kb, c0:QG], in0=pss[:, c0:QG], scalar=-1.0, in1=lbuf[:, kb, c0:QG],
                        op0=ALU.mult, op1=ALU.subtract),
                       reads=[psreg, ("lbuf", kb)], writes=[("l1m", kb)])
                    if j >= 0:
                        op("dve", lambda e, kb=kb, c0=c0: e.tensor_tensor(out=l1m[:, kb, c0:c0 + 128],
                                                                           in0=l1m[:, kb, c0:c0 + 128], in1=tri_mul, op=ALU.mult),
                           reads=[("l1m", kb), "cb"], writes=[("l1m", kb)])
                first_po = [True]
                items = []
                for kb in range(nkb):
                    st = {}

                    def s1(kb=kb, st=st, qg=qg, nkb=nkb):
                        j = kb - 2 * qg
                        c0 = 0 if j < 0 else j * 128
                        sbk = pj_i[0] % 2
                        pj_i[0] += 1
                        pss, psreg = ps[sbk], PS[sbk]
                        later = list(range(kb + 1, nkb))
                        mm(pss[:, c0:QG], triL, l1m[:, kb, c0:QG], True, len(later) == 0,
                           reads=[("l1m", kb), "triL"], writes=[psreg])
                        for n_, kb2 in enumerate(later):
                            j2 = kb2 - 2 * qg
                            c2 = 0 if j2 < 0 else j2 * 128
                            cc = max(c0, c2)
                            mm(pss[:, cc:QG], ones_b, l1m[:, kb2, cc:QG], False, n_ == len(later) - 1,
                               reads=[("l1m", kb2), "cb"], writes=[psreg])
                        st.update(sbk=sbk, c0=c0, j=j)

                    def s2(kb=kb, st=st, qg=qg, h=h, po=po, poreg=poreg, first_po=first_po, last=(kb == nkb - 1)):
                        sbk, c0, j = st["sbk"], st["c0"], st["j"]
                        pss, psreg = ps[sbk], PS[sbk]
                        ab_ = argb[sbk]
                        op("dve", lambda e, ab_=ab_, pss=pss, kb=kb, c0=c0: e.tensor_tensor(
                            out=ab_[:, c0:QG], in0=pss[:, c0:QG], in1=lbuf[:, kb, c0:QG], op=ALU.subtract),
                           reads=[psreg, ("lbuf", kb)], writes=[("argb", sbk)])
                        wt_ = wTb[sbk]
                        op("act", lambda e, ab_=ab_, wt_=wt_, c0=c0: e.activation(out=wt_[:, c0:QG], in_=ab_[:, c0:QG], func=AF.Exp),
                           reads=[("argb", sbk)], writes=[("wTb", sbk)])
                        if j >= 0:
                            op("dve", lambda e, wt_=wt_, c0=c0: e.tensor_tensor(out=wt_[:, c0:c0 + 128], in0=wt_[:, c0:c0 + 128],
                                                                                 in1=tri_mul, op=ALU.mult),
                               reads=[("wTb", sbk), "cb"], writes=[("wTb", sbk)])
                        for i in range(c0 // 128, 2):
                            mm(po[:, i * 65:i * 65 + 64], wt_[:, i * 128:(i + 1) * 128], Vaug[:, kb, h, 0:64], first_po[0], False,
                               reads=[("wTb", sbk), "V"], writes=[poreg])
                            first_po[0] = False
                        if not last:
                            return
                        for i in range(2):
                            t = qg * 2 + i
                            op("act", lambda e, i=i, t=t, po=po, h=h: e.activation(out=mix_tok[:, t, h * 64:(h + 1) * 64],
                                                                                   in_=po[:, i * 65:i * 65 + 64], func=AF.Copy),
                               reads=[poreg], writes=[("mix_tok", t)])

                    items.append((s1, s2))
                run_pipelined(items)

    triL = sb("triL", [128, 128], BF16)
    op("pe", lambda e: e.transpose(ps[5].bitcast(BF16)[:, 0:128], tri_mul, ident_b), reads=["cb"], writes=[PS[5]])
    op("dve", lambda e: e.tensor_copy(out=triL, in_=ps[5].bitcast(BF16)[:, 0:128]), reads=[PS[5]], writes=["triL"])

    def mixer_C(l):
        lam_init = 0.8 - 0.6 * math.exp(-0.3 * l)
        set_v_ones()
        lamrow = rowp[:, RP_LAM - 2048:RP_LAM - 2048 + 128]
        lt = small[:, 64:128]
        ls = small[:, 24:28]
        op("dve", lambda e: e.tensor_tensor(out=lt[:, 0:32], in0=lamrow[:, 0:32], in1=lamrow[:, 32:64], op=ALU.mult),
           reads=["rowp"], writes=["lt"])
        op("dve", lambda e: e.tensor_tensor(out=lt[:, 32:64], in0=lamrow[:, 64:96], in1=lamrow[:, 96:128], op=ALU.mult),
           reads=["rowp"], writes=["lt"])
        op("dve", lambda e: e.reduce_sum(out=ls[:, 0:1], in_=lt[:, 0:32], axis=mybir.AxisListType.X), reads=["lt"], writes=["ls"])
        op("dve", lambda e: e.reduce_sum(out=ls[:, 1:2], in_=lt[:, 32:64], axis=mybir.AxisListType.X), reads=["lt"], writes=["ls"])
        op("act", lambda e: e.activation(out=ls[:, 0:2], in_=ls[:, 0:2], func=AF.Exp), reads=["ls"], writes=["ls"])
        op("dve", lambda e: e.tensor_tensor(out=ls[:, 2:3], in0=ls[:, 1:2], in1=ls[:, 0:1], op=ALU.subtract),
           reads=["ls"], writes=["ls"])
        op("dve", lambda e: e.tensor_scalar(out=ls[:, 2:3], in0=ls[:, 2:3], scalar1=-lam_init, scalar2=None, op0=ALU.add),
           reads=["ls"], writes=["ls"])
        nlam = ls[:, 2:3]
        if stopC <= 1:
            return
        bufK1 = carve(A_SCR + 4096, [128, 2, S], BF16)
        for (c0, dst, gc, lns, dreg) in ((1540, bufQ, CP_QNC, 2.0, "bufQ"), (1796, bufK, CP_KNC, 0.0, "bufK")):
            for ch in range(2):
                wt, wreg = load_w(l, c0 + ch * 128, 128)
                for tg in range(4):
                    pb, preg = projT(wt, wreg, 128, tg)
                    if dst is bufQ:
                        o2_ = (colp[:, CP_QNC1:CP_QNC1 + 1], bufIQ[:, ch, tg * 512:(tg + 1) * 512], "bufIQ")
                    else:
                        o2_ = (colp[:, gc:gc + 1], bufK1[:, ch, tg * 512:(tg + 1) * 512], "bufK1")
                    qk_norm_store(pb, preg, 128, bones32, 1.0 / 32, colp[:, gc:gc + 1], lns,
                                  dst[:, ch, tg * 512:(tg + 1) * 512], dreg, out2=o2_)
        if stopC <= 2:
            return
        for h in range(4):
            hp, ch = (h % 2) * 64, h // 2
            for c, (qc, qreg, kc, kreg) in enumerate(((bufQ, "bufQ", bufK, "bufK"), (bufIQ, "bufIQ", bufK1, "bufK1"))):
                r0 = hp + 32 * (1 - c)
                dma("sp", f"alq{c}", lambda e, qc=qc, r0=r0, ch=ch, h=h: e.dma_start(out=qc[r0:r0 + 2, ch, :], in_=alc_d[h]),
                    writes=[qreg])
                dma("sp", f"alk{c}", lambda e, kc=kc, r0=r0, ch=ch: e.dma_start(out=kc[r0:r0 + 2, ch, :], in_=onesr_d),
                    writes=[kreg])
        proj_v(l, 2052, 4, Vaug, "V")
        if stopC <= 3:
            return
        o1 = sb("o1c", [128, 64], F32)
        o2 = sb("o2c", [128, 64], F32)
        keep = {}
        specs = []
        for h in range(4):
            for c in range(2):
                hp, ch = (h % 2) * 64, h // 2
                qsrc = bufQ if c == 0 else bufIQ
                ksrc = bufK if c == 0 else bufK1
                specs.append(dict(
                    kT=lambda kb, hp=hp, ch=ch, ksrc=ksrc: ksrc[hp:hp + 64, ch, kb * 128:(kb + 1) * 128],
                    qT=lambda q0, q1, hp=hp, ch=ch, qsrc=qsrc: qsrc[hp:hp + 64, ch, q0:q1],
                    kq_regs=["bufQ", "bufIQ", "bufK", "bufK1"], extra=None, diag_mask=("nodiag" not in cdbg),
                    bias=lambda kb, qg, h=h: cf[:, CF_ALIBI + h * AL_N + (kb - 4 * qg) + AL_O:CF_ALIBI + h * AL_N + (kb - 4 * qg) + AL_O + 1],
                    bias_regs=["cf"],
                    V=lambda kb, h=h: Vaug[:, kb, h, :], v_regs=["V"]))

        stash = carve(A_SCR, [128, NT, 64], F32)

        def fin(si, qg, po, poreg):
            h, c = si // 2, si % 2
            if "nofin" in cdbg:
                return
            for i in range(4):
                t = qg * 4 + i
                op("dve", lambda e, i=i: e.reciprocal(out=recs[:, i:i + 1], in_=po[:, i * 65 + 64:i * 65 + 65]),
                   reads=[poreg], writes=["recs"])
                if c == 0:
                    op("dve", lambda e, i=i, t=t: e.tensor_scalar(out=stash[:, t, :], in0=po[:, i * 65:i * 65 + 64],
                                                                  scalar1=recs[:, i:i + 1], scalar2=None, op0=ALU.mult),
                       reads=[poreg, "recs"], writes=[("stash", t)])
                else:
                    op("dve", lambda e, i=i: e.tensor_scalar(out=o1, in0=po[:, i * 65:i * 65 + 64], scalar1=recs[:, i:i + 1],
                                                             scalar2=nlam, op0=ALU.mult, op1=ALU.mult),
                       reads=[poreg, "recs", "ls"], writes=["o1"])
                    op("dve", lambda e, t=t: e.tensor_tensor(out=o1, in0=o1, in1=stash[:, t, :], op=ALU.add),
                       reads=["o1", ("stash", t)], writes=["o1"])
                    op("act", lambda e: e.activation(out=o2, in_=o1, func=AF.Square, accum_out=small[:, 28:29]),
                       reads=["o1"], writes=["o2", "cs"])
                    op("act", lambda e: e.activation(out=small[:, 28:29], in_=small[:, 28:29], func=AF.Ln, bias=eps_t, scale=1.0 / 64),
                       reads=["cs", "eps"], writes=["cs"])
                    op("act", lambda e: e.activation(out=small[:, 28:29], in_=small[:, 28:29], func=AF.Exp,
                                                     bias=small[:, 21:22], scale=-0.5),
                       reads=["cs", "lnb2"], writes=["cs"])
                    op("dve", lambda e, t=t, h=h: e.scalar_tensor_tensor(out=mix_tok[:, t, h * 64:(h + 1) * 64], in0=o1,
                                                                         scalar=small[:, 28:29],
                                                                         in1=rowp[:, RP_SUBLN - 2048:RP_SUBLN - 2048 + 64],
                                                                         op0=ALU.mult, op1=ALU.mult),
                       reads=["o1", "cs", "rowp"], writes=[("mix_tok", t)])

        op("dve", lambda e: e.memset(small[:, 21:22], math.log(1.0 - lam_init)), writes=["lnb2"])
        attn_core(specs, fin)

    def mixer_D(l):
        set_v_ones()
        rtmp = sb("mixscr", [128, 512], F32)
        mx8 = sb("mx8", [128, 8], F32)
        thr = small[:, 128:144]
        iw = sb("iw", [128, NT, 8], F32)
        iwa = sb("iwa", [128, NT, 8], F32)
        iws = sb("iws", [128, NT, 8], F32)
        iqm = [sb(f"iqm{k_}", [128, 128], BF16) for k_ in range(8)]
        for ch in range(2):
            wt, wreg = load_w(l, 2308 + ch * 128, 128)
            for tg in range(4):
                pb, preg = projT(wt, wreg, 128, tg)
                qk_norm_store(pb, preg, 128, bones64, 1.0 / 64, colp[:, CP_QND:CP_QND + 1], 1.0,
                              bufQ[:, ch, tg * 512:(tg + 1) * 512], "bufQ")
        wt, wreg = load_w(l, 2564, 64)
        ik2 = wst_i[0] % 3
        wst_i[0] += 1
        wk2 = wst[ik2]
        for r in range(2):
            op("dve", lambda e, r=r, wt=wt: e.tensor_copy(out=wk2[:, :, r * 64:(r + 1) * 64], in_=wt), reads=[wreg], writes=[("wst", ik2)])
        for tg in range(4):
            pb, preg = projT(wk2, ("wst", ik2), 128, tg)
            qk_norm_store(pb, preg, 128, bones64, 1.0 / 64, colp[:, CP_KND:CP_KND + 1], 0.0,
                          bufK[:, 0, tg * 512:(tg + 1) * 512], "bufK")
        if stopD <= 1:
            return
        for ch in range(2):
            wt, wreg = load_w(l, 2692 + ch * 128, 128)
            for tg in range(4):
                pb, preg = projT(wt, wreg, 128, tg)
                op("dve", lambda e, pb=pb, ch=ch, tg=tg: e.tensor_scalar(out=bufIQ[:, ch, tg * 512:(tg + 1) * 512], in0=pb,
                                                                         scalar1=32 ** -0.5, scalar2=None, op0=ALU.mult),
                   reads=[preg], writes=["bufIQ"])
        wt, wreg = load_w(l, 2948, 32)
        i4 = wst_i[0] % 3
        wst_i[0] += 1
        w4 = wst[i4]
        for r in range(4):
            op("dve", lambda e, r=r, wt=wt: e.tensor_copy(out=w4[:, :, r * 32:(r + 1) * 32], in_=wt), reads=[wreg], writes=[("wst", i4)])
        for tg in range(4):
            pb, preg = projT(w4, ("wst", i4), 128, tg)
            op("act", lambda e, pb=pb, tg=tg: e.activation(out=bufIK[:, tg * 512:(tg + 1) * 512], in_=pb, func=AF.Copy),
               reads=[preg], writes=["bufIK"])
        wt, wreg = load_w(l, 2980, 8)
        for t in range(NT):
            b = pj_i[0] % 2
            pj_i[0] += 1
            for c in range(8):
                mm(ps[b][:, 0:8], hT[:, c, t * 128:(t + 1) * 128], wt[:, c, :], c == 0, c == 7,
                   reads=[wreg, ("hT", t // 4)], writes=[PS[b]])
            op("dve", lambda e, t=t, b=b: e.tensor_scalar(out=iw[:, t, :], in0=ps[b][:, 0:8], scalar1=8 ** -0.5, scalar2=None, op0=ALU.mult),
               reads=[PS[b]], writes=["iw"])
        op("act", lambda e: e.activation(out=iwa, in_=iw, func=AF.Abs), reads=["iw"], writes=["iwa"])
        op("act", lambda e: e.activation(out=iws, in_=iw, func=AF.Sign), reads=["iw"], writes=["iws"])
        proj_v(l, 2628, 1, Vaug, "V")

        if stopD <= 2:
            return
        QG = 256
        scoreb = [carve(A_MIXT, [128, S], F32), carve(A_MIXT + 8192, [128, S], F32)]
        mbvb = [carve(A_MIXT + 16384, [128, 2, S], BF16), carve(A_MIXT + 24576, [128, 2, S], BF16)]
        rt = [rtmp, carve(A_BUFK + 4096, [128, 512], F32)]
        posb = carve(A_W, [128, 2, 132], F32)
        den = small[:, 144:148]
        rt_i = [0]

        def index_topk(qg):
            for i in range(2):
                t = qg * 2 + i
                nk = (t + 1) * 128
                score, sreg = scoreb[t % 2], ("score", t % 2)
                mbv = mbvb[qg % 2]
                for hh in range(8):
                    g, ch = hh % 4, hh // 4
                    op("pool", lambda e, hh=hh, g=g, ch=ch, t=t: e.tensor_scalar(
                        out=iqm[hh], in0=bufIQ[:, ch, t * 128:(t + 1) * 128], scalar1=cf[:, CF_GMASK + g:CF_GMASK + g + 1],
                        scalar2=None, op0=ALU.mult), reads=["bufIQ", "cf"], writes=[("iqm", hh)])
                for k0 in range(0, nk, 512):
                    kn = min(512, nk - k0)
                    for hh in range(8):
                        b = pj_i[0] % 2
                        pj_i[0] += 1
                        r = rt_i[0] % 2
                        rt_i[0] += 1
                        mm(ps[b][:, 0:kn], iqm[hh], bufIK[:, k0:k0 + kn], True, True,
                           reads=[("iqm", hh), "bufIK"], writes=[PS[b]])
                        op("act", lambda e, b=b, kn=kn, t=t, hh=hh, r=r: e.activation(out=rt[r][:, 0:kn], in_=ps[b][:, 0:kn], func=AF.Relu,
                                                                                      scale=iwa[:, t, hh:hh + 1]),
                           reads=[PS[b], "iwa"], writes=[("rtmp", r)])
                        if hh == 0:
                            op("act", lambda e, k0=k0, kn=kn, t=t, r=r, score=score: e.activation(
                                out=score[:, k0:k0 + kn], in_=rt[r][:, 0:kn], func=AF.Copy, scale=iws[:, t, 0:1]),
                               reads=[("rtmp", r), "iws"], writes=[sreg])
                        else:
                            op("act", lambda e, kn=kn, t=t, hh=hh, r=r: e.activation(
                                out=rt[r][:, 0:kn], in_=rt[r][:, 0:kn], func=AF.Copy, scale=iws[:, t, hh:hh + 1]),
                               reads=[("rtmp", r), "iws"], writes=[("rtmp", r)])
                            op("pool", lambda e, k0=k0, kn=kn, r=r, score=score: e.tensor_tensor(
                                out=score[:, k0:k0 + kn], in0=score[:, k0:k0 + kn], in1=rt[r][:, 0:kn], op=ALU.add),
                               reads=[("rtmp", r), sreg], writes=[sreg])
                op("pool", lambda e, t=t, score=score: e.tensor_tensor(out=score[:, t * 128:(t + 1) * 128], in0=score[:, t * 128:(t + 1) * 128],
                                                                       in1=cf[:, CF_NEGTRI:CF_NEGTRI + 128], op=ALU.add),
                   reads=[sreg, "cf"], writes=[sreg])
                if t < 2:
                    op("dve", lambda e, i=i, nk=nk, score=score, mbv=mbv: e.tensor_scalar(
                        out=mbv[:, i, 0:nk], in0=score[:, 0:nk], scalar1=-1e29, scalar2=NEG, op0=ALU.is_lt, op1=ALU.mult),
                       reads=[sreg], writes=[("mbv", qg % 2, i)])
                else:
                    for r_ in range(32):
                        op("dve", lambda e, score=score, nk=nk: e.max(out=mx8, in_=score[:, 0:nk]), reads=[sreg], writes=["mx8"])
                        op("dve", lambda e, score=score, nk=nk: e.match_replace(out=score[:, 0:nk], in_to_replace=mx8,
                                                                                in_values=score[:, 0:nk], imm_value=-3e38),
                           reads=[sreg, "mx8"], writes=[sreg])
                    op("dve", lambda e, i=i, nk=nk, score=score, mbv=mbv: e.tensor_scalar(
                        out=mbv[:, i, 0:nk], in0=score[:, 0:nk], scalar1=-1e35, scalar2=NEG, op0=ALU.is_ge, op1=ALU.mult),
                       reads=[sreg], writes=[("mbv", qg % 2, i)])

        def attention(qg):
            nkb = 2 * (qg + 1)
            mbv = mbvb[qg % 2]
            items = []
            for h in range(4):
                hp, ch = (h % 2) * 64, h // 2
                pb_ = 2 + (po_i[0] % 2)
                po_i[0] += 1
                po, poreg = ps[pb_], PS[pb_]
                first_po = [True]
                for kb in range(nkb):
                    st = {}

                    def s1(h=h, hp=hp, ch=ch, kb=kb, st=st):
                        j = kb - 2 * qg
                        c0 = 0 if j < 0 else j * 128
                        sbk = pj_i[0] % 2
                        pj_i[0] += 1
                        pss, psreg = ps[sbk], PS[sbk]
                        mm(pss[:, c0:QG], bufK[hp:hp + 64, 0, kb * 128:(kb + 1) * 128], bufQ[hp:hp + 64, ch, qg * QG + c0:(qg + 1) * QG],
                           True, False, reads=["bufQ", "bufK"], writes=[psreg])
                        mm(pss[:, c0:QG], cb[0:2, CB_NSLOPE + (4 + h) * 128:CB_NSLOPE + (5 + h) * 128],
                           cb[0:2, CB_POS + c0:CB_POS + QG], False, False, reads=["cb"], writes=[psreg])
                        for i in range(c0 // 128, 2):
                            mm(pss[:, i * 128:(i + 1) * 128], mbv[:, i, kb * 128:(kb + 1) * 128], ident_b, False, i == 1,
                               reads=[("mbv", qg % 2, i), "cb"], writes=[psreg])
                        st.update(sbk=sbk, c0=c0)

                    def s2(h=h, kb=kb, st=st, po=po, poreg=poreg, first_po=first_po, pi=pb_ - 2, last=(kb == nkb - 1)):
                        sbk, c0 = st["sbk"], st["c0"]
                        pss, psreg = ps[sbk], PS[sbk]
                        pt = pT[sbk]
                        dlt = kb - 2 * qg
                        bias_ap = cf[:, CF_ALIBI + (4 + h) * AL_N + dlt + AL_O:CF_ALIBI + (4 + h) * AL_N + dlt + AL_O + 1]
                        op("act", lambda e, pt=pt, pss=pss, c0=c0, bias_ap=bias_ap: e.activation(
                            out=pt[:, c0:QG], in_=pss[:, c0:QG], func=AF.Exp, bias=bias_ap),
                           reads=[psreg, "cf"], writes=[("pT", sbk)])
                        for i in range(c0 // 128, 2):
                            mm(po[:, i * 65:(i + 1) * 65], pt[:, i * 128:(i + 1) * 128], Vaug[:, kb, 0, :], first_po[0], False,
                               reads=[("pT", sbk), "V"], writes=[poreg])
                            first_po[0] = False
                        if not last:
                            return
                        dn = den[:, pi * 2:pi * 2 + 2]
                        dsrc = po[:, 0:130].rearrange("p (i c) -> p i c", c=65)[:, :, 64]
                        op("act", lambda e, dn=dn, dsrc=dsrc: e.activation(out=dn, in_=dsrc, func=AF.Ln, bias=1e-30),
                           reads=[poreg], writes=[("den", pi)])
                        op("act", lambda e, dn=dn: e.activation(out=dn, in_=dn, func=AF.Exp, scale=-1.0),
                           reads=[("den", pi)], writes=[("den", pi)])
                        for i in range(2):
                            t = qg * 2 + i
                            op("act", lambda e, i=i, t=t, h=h, po=po, dn=dn: e.activation(
                                out=mix_tok[:, t, h * 64:(h + 1) * 64], in_=po[:, i * 65:i * 65 + 64], func=AF.Copy, scale=dn[:, i:i + 1]),
                               reads=[poreg, ("den", pi)], writes=[("mix_tok", t)])

                    items.append((s1, s2))
            run_pipelined(items)

        nqg = S // QG
        for qg in range(nqg):
            index_topk(qg)
            if stopD > 3 and qg >= 1:
                attention(qg - 1)
        if stopD > 3:
            attention(nqg - 1)

    def phase_moe(l):
        NE = 2
        M_W = 0
        wb = [[dict(w1=carve(M_W + (s_ * NE + j) * 12288, [128, 8, 256], BF16),
                    w3=carve(M_W + (s_ * NE + j) * 12288 + 4096, [128, 8, 256], BF16),
                    w2f=carve(M_W + (s_ * NE + j) * 12288 + 8192, [128, 2, D], BF16)) for j in range(NE)] for s_ in range(2)]
        M_O = 2 * NE * 12288
        w2stage = [carve(M_O, [128, 2, D], F32)]
        hid = carve(M_O + 8192, [128, NE * 2, 512], BF16)
        t1 = [carve(M_O + 12288, [128, 512], F32), carve(M_O + 14336, [128, 512], F32)]
        t2 = [carve(M_O + 16384, [128, 512], F32), carve(M_O + 18432, [128, 512], F32)]
        chi_ = carve(M_O + 20480, [32, S], BF16)
        clo_ = carve(M_O + 24576, [32, S], BF16)
        comb = carve(M_O + 28672, [128, NT, 32], F32)
        lg = carve(M_O + 30720, [128, 64], F32)[:, 0:36]
        wr = carve(M_O + 30976, [128, 8, 36], BF16)
        rs = small[:, 160:200]
        ohb = [sb(f"ohb_{i_}", [32, 128], BF16) for i_ in range(2)]
        dma("sp", "gate", lambda e: e.dma_start(out=gate, in_=modb_d[l][:, 5 * D:6 * D]), reads=[("modb", l, 5)], writes=["gate"])
        dma("pool", "wr", lambda e: e.dma_start(out=wr, in_=w_r_d[l].rearrange("(c p) n -> p c n", p=128)), writes=["wr"])
        brow = rowp[:, RP_BR - 2048:RP_BR - 2048 + 36]
        for t in range(NT):
            b = pj_i[0] % 2
            pj_i[0] += 1
            for c in range(8):
                mm(ps[b][:, 0:36], hT[:, c, t * 128:(t + 1) * 128], wr[:, c, :], c == 0, c == 7,
                   reads=["wr", ("hT", t // 4)], writes=[PS[b]])
            op("dve", lambda e, b=b: e.tensor_tensor(out=lg, in0=ps[b][:, 0:36], in1=brow, op=ALU.add),
               reads=[PS[b], "rowp"], writes=["lg"])
            R_ = ["lg", "rs"]
            gmax, gsum, v1, v2, w1c, w2c = (rs[:, k:k + 1] for k in range(6))
            goh = rs[:, 8:12]
            gex = rs[:, 12:16]
            op("dve", lambda e: e.reduce_max(out=gmax, in_=lg[:, 0:4], axis=mybir.AxisListType.X), reads=R_, writes=["rs"])
            op("dve", lambda e: e.tensor_scalar(out=goh, in0=lg[:, 0:4], scalar1=gmax, scalar2=None, op0=ALU.is_ge), reads=R_, writes=["rs"])
            op("dve", lambda e: e.tensor_scalar(out=gex, in0=lg[:, 0:4], scalar1=gmax, scalar2=None, op0=ALU.subtract), reads=R_, writes=["rs"])
            op("act", lambda e: e.activation(out=gex, in_=gex, func=AF.Exp, accum_out=gsum), reads=R_, writes=["rs"])
            op("dve", lambda e: e.reciprocal(out=gsum, in_=gsum), reads=R_, writes=["rs"])
            op("dve", lambda e: e.tensor_scalar(out=gex, in0=goh, scalar1=-1.0, scalar2=1e9, op0=ALU.add, op1=ALU.mult), reads=R_, writes=["rs"])
            for g in range(4):
                op("dve", lambda e, g=g: e.tensor_scalar(out=lg[:, 4 + g * 8:12 + g * 8], in0=lg[:, 4 + g * 8:12 + g * 8],
                                                         scalar1=gex[:, g:g + 1], scalar2=None, op0=ALU.add), reads=R_, writes=["lg"])
            el = lg[:, 4:36]
            eq1 = lnv[:, 0:32]
            eq2 = lnv[:, 32:64]
            el2 = lnv[:, 64:96]
            R2 = ["lg", "rs", "lnv"]
            op("dve", lambda e: e.reduce_max(out=v1, in_=el, axis=mybir.AxisListType.X), reads=R2, writes=["rs"])
            op("dve", lambda e: e.tensor_scalar(out=eq1, in0=el, scalar1=v1, scalar2=None, op0=ALU.is_ge), reads=R2, writes=["lnv"])
            op("dve", lambda e: e.scalar_tensor_tensor(out=el2, in0=eq1, scalar=-1e9, in1=el, op0=ALU.mult, op1=ALU.add), reads=R2, writes=["lnv"])
            op("dve", lambda e: e.reduce_max(out=v2, in_=el2, axis=mybir.AxisListType.X), reads=R2, writes=["rs"])
            op("dve", lambda e: e.tensor_scalar(out=eq2, in0=el2, scalar1=v2, scalar2=None, op0=ALU.is_ge), reads=R2, writes=["lnv"])
            op("dve", lambda e: e.tensor_tensor(out=w2c, in0=v1, in1=v2, op=ALU.subtract), reads=R2, writes=["rs"])
            op("act", lambda e: e.activation(out=w2c, in_=w2c, func=AF.Exp), reads=R2, writes=["rs"])
            op("dve", lambda e: e.tensor_scalar(out=w2c, in0=w2c, scalar1=1.0, scalar2=None, op0=ALU.add), reads=R2, writes=["rs"])
            op("dve", lambda e: e.reciprocal(out=w2c, in_=w2c), reads=R2, writes=["rs"])
            op("dve", lambda e: e.tensor_scalar(out=w1c, in0=w2c, scalar1=-1.0, scalar2=1.0, op0=ALU.mult, op1=ALU.add), reads=R2, writes=["rs"])
            op("dve", lambda e: e.tensor_tensor(out=w1c, in0=w1c, in1=gsum, op=ALU.mult), reads=R2, writes=["rs"])
            op("dve", lambda e: e.tensor_tensor(out=w2c, in0=w2c, in1=gsum, op=ALU.mult), reads=R2, writes=["rs"])
            op("dve", lambda e: e.tensor_scalar(out=eq1, in0=eq1, scalar1=w1c, scalar2=None, op0=ALU.mult), reads=R2, writes=["lnv"])
            op("dve", lambda e, t=t: e.scalar_tensor_tensor(out=comb[:, t, :], in0=eq2, scalar=w2c, in1=eq1, op0=ALU.mult, op1=ALU.add),
               reads=R2, writes=["comb"])
        chl = carve(M_O + 12288, [128, NT, 64], BF16)
        ctmp = carve(M_O + 16384, [128, NT, 32], F32)
        op("dve", lambda e: e.tensor_copy(out=chl[:, :, 0:32], in_=comb), reads=["comb"], writes=["chl"])
        op("dve", lambda e: e.tensor_tensor(out=ctmp, in0=comb, in1=chl[:, :, 0:32], op=ALU.subtract), reads=["comb", "chl"], writes=["ctmp"])
        op("dve", lambda e: e.tensor_copy(out=chl[:, :, 32:64], in_=ctmp), reads=["ctmp"], writes=["chl"])
        pstb = ps[5].bitcast(BF16)
        for t in range(NT):
            op("pe", lambda e, t=t: e.transpose(pstb[0:64, (t % 8) * 128:(t % 8 + 1) * 128], chl[:, t, :], ident_b), reads=["chl", "cb"], writes=[PS[5]])
            if t % 8 == 7:
                t0_ = (t // 8) * 1024
                op("dve", lambda e, t0_=t0_: e.tensor_copy(out=chi_[:, t0_:t0_ + 1024], in_=pstb[0:32, :]), reads=[PS[5]], writes=["chi_"])
                op("dve", lambda e, t0_=t0_: e.tensor_copy(out=clo_[:, t0_:t0_ + 1024], in_=pstb[32:64, :]), reads=[PS[5]], writes=["clo_"])
        em.barrier()

        nsets = 32 // NE

        def load_set(s_):
            sl = s_ % 2
            for j in range(NE):
                e_ = s_ * NE + j
                w = wb[sl][j]
                dma("pool", f"mw{sl}{j}a", lambda e, w=w, e_=e_: e.dma_start(out=w["w1"], in_=w1_d[l][e_].rearrange("(c p) n -> p c n", p=128)),
                    writes=[("w13", sl, j)])
                dma("pool", f"mw{sl}{j}b", lambda e, w=w, e_=e_: e.dma_start(out=w["w3"], in_=w3_d[l][e_].rearrange("(c p) n -> p c n", p=128)),
                    writes=[("w13", sl, j)])
                st = w2stage[0]
                dma("sp", "mw2s", lambda e, st=st, e_=e_: e.dma_start(out=st, in_=w2_d[l][e_].rearrange("(c p) n -> p c n", p=128)),
                    writes=[("w2st", 0)])
                for c in range(2):
                    op("pool", lambda e, w=w, st=st, c=c: e.tensor_tensor(out=w["w2f"][:, c, :], in0=st[:, c, :], in1=gate, op=ALU.mult),
                       reads=[("w2st", 0), "gate"], writes=[("w2", sl, j)])

        hidb = [hid, carve(M_O + 28672, [128, NE * 2, 512], BF16)]
        steps = [(s_, tg) for s_ in range(nsets) for tg in range(4)]

        def stepA(i):
            s_, tg = steps[i]
            sl = s_ % 2
            hb = hidb[i % 2]
            for j in range(NE):
                e_ = s_ * NE + j
                w = wb[sl][j]
                n_ = i * NE + j
                cbk = 4 + n_ % 2
                oh = ohb[n_ % 2]
                op("dve", lambda e, oh=oh, e_=e_: e.tensor_scalar(out=oh, in0=ones_b[0:32, :], scalar1=ident_f[0:32, e_:e_ + 1],
                                                                  scalar2=None, op0=ALU.mult),
                   reads=["cb", "cf"], writes=[("ohb", n_ % 2)])
                mm(ps[cbk], oh, chi_[:, tg * 512:(tg + 1) * 512], True, False, reads=[("ohb", n_ % 2), "chi_"], writes=[PS[cbk]])
                mm(ps[cbk], oh, clo_[:, tg * 512:(tg + 1) * 512], False, True, reads=[("ohb", n_ % 2), "clo_"], writes=[PS[cbk]])
                for fc in range(2):
                    k = pj_i[0] % 2
                    pj_i[0] += 1
                    p1, p3 = ps[k], ps[2 + k]
                    for c in range(8):
                        mm(p1, w["w1"][:, c, fc * 128:(fc + 1) * 128], hT[:, c, tg * 512:(tg + 1) * 512], c == 0, c == 7,
                           reads=[("w13", sl, j), ("hT", tg)], writes=[PS[k]], inc=(c == 7))
                    for c in range(8):
                        mm(p3, w["w3"][:, c, fc * 128:(fc + 1) * 128], hT[:, c, tg * 512:(tg + 1) * 512], c == 0, c == 7,
                           reads=[("w13", sl, j), ("hT", tg)], writes=[PS[2 + k]], inc=(c == 7))
                    op("act", lambda e, k=k, p1=p1: e.activation(out=t1[k], in_=p1, func=AF.Silu), reads=[PS[k]], writes=[("t1", k)])
                    op("dve", lambda e, k=k, p3=p3: e.tensor_tensor(out=t2[k], in0=t1[k], in1=p3, op=ALU.mult),
                       reads=[("t1", k), PS[2 + k]], writes=[("t2", k)])
                    op("dve", lambda e, k=k, j=j, fc=fc, hb=hb, cbk=cbk: e.tensor_tensor(out=hb[:, j * 2 + fc, :], in0=t2[k], in1=ps[cbk], op=ALU.mult),
                       reads=[("t2", k), PS[cbk]], writes=[("hid", i % 2, j * 2 + fc)])

        def stepB(i):
            s_, tg = steps[i]
            sl = s_ % 2
            hb = hidb[i % 2]
            for ti in range(4):
                t = tg * 4 + ti
                for hf in range(2):
                    b = 6 + (ti * 2 + hf) % 2
                    n = 0
                    for j in range(NE):
                        for fc in range(2):
                            mm(ps[b], hb[:, j * 2 + fc, ti * 128:(ti + 1) * 128], wb[sl][j]["w2f"][:, fc, hf * 512:(hf + 1) * 512],
                               n == 0, n == NE * 2 - 1, reads=[("hid", i % 2, j * 2 + fc), ("w2", sl, j)], writes=[PS[b]],
                               inc=(n == NE * 2 - 1))
                            n += 1
                    op("dve", lambda e, t=t, hf=hf, b=b: e.tensor_tensor(out=x[:, t, hf * 512:(hf + 1) * 512],
                                                                         in0=x[:, t, hf * 512:(hf + 1) * 512], in1=ps[b], op=ALU.add),
                       reads=[PS[b], ("x", t)], writes=[("x", t)])

        load_set(0)
        load_set(1)
        stepA(0)
        for i, (s_, tg) in enumerate(steps):
            if tg == 0 and s_ >= 1 and s_ + 1 < nsets and "moe_nodma" not in cdbg:
                load_set(s_ + 1)
            if i + 1 < len(steps):
                stepA(i + 1)
            stepB(i)

    set_lnb()

    for l in range(nl):
        phase_mod(l)
        em.barrier()
        phase_norm(l, 0)
        dma("sp", "gate", lambda e, l=l: e.dma_start(out=gate, in_=modb_d[l][:, 2 * D:3 * D]), reads=[("modb", l, 2)], writes=["gate"])
        em.barrier()
        for m, name in enumerate("ABCD"):
            if name in mixers:
                {"A": mixer_A, "B": mixer_B, "C": mixer_C, "D": mixer_D}[name](l)
                em.barrier()
                mixer_out(l, m)
                em.barrier()
        phase_norm(l, 1)
        em.barrier()
        if do_moe:
            phase_moe(l)
            em.barrier()
    for t in range(NT):
        dma("sp", f"xo{t % 4}", lambda e, t=t: e.dma_start(out=out_d[t * 128:(t + 1) * 128, :], in_=x[:, t, :]),
            reads=[("x", t)], writes=[("out", t)])
    em.wait_all("sp", [("out", t) for t in range(NT)])
    em.finish()
    return nc


def prep_inputs(inp, n_cores=8):
    f = lambda a: np.ascontiguousarray(np.asarray(a, dtype=np.float32))
    cbv, cfv = make_consts()
    rowp = np.zeros((L, 128, NRP), np.float32)
    colp = np.zeros((L, 128, NCP), np.float32)
    for l in range(L):
        row = np.concatenate([f(inp["norm1_g"])[l], f(inp["norm2_g"])[l], f(inp["subln_g"])[l],
                              f(inp["b_group"])[l], f(inp["b_expert"])[l],
                              f(inp["lam_q1"])[l], f(inp["lam_k1"])[l], f(inp["lam_q2"])[l], f(inp["lam_k2"])[l]])
        rowp[l] = np.broadcast_to(row[None, :], (128, NRP))
        colp[l, :, CP_QNA] = np.tile(f(inp["qn_a"])[l], 2)
        colp[l, :, CP_KNA] = np.tile(f(inp["kn_a"])[l], 2)
        qc = np.tile(f(inp["qn_c"])[l], 4)
        m0 = (np.arange(128) % 64) // 32 == 0
        colp[l, m0, CP_QNC] = qc[m0]
        colp[l, ~m0, CP_QNC1] = qc[~m0]
        colp[l, :, CP_KNC] = np.tile(f(inp["kn_c"])[l], 4)
        colp[l, :, CP_QND] = np.tile(f(inp["qn_d"])[l], 2)
        colp[l, :, CP_KND] = np.tile(f(inp["kn_d"])[l], 2)
        colp[l, :, CP_BF] = np.tile(f(inp["b_f"])[l], 32)
        colp[l, :, CP_BETA:CP_BETA + 8] = f(inp["mix_beta"])[l].reshape(8, 128).T
    shared = dict(
        ada_w=f(inp["ada_w"]),
        ada_b=np.ascontiguousarray(np.broadcast_to(f(inp["ada_b"])[:, None, :], (L, 128, 6 * D))),
        w_in=f(inp["w_in"]), w_out=f(inp["w_out"]),
        w_r=np.ascontiguousarray(np.concatenate([f(inp["w_group"]), f(inp["w_expert"])], axis=-1)),
        w1=f(inp["w1"]).reshape(L, 32, D, 256), w3=f(inp["w3"]).reshape(L, 32, D, 256),
        w2=f(inp["w2"]).reshape(L, 32, 256, D),
        rowp=rowp, colp=colp, cb=cbv, cf=cfv, alc=make_alc(), onesr=np.ones((2, S), ml_dtypes.bfloat16))
    xs, cs = f(inp["x"]), f(inp["c"])
    maps = []
    for b in range(n_cores):
        m = dict(shared)
        m["x"] = xs[b]
        m["cT"] = np.ascontiguousarray(cs[b].reshape(8, 128).T)
        maps.append(m)
    return maps


_NC_CACHE = {}


def kernel(**inputs):
    if "nc" not in _NC_CACHE:
        nc = bass.Bass("TRN2", target_bir_lowering=False)
        _NC_CACHE["nc"] = build(nc, {})
    nc = _NC_CACHE["nc"]
    maps = prep_inputs(inputs, 8)
    res = run_bass_kernel_spmd(nc, maps, core_ids=list(range(8)))
    return np.stack([np.asarray(r["out"], dtype=np.float32) for r in res.results], axis=0)
```

```python
import math
import numpy as np
import ml_dtypes
import concourse.bass as bass
import concourse.mybir as mybir
from concourse.bass_utils import run_bass_kernel_spmd

F32 = mybir.dt.float32
BF16 = mybir.dt.bfloat16
ALU = mybir.AluOpType
AF = mybir.ActivationFunctionType

D = 1024
S = 2048
NT = 16
L = 2
P_IN = 2988
EPS = 1e-6
NEG = -30000.0
SEM_LIMIT = 20000


class Ticket:
    __slots__ = ("sem", "val", "eng")

    def __init__(self, sem, val, eng):
        self.sem, self.val, self.eng = sem, val, eng


class Region:
    __slots__ = ("w", "r")

    def __init__(self):
        self.w = None
        self.r = []


class Emitter:
    ENGS = ("pe", "act", "dve", "pool", "sp")

    def __init__(self, nc):
        self.nc = nc
        self.prog = {e: [] for e in self.ENGS}
        self.cur_sem = {}
        self.cnt = {}
        self.nsem = 0
        for e in self.ENGS:
            self._new_eng_sem(e)
        self.waited = {}
        self.chan = {}
        self.regions = {}
        self.pending_dma = []

    def _alloc_sem(self, name):
        self.nsem += 1
        return self.nc.alloc_semaphore(name=f"{name}_{self.nsem}")

    def _new_eng_sem(self, e):
        self.cur_sem[e] = self._alloc_sem("e" + e)
        self.cnt[e] = 0

    def R(self, key):
        r = self.regions.get(key)
        if r is None:
            r = self.regions[key] = Region()
        return r

    def _regs(self, lst):
        return [x if isinstance(x, Region) else self.R(x) for x in (lst or [])]

    def _waits_for(self, eng, deps):
        best = {}
        for t in deps:
            if t.eng == "pe" and eng == "pe":
                continue
            k = id(t.sem)
            if k not in best or best[k].val < t.val:
                best[k] = t
        waits = []
        for k, t in best.items():
            if t.eng != "dma":
                wk = (eng, k)
                if self.waited.get(wk, 0) >= t.val:
                    continue
                self.waited[wk] = t.val
            waits.append(t)
        return waits

    def _collect(self, eng, reads, writes):
        deps = []
        for r in reads:
            if r.w is not None:
                deps.append(r.w)
        for w in writes:
            if w.w is not None:
                deps.append(w.w)
            deps.extend(w.r)
        return self._waits_for(eng, deps)

    def _update(self, ticket, reads, writes):
        for w in writes:
            w.w = ticket
            w.r = []
        for r in reads:
            if r not in writes:
                r.r.append(ticket)

    def op(self, eng, fn, reads=None, writes=None, inc=True):
        reads, writes = self._regs(reads), self._regs(writes)
        waits = self._collect(eng, reads, writes)
        if self.cnt[eng] >= SEM_LIMIT:
            self._new_eng_sem(eng)
        sem = self.cur_sem[eng]
        if inc:
            self.cnt[eng] += 1
            ticket = Ticket(sem, self.cnt[eng], eng)
        else:
            assert eng == "pe"
            ticket = Ticket(sem, self.cnt[eng] + 1, eng)

        def emit(e, waits=waits, fn=fn, sem=sem, inc=inc):
            for t in waits:
                e.wait_ge(t.sem, t.val)
            if inc:
                fn(e).then_inc(sem, 1)
            else:
                fn(e)

        self.prog[eng].append(emit)
        self._update(ticket, reads, writes)
        return ticket

    def dma(self, queue, chan, fn, reads=None, writes=None):
        reads, writes = self._regs(reads), self._regs(writes)
        waits = self._collect(queue, reads, writes)
        c = self.chan.get(chan)
        if c is None:
            c = self.chan[chan] = [self._alloc_sem("d"), 0]
        c[1] += 16
        ticket = Ticket(c[0], c[1], "dma")
        sem = c[0]

        def emit(e, waits=waits, fn=fn, sem=sem):
            for t in waits:
                e.wait_ge(t.sem, t.val)
            fn(e).then_inc(sem, 16)

        self.prog[queue].append(emit)
        self._update(ticket, reads, writes)
        self.pending_dma.append(ticket)
        return ticket

    def barrier(self):
        deps = [Ticket(self.cur_sem[e], self.cnt[e], e) for e in self.ENGS if self.cnt[e] > 0]
        last = {}
        for t in self.pending_dma:
            last[id(t.sem)] = t if (id(t.sem) not in last or last[id(t.sem)].val < t.val) else last[id(t.sem)]
        dmas = list(last.values())
        self.pending_dma = dmas
        for eng in self.ENGS:
            ws = [t for t in deps if t.eng != eng] + dmas

            def emit(e, ws=ws):
                for t in ws:
                    e.wait_ge(t.sem, t.val)

            self.prog[eng].append(emit)
            for t in deps:
                if t.eng != eng:
                    self.waited[(eng, id(t.sem))] = max(self.waited.get((eng, id(t.sem)), 0), t.val)

    def wait_all(self, eng, regions):
        regs = self._regs(regions)
        waits = self._collect(eng, regs, regs)

        def emit(e, waits=waits):
            for t in waits:
                e.wait_ge(t.sem, t.val)

        self.prog[eng].append(emit)

    def finish(self):
        nc = self.nc
        with nc.Block() as block:
            @block.tensor
            def _(e):
                for f in self.prog["pe"]:
                    f(e)

            @block.scalar
            def _(e):
                for f in self.prog["act"]:
                    f(e)

            @block.vector
            def _(e):
                for f in self.prog["dve"]:
                    f(e)

            @block.gpsimd
            def _(e):
                for f in self.prog["pool"]:
                    f(e)

            @block.sync
            def _(e):
                for f in self.prog["sp"]:
                    f(e)


CB_IDENT, CB_TRIADD, CB_TRIMUL, CB_B64, CB_B32, CB_ONES, CB_NSLOPE, CB_POS = (
    0, 128, 256, 384, 512, 640, 768, 1792)
NCB = 2304
CF_IDENT, CF_ALIBI, CF_NEGTRI, CF_GMASK = 0, 128, 288, 416
NCF = 420
AL_N, AL_O = 20, 16
SLOPES = [2.0 ** (-8.0 * i / 8) for i in range(1, 9)]
SLOPES_CD = SLOPES[0::2] + SLOPES[1::2]


def make_consts():
    cb = np.zeros((128, NCB), np.float32)
    p = np.arange(128)
    cb[:, CB_IDENT:CB_IDENT + 128] = np.eye(128)
    cb[:, CB_TRIADD:CB_TRIADD + 128] = np.where(p[:, None] <= p[None, :], 0.0, NEG)
    cb[:, CB_TRIMUL:CB_TRIMUL + 128] = (p[:, None] < p[None, :]).astype(np.float32)
    cb[:, CB_B64:CB_B64 + 128] = (p[:, None] // 64 == p[None, :] // 64)
    cb[:, CB_B32:CB_B32 + 128] = (p[:, None] // 32 == p[None, :] // 32)
    cb[:, CB_ONES:CB_ONES + 128] = 1.0
    for h in range(8):
        cb[0:2, CB_NSLOPE + h * 128:CB_NSLOPE + (h + 1) * 128] = -SLOPES_CD[h]
    t = np.arange(512)
    cb[0, CB_POS:CB_POS + 512] = 128 * (t // 128)
    cb[1, CB_POS:CB_POS + 512] = t % 128
    cf = np.zeros((128, NCF), np.float32)
    cf[:, CF_IDENT:CF_IDENT + 128] = np.eye(128)
    for h in range(8):
        for d in range(-16, 4):
            cf[:, CF_ALIBI + h * AL_N + d + AL_O] = SLOPES_CD[h] * (p + 128 * d)
    cf[:, CF_NEGTRI:CF_NEGTRI + 128] = np.where(p[None, :] <= p[:, None], 0.0, -1e30)
    for g in range(4):
        cf[:, CF_GMASK + g] = (p // 32 == g)
    return cb.astype(ml_dtypes.bfloat16), cf


def make_alc():
    t = np.arange(S)
    a = np.zeros((4, 2, S), np.float32)
    for h in range(4):
        a[h, 0] = -SLOPES_CD[h] * 128 * ((t % 512) // 128)
        a[h, 1] = -SLOPES_CD[h] * (t % 128)
    return a.astype(ml_dtypes.bfloat16)


RP_N1, RP_N2, RP_SUBLN, RP_BR, RP_LAM = 0, 1024, 2048, 2112, 2148
NRP = 2276
CP_QNA, CP_KNA, CP_QNC, CP_KNC, CP_QND, CP_KND, CP_BF, CP_BETA, CP_QNC1 = 0, 1, 2, 3, 4, 5, 6, 7, 15
NCP = 16


def build(nc, cfg):
    nl = cfg.get("n_layers", L)
    mixers = cfg.get("mixers", "ABCD")
    do_moe = cfg.get("moe", True)
    stopC = cfg.get("stopC", 99)
    stopD = cfg.get("stopD", 99)
    cdbg = cfg.get("cdbg", "")
    em = Emitter(nc)
    op, dma = em.op, em.dma

    def dram(name, shape, dt=F32, kind="ExternalInput"):
        return nc.dram_tensor(name, list(shape), dt, kind=kind).ap()

    x_d = dram("x", [S, D])
    out_d = dram("out", [S, D], kind="ExternalOutput")
    cT_d = dram("cT", [128, 8])
    ada_w_d = dram("ada_w", [L, D, 6 * D])
    ada_b_d = dram("ada_b", [L, 128, 6 * D])
    w_in_d = dram("w_in", [L, D, P_IN])
    w_out_d = dram("w_out", [L, D, D])
    w_r_d = dram("w_r", [L, D, 36])
    w1_d = dram("w1", [L, 32, D, 256])
    w3_d = dram("w3", [L, 32, D, 256])
    w2_d = dram("w2", [L, 32, 256, D])
    rowp_d = dram("rowp", [L, 128, NRP])
    colp_d = dram("colp", [L, 128, NCP])
    cb_d = dram("cb", [128, NCB], BF16)
    cf_d = dram("cf", [128, NCF])
    alc_d = dram("alc", [4, 2, S], BF16)
    onesr_d = dram("onesr", [2, S], BF16)
    modb_d = dram("modb", [L, 128, 6 * D], kind="Internal")

    _sbc = {}

    def sb(name, shape, dt):
        if name not in _sbc:
            _sbc[name] = nc.alloc_sbuf_tensor("sb_" + name, list(shape), dt).ap()
        return _sbc[name]

    x = sb("x", [128, NT, D], F32)
    hT = sb("hT", [128, 8, S], BF16)
    cb = sb("cbs", [128, NCB], BF16)
    cf = sb("cfs", [128, NCF], F32)
    rowp = sb("rowps", [128, NRP - 2048], F32)
    colp = sb("colps", [128, NCP], F32)
    gate = sb("gate", [128, D], F32)
    small = sb("small", [128, 256], F32)
    arena = sb("arena", [128, 84224 // 2], BF16)
    ps = [nc.alloc_psum_tensor(f"ps{i}", [128, 512], F32).ap() for i in range(8)]
    PS = [f"ps{i}" for i in range(8)]

    ident_b = cb[:, CB_IDENT:CB_IDENT + 128]
    tri_add = cb[:, CB_TRIADD:CB_TRIADD + 128]
    tri_mul = cb[:, CB_TRIMUL:CB_TRIMUL + 128]
    bones64 = cb[:, CB_B64:CB_B64 + 128]
    bones32 = cb[:, CB_B32:CB_B32 + 128]
    ones_b = cb[:, CB_ONES:CB_ONES + 128]
    ident_f = cf[:, CF_IDENT:CF_IDENT + 128]

    def carve(off_bytes, shape, dt):
        n = int(np.prod(shape[1:]))
        esz = 4 if dt == F32 else 2
        a = arena[0:shape[0], off_bytes // 2: off_bytes // 2 + n * esz // 2]
        if dt == F32:
            a = a.bitcast(F32)
        if len(shape) == 3:
            a = a.rearrange("p (a b) -> p a b", a=shape[1])
        elif len(shape) == 4:
            a = a.rearrange("p (a b c) -> p a b c", a=shape[1], b=shape[2])
        return a

    def mm(out, lhsT, rhs, start, stop, reads, writes, inc=True):
        kw = {"skip_group_check": True}
        try:
            lhsT.base_partition()
        except BaseException:
            kw["tile_position"] = (96, 0)
        return op("pe", lambda e: e.matmul(out, lhsT=lhsT, rhs=rhs, start=start, stop=stop, **kw),
                  reads=reads, writes=writes, inc=inc)

    dma("sp", "c0", lambda e: e.dma_start(out=cb, in_=cb_d), writes=["cb"])
    dma("sp", "c1", lambda e: e.dma_start(out=cf, in_=cf_d), writes=["cf"])
    for t in range(NT):
        dma("sp", f"xin{t}", lambda e, t=t: e.dma_start(out=x[:, t, :], in_=x_d[t * 128:(t + 1) * 128, :]),
            writes=[("x", t)])
    cTs = small[:, 0:8]
    dma("sp", "c2", lambda e: e.dma_start(out=cTs, in_=cT_d), writes=["cT"])
    sc_e = small[:, 8:16]
    op("act", lambda e: e.activation(out=sc_e, in_=cTs, func=AF.Exp, scale=-1.0), reads=["cT"], writes=["sc_e"])
    op("dve", lambda e: e.tensor_scalar(out=sc_e, in0=sc_e, scalar1=1.0, scalar2=None, op0=ALU.add),
       reads=["sc_e"], writes=["sc_e"])
    op("dve", lambda e: e.reciprocal(out=sc_e, in_=sc_e), reads=["sc_e"], writes=["sc_e"])
    op("dve", lambda e: e.tensor_tensor(out=sc_e, in0=sc_e, in1=cTs, op=ALU.mult), reads=["sc_e", "cT"],
       writes=["sc_e"])
    eps_t = small[:, 16:17]
    op("dve", lambda e: e.memset(eps_t, EPS), writes=["eps"])
    lhs_rep = sb("lhs_rep", [128, 8, 128], BF16)
    gs = carve(20480, [128, D], F32)
    shf = carve(24576, [128, D], F32)
    for c in range(8):
        op("dve", lambda e, c=c: e.tensor_scalar(out=lhs_rep[:, c, :], in0=ones_b, scalar1=sc_e[:, c:c + 1],
                                                 scalar2=None, op0=ALU.mult),
           reads=["cb", "sc_e"], writes=["lhs_rep"])

    def phase_mod(l):
        aw = [carve(0, [128, 8, 512], BF16), carve(8192, [128, 8, 512], BF16)]
        ab = [carve(16384, [128, 512], F32), carve(18432, [128, 512], F32)]
        mo = [carve(20480, [128, 512], F32), carve(22528, [128, 512], F32)]
        awv = ada_w_d[l].rearrange("(c p) n -> p c n", p=128)
        for pc in range(12):
            b = pc % 2
            dma("pool", f"aw{b}", lambda e, b=b, pc=pc: e.dma_start(out=aw[b], in_=awv[:, :, pc * 512:(pc + 1) * 512]),
                writes=[("aw", b)])
            dma("sp", f"ab{b}", lambda e, b=b, pc=pc: e.dma_start(out=ab[b], in_=ada_b_d[l][:, pc * 512:(pc + 1) * 512]),
                writes=[("ab", b)])
            for c in range(8):
                mm(ps[b], lhs_rep[:, c, :], aw[b][:, c, :], c == 0, c == 7,
                   reads=["lhs_rep", ("aw", b)], writes=[PS[b]], inc=(c == 7))
            op("dve", lambda e, b=b: e.tensor_tensor(out=mo[b], in0=ps[b], in1=ab[b], op=ALU.add),
               reads=[PS[b], ("ab", b)], writes=[("mo", b)])
            dma("sp", f"mo{b}", lambda e, b=b, pc=pc: e.dma_start(out=modb_d[l][:, pc * 512:(pc + 1) * 512], in_=mo[b]),
                reads=[("mo", b)], writes=[("modb", l, pc // 2)])
        dma("sp", "rp", lambda e: e.dma_start(out=rowp, in_=rowp_d[l][:, 2048:NRP]), writes=["rowp"])
        dma("sp", "cp", lambda e: e.dma_start(out=colp, in_=colp_d[l]), writes=["colp"])

    def phase_norm(l, which):
        i_shift, i_scale = (0, 1) if which == 0 else (3, 4)
        tmpg = carve(0, [128, D], F32)
        junk = carve(4096, [128, D], BF16)
        tf = [carve(8192, [128, D], F32), carve(12288, [128, D], F32)]
        hb = [carve(16384, [128, D], BF16), carve(18432, [128, D], BF16)]
        ss = small[:, 32:48]
        rstd = small[:, 48:64]
        dma("sp", "gs", lambda e: e.dma_start(out=gs, in_=modb_d[l][:, i_scale * D:(i_scale + 1) * D]),
            reads=[("modb", l, i_scale)], writes=["gs"])
        dma("sp", "tg", lambda e: e.dma_start(out=tmpg, in_=rowp_d[l][:, which * D:(which + 1) * D]), writes=["tmpg"])
        dma("sp", "sh", lambda e: e.dma_start(out=shf, in_=modb_d[l][:, i_shift * D:(i_shift + 1) * D]),
            reads=[("modb", l, i_shift)], writes=["shf"])
        op("dve", lambda e: e.scalar_tensor_tensor(out=gs, in0=gs, scalar=1.0, in1=tmpg, op0=ALU.add, op1=ALU.mult),
           reads=["gs", "tmpg"], writes=["gs"])
        for t in range(NT):
            op("act", lambda e, t=t: e.activation(out=junk, in_=x[:, t, :], func=AF.Square, accum_out=ss[:, t:t + 1]),
               reads=[("x", t)], writes=["junk", "ss"])
        op("act", lambda e: e.activation(out=rstd, in_=ss, func=AF.Ln, bias=eps_t, scale=1.0 / D),
           reads=["ss", "eps"], writes=["rstd"])
        op("act", lambda e: e.activation(out=rstd, in_=rstd, func=AF.Exp, scale=-0.5), reads=["rstd"], writes=["rstd"])
        pst = ps[5].bitcast(BF16).rearrange("p (a b) -> p a b", a=8)
        for t in range(NT):
            b = t % 2
            op("dve", lambda e, t=t, b=b: e.scalar_tensor_tensor(out=tf[b], in0=x[:, t, :], scalar=rstd[:, t:t + 1], in1=gs,
                                                                 op0=ALU.mult, op1=ALU.mult),
               reads=[("x", t), "rstd", "gs"], writes=[("tf", b)])
            op("pool", lambda e, b=b: e.tensor_tensor(out=hb[b], in0=tf[b], in1=shf, op=ALU.add),
               reads=[("tf", b), "shf"], writes=[("hb", b)])
            for c in range(8):
                op("pe", lambda e, b=b, c=c: e.transpose(pst[:, c, :], hb[b][:, c * 128:(c + 1) * 128], ident_b),
                   reads=[("hb", b), "cb"], writes=[PS[5]])
            op("act", lambda e, t=t: e.activation(out=hT[:, :, t * 128:(t + 1) * 128], in_=pst, func=AF.Copy),
               reads=[PS[5]], writes=[("hT", t // 4)])

    A_BUFQ, A_BUFK, A_BUFIQ, A_BUFIK = 0, 8192, 16384, 24576
    A_V = 28672
    A_MIXTOK = 37120
    A_MIXT = 45312
    A_SCR = 53504
    A_W = 78080
    bufQ = carve(A_BUFQ, [128, 2, S], BF16)
    bufK = carve(A_BUFK, [128, 2, S], BF16)
    bufIQ = carve(A_BUFIQ, [128, 2, S], BF16)
    bufIK = carve(A_BUFIK, [128, S], BF16)
    Vaug = carve(A_V, [128, NT, 4, 65], BF16)
    mix_tok = carve(A_MIXTOK, [128, NT, 256], BF16)
    mixT = carve(A_MIXT, [128, 2, S], BF16)
    wst = [carve(A_W, [128, 8, 128], BF16), carve(A_W + 2048, [128, 8, 128], BF16),
           carve(A_W + 4096, [128, 8, 128], BF16)]
    wst_i = [0]
    sqb = sb("sqb", [128, 512], BF16)
    lnv = sb("lnv", [128, 512], F32)
    pT = [sb("pT0", [128, 512], BF16), sb("pT1", [128, 512], BF16), sb("pT2", [128, 512], BF16)]
    SBANK = [0, 1, 6]
    ac_i = [0]
    recs = sb("recs", [128, 16], F32)

    def load_w(l, c0, ncols):
        i = wst_i[0] % 3
        wst_i[0] += 1
        wv = w_in_d[l].rearrange("(c p) n -> p c n", p=128)
        dst = wst[i][:, :, 0:ncols]
        dma("pool", f"wst{i}", lambda e: e.dma_start(out=dst, in_=wv[:, :, c0:c0 + ncols]), writes=[("wst", i)])
        return dst, ("wst", i)

    pj_i = [0]

    def projT(wt, wreg, M, tg):
        b = pj_i[0] % 2
        pj_i[0] += 1
        for c in range(8):
            mm(ps[b][0:M, :], wt[:, c, 0:M], hT[:, c, tg * 512:(tg + 1) * 512], c == 0, c == 7,
               reads=[wreg, ("hT", tg)], writes=[PS[b]], inc=(c == 7))
        return ps[b], PS[b]

    def qk_norm_store(pb, preg, M, bones, inv_d, gcol, lnscale, dst, dreg, out2=None):
        op("act", lambda e: e.activation(out=sqb[0:M, :], in_=pb[0:M, :], func=AF.Square), reads=[preg], writes=["sqb"])
        mm(ps[4][0:M, :], bones[0:M, 0:M], sqb[0:M, :], True, True, reads=["sqb", "cb"], writes=[PS[4]])
        op("act", lambda e: e.activation(out=lnv[0:M, :], in_=ps[4][0:M, :], func=AF.Ln, bias=eps_t[0:M, :], scale=inv_d),
           reads=[PS[4], "eps"], writes=["lnv"])
        lb = small[:, 17 + int(lnscale):18 + int(lnscale)]
        op("act", lambda e: e.activation(out=lnv[0:M, :], in_=lnv[0:M, :], func=AF.Exp, bias=lb[0:M, :], scale=-0.5),
           reads=["lnv", "lnb"], writes=["lnv"])
        op("dve", lambda e: e.scalar_tensor_tensor(out=dst, in0=pb[0:M, :], scalar=gcol[0:M, :], in1=lnv[0:M, :],
                                                   op0=ALU.mult, op1=ALU.mult),
           reads=[preg, "lnv", "colp"], writes=[dreg])
        if out2 is not None:
            gcol2, dst2, dreg2 = out2
            op("dve", lambda e: e.scalar_tensor_tensor(out=dst2, in0=pb[0:M, :], scalar=gcol2[0:M, :], in1=lnv[0:M, :],
                                                       op0=ALU.mult, op1=ALU.mult),
               reads=[preg, "lnv", "colp"], writes=[dreg2])

    def proj_v(l, c0, nheads, dstV, vreg):
        ncols = nheads * 64
        wparts = []
        for j in range(0, ncols, 128):
            wparts.append(load_w(l, c0 + j, min(128, ncols - j)))
        for t in range(NT):
            b = pj_i[0] % 2
            pj_i[0] += 1
            for j, (wt, wreg) in enumerate(wparts):
                n = wt.shape[2]
                for c in range(8):
                    mm(ps[b][:, j * 128:j * 128 + n], hT[:, c, t * 128:(t + 1) * 128], wt[:, c, :], c == 0 and j == 0, c == 7,
                       reads=[wreg, ("hT", t // 4)], writes=[PS[b]])
            src = ps[b][:, 0:ncols].rearrange("p (h d) -> p h d", h=nheads)
            op("act", lambda e, t=t, src=src: e.activation(out=dstV[:, t, 0:nheads, 0:64], in_=src, func=AF.Copy),
               reads=[PS[b]], writes=[vreg])

    po_i = [0]

    def run_pipelined(items, depth=1):
        for k in range(min(depth, len(items))):
            items[k][0]()
        for i, (_, s2) in enumerate(items):
            if i + depth < len(items):
                items[i + depth][0]()
            s2()

    def attn_core(nheads_spec, finish_fn, QG=512):
        nqb = QG // 128
        items = []
        for si, sp_ in enumerate(nheads_spec):
            for qg in range(S // QG):
                pb = 2 + (po_i[0] % 2)
                po_i[0] += 1
                po, poreg = ps[pb], PS[pb]
                first_po = [True]
                nkb = nqb * (qg + 1)
                for kb in range(nkb):
                    st = {}

                    def s1(sp_=sp_, qg=qg, kb=kb, st=st):
                        j = kb - nqb * qg
                        c0 = 0 if j < 0 else j * 128
                        sbk = ac_i[0] % 3
                        ac_i[0] += 1
                        pss, psreg = ps[SBANK[sbk]], PS[SBANK[sbk]]
                        has_extra = sp_.get("extra") is not None
                        dm = sp_.get("diag_mask", False) and j >= 0
                        mm(pss[:, c0:QG], sp_["kT"](kb), sp_["qT"](qg * QG + c0, (qg + 1) * QG), True,
                           not (has_extra or dm), reads=sp_["kq_regs"], writes=[psreg])
                        if has_extra:
                            sp_["extra"](pss, psreg, qg, kb, c0, not dm)
                        if dm:
                            mm(pss[:, c0:c0 + 128], ident_b, tri_add, False, True, reads=["cb"], writes=[psreg])
                        st.update(sbk=sbk, c0=c0)

                    def s2(sp_=sp_, si=si, qg=qg, kb=kb, st=st, po=po, poreg=poreg, first_po=first_po, last=(kb == nkb - 1)):
                        sbk, c0 = st["sbk"], st["c0"]
                        pss, psreg = ps[SBANK[sbk]], PS[SBANK[sbk]]
                        pt = pT[sbk]
                        bias_ap = sp_["bias"](kb, qg)
                        op("act", lambda e, pt=pt, pss=pss, c0=c0, bias_ap=bias_ap: e.activation(
                            out=pt[:, c0:QG], in_=pss[:, c0:QG], func=AF.Exp, bias=bias_ap),
                           reads=[psreg] + sp_["bias_regs"], writes=[("pT", sbk)])
                        for i in range(c0 // 128, nqb):
                            mm(po[:, i * 65:(i + 1) * 65], pt[:, i * 128:(i + 1) * 128], sp_["V"](kb), first_po[0], False,
                               reads=[("pT", sbk)] + sp_["v_regs"], writes=[poreg])
                            first_po[0] = False
                        if last:
                            finish_fn(si, qg, po, poreg)

                    items.append((s1, s2))
        run_pipelined(items, depth=2)

    def finish_softmax(mcol0):
        def fin(si, qg, po, poreg):
            for i in range(4):
                t = qg * 4 + i
                op("dve", lambda e, i=i: e.reciprocal(out=recs[:, i:i + 1], in_=po[:, i * 65 + 64:i * 65 + 65]),
                   reads=[poreg], writes=["recs"])
                op("dve", lambda e, i=i, t=t: e.tensor_scalar(out=mix_tok[:, t, mcol0 + si * 64:mcol0 + (si + 1) * 64],
                                                              in0=po[:, i * 65:i * 65 + 64], scalar1=recs[:, i:i + 1],
                                                              scalar2=None, op0=ALU.mult),
                   reads=[poreg, "recs"], writes=[("mix_tok", t)])
        return fin

    def mixer_out(l, m):
        pst = ps[5].bitcast(BF16).rearrange("p (a b) -> p a b", a=8)
        for t in range(NT):
            for c in range(2):
                op("pe", lambda e, t=t, c=c: e.transpose(pst[:, c, :], mix_tok[:, t, c * 128:(c + 1) * 128], ident_b),
                   reads=[("mix_tok", t), "cb"], writes=[PS[5]])
            op("act", lambda e, t=t: e.activation(out=mixT[:, :, t * 128:(t + 1) * 128], in_=pst[:, 0:2, :], func=AF.Copy),
               reads=[PS[5]], writes=["mixT"])
        wo_f = carve(A_SCR, [128, 2, D], F32)
        wo_b = carve(A_SCR + 8192, [128, 2, D], BF16)
        wv = w_out_d[l][m * 256:(m + 1) * 256, :].rearrange("(c p) n -> p c n", p=128)
        dma("sp", "wo", lambda e: e.dma_start(out=wo_f, in_=wv), writes=["wo_f"])
        for c in range(2):
            bc = colp[:, CP_BETA + m * 2 + c:CP_BETA + m * 2 + c + 1]
            op("dve", lambda e, c=c, bc=bc: e.scalar_tensor_tensor(out=wo_b[:, c, :], in0=wo_f[:, c, :], scalar=bc, in1=gate,
                                                                   op0=ALU.mult, op1=ALU.mult),
               reads=["wo_f", "colp", "gate"], writes=["wo_b"])
        for t in range(NT):
            for hf in range(2):
                b = 6 + (t * 2 + hf) % 2
                for c in range(2):
                    mm(ps[b], mixT[:, c, t * 128:(t + 1) * 128], wo_b[:, c, hf * 512:(hf + 1) * 512], c == 0, c == 1,
                       reads=["mixT", "wo_b"], writes=[PS[b]])
                op("dve", lambda e, t=t, hf=hf, b=b: e.tensor_tensor(out=x[:, t, hf * 512:(hf + 1) * 512],
                                                                     in0=x[:, t, hf * 512:(hf + 1) * 512], in1=ps[b], op=ALU.add),
                   reads=[PS[b], ("x", t)], writes=[("x", t)])

    def set_v_ones():
        op("pool", lambda e: e.memset(Vaug[:, :, :, 64:65], 1.0), writes=["V"])

    def set_lnb():
        op("dve", lambda e: e.memset(small[:, 17:18], 0.0), writes=["lnb"])
        op("dve", lambda e: e.memset(small[:, 18:19], math.log(0.125)), writes=["lnb"])
        op("dve", lambda e: e.memset(small[:, 19:20], math.log(32 ** -0.5)), writes=["lnb"])

    def mixer_A(l):
        set_v_ones()
        spT = carve(A_SCR, [4, S], F32)
        cums = carve(A_SCR + 8192, [4, S], F32)
        chiT = carve(A_SCR + 16384, [4, S], BF16)
        ones4 = carve(A_SCR + 20480, [4, 512], F32)
        poscum = sb("poscum", [128, NT, 4], F32)
        nbf = small[:, 20:21]
        op("dve", lambda e: e.tensor_scalar(out=nbf, in0=colp[:, CP_BF:CP_BF + 1], scalar1=-1.0, scalar2=None, op0=ALU.mult),
           reads=["colp"], writes=["nbf"])
        op("dve", lambda e: e.memset(ones4, 1.0), writes=["ones4"])
        for (c0, dst, gc, lns, dreg) in ((0, bufQ, CP_QNA, 1.0, "bufQ"), (256, bufK, CP_KNA, 0.0, "bufK")):
            for ch in range(2):
                wt, wreg = load_w(l, c0 + ch * 128, 128)
                for tg in range(4):
                    pb, preg = projT(wt, wreg, 128, tg)
                    qk_norm_store(pb, preg, 128, bones64, 1.0 / 64, colp[:, gc:gc + 1], lns,
                                  dst[:, ch, tg * 512:(tg + 1) * 512], dreg)
        wt, wreg = load_w(l, 768, 4)
        for tg in range(4):
            pb, preg = projT(wt, wreg, 4, tg)
            op("act", lambda e, pb=pb, tg=tg: e.activation(out=spT[:, tg * 512:(tg + 1) * 512], in_=pb[0:4, :], func=AF.Exp,
                                                           bias=nbf[0:4, :], scale=-1.0),
               reads=[preg, "nbf"], writes=["spT"])
        op("act", lambda e: e.activation(out=spT, in_=spT, func=AF.Ln, bias=1.0), reads=["spT"], writes=["spT"])
        for tg in range(4):
            init = 0.0 if tg == 0 else cums[:, tg * 512 - 1:tg * 512]
            op("dve", lambda e, tg=tg, init=init: e.tensor_tensor_scan(out=cums[:, tg * 512:(tg + 1) * 512], data0=ones4,
                                                                       data1=spT[:, tg * 512:(tg + 1) * 512], initial=init,
                                                                       op0=ALU.mult, op1=ALU.add),
               reads=["spT", "ones4", "cums"], writes=["cums"])
        op("dve", lambda e: e.tensor_scalar(out=chiT, in0=cums, scalar1=-1.0, scalar2=None, op0=ALU.mult),
           reads=["cums"], writes=["chiT"])
        for t in range(NT):
            op("pe", lambda e, t=t: e.transpose(ps[4][:, t * 4:(t + 1) * 4], cums[0:4, t * 128:(t + 1) * 128], ident_f[0:4, 0:4]),
               reads=["cums", "cf"], writes=[PS[4]])
        op("dve", lambda e: e.tensor_copy(out=poscum, in_=ps[4][:, 0:64].rearrange("p (t h) -> p t h", t=NT)),
           reads=[PS[4]], writes=["poscum"])
        proj_v(l, 512, 4, Vaug, "V")
        sel4 = [sb(f"sel4_{h_}", [4, 128], BF16) for h_ in range(4)]
        for h_ in range(4):
            op("dve", lambda e, h_=h_: e.tensor_scalar(out=sel4[h_], in0=ones_b[0:4, :], scalar1=ident_f[0:4, h_:h_ + 1],
                                                       scalar2=None, op0=ALU.mult), reads=["cb", "cf"], writes=["sel4"])
        specs = []
        for h in range(4):
            hp, ch = (h % 2) * 64, h // 2

            def extra(pss, psreg, qg, kb, c0, last, h=h):
                mm(pss[:, c0:512], sel4[h],
                   chiT[0:4, qg * 512 + c0:(qg + 1) * 512], False, last, reads=["sel4", "chiT"], writes=[psreg])

            specs.append(dict(
                kT=lambda kb, hp=hp, ch=ch: bufK[hp:hp + 64, ch, kb * 128:(kb + 1) * 128],
                qT=lambda q0, q1, hp=hp, ch=ch: bufQ[hp:hp + 64, ch, q0:q1],
                kq_regs=["bufQ", "bufK"], extra=extra, diag_mask=True,
                bias=lambda kb, qg, h=h: poscum[:, kb, h:h + 1], bias_regs=["poscum"],
                V=lambda kb, h=h: Vaug[:, kb, h, :], v_regs=["V"]))
        attn_core(specs, finish_softmax(0))

    def mixer_B(l):
        QG = 256
        lbuf = carve(A_SCR, [128, NT, QG], F32)
        l1m = carve(A_SCR + 16384, [128, NT, QG], BF16)
        l1m2 = carve(A_BUFIK, [128, 8, QG], BF16)
        mixscr = sb("mixscr", [128, 512], F32)
        argb = [mixscr[:, 0:QG], mixscr[:, QG:2 * QG], carve(A_W, [128, QG], F32),
                carve(A_W + 1024, [128, QG], F32)]
        wTb = [sb("wTb0", [128, QG], BF16), sb("wTb1", [128, QG], BF16), carve(A_W + 2048, [128, QG], BF16),
               carve(A_W + 2560, [128, QG], BF16)]
        BBANK = [0, 1, 6, 7]
        b2_i = [0]
        for (c0, dst, scl, dreg) in ((772, bufQ, 0.125, "bufQ"), (1028, bufK, 1.0, "bufK")):
            for ch in range(2):
                wt, wreg = load_w(l, c0 + ch * 128, 128)
                for tg in range(4):
                    pb, preg = projT(wt, wreg, 128, tg)
                    op("dve", lambda e, pb=pb, dst=dst, ch=ch, tg=tg, scl=scl: e.tensor_scalar(
                        out=dst[:, ch, tg * 512:(tg + 1) * 512], in0=pb, scalar1=scl, scalar2=None, op0=ALU.mult),
                       reads=[preg], writes=[dreg])
        proj_v(l, 1284, 4, Vaug, "V")
        for h in range(4):
            hp, ch = (h % 2) * 64, h // 2
            for qg in range(S // QG):
                nkb = 2 * (qg + 1)
                pb_ = 2 + (po_i[0] % 2)
                po_i[0] += 1
                po, poreg = ps[pb_], PS[pb_]
                for kb in range(nkb):
                    j = kb - 2 * qg
                    c0 = 0 if j < 0 else j * 128
                    sbk = pj_i[0] % 2
                    pj_i[0] += 1
                    pss, psreg = ps[sbk], PS[sbk]
                    mm(pss[:, c0:QG], bufK[hp:hp + 64, ch, kb * 128:(kb + 1) * 128],
                       bufQ[hp:hp + 64, ch, qg * QG + c0:(qg + 1) * QG], True, True, reads=["bufQ", "bufK"], writes=[psreg])
                    op("act", lambda e, kb=kb, c0=c0, pss=pss: e.activation(out=lbuf[:, kb, c0:QG], in_=pss[:, c0:QG],
                                                                            func=AF.Exp, scale=-1.0),
                       reads=[psreg], writes=[("lbuf", kb)])
                    op("act", lambda e, kb=kb, c0=c0: e.activation(out=lbuf[:, kb, c0:QG], in_=lbuf[:, kb, c0:QG],
                                                                   func=AF.Ln, bias=1.0),
                       reads=[("lbuf", kb)], writes=[("lbuf", kb)])
                    op("dve", lambda e, kb=kb, c0=c0, pss=pss: e.scalar_tensor_tensor(
                        out=l1m[:, kb, c0:QG], in0=pss[:, c0:QG], scalar=-1.0, in1=lbuf[:, kb, c0:QG],
                        op0=ALU.mult, op1=ALU.subtract),
                       reads=[psreg, ("lbuf", kb)], writes=[("l1m", kb)])
                    if j >= 0:
                        op("dve", lambda e, kb=kb, c0=c0: e.tensor_tensor(out=l1m[:, kb, c0:c0 + 128],
                                                                           in0=l1m[:, kb, c0:c0 + 128], in1=tri_mul, op=ALU.mult),
                           reads=[("l1m", kb), "cb"], writes=[("l1m", kb)])
                    elif kb % 2 == 1:
                        op("pool", lambda e, kb=kb: e.tensor_tensor(out=l1m2[:, kb // 2, :], in0=l1m[:, kb - 1, :], in1=l1m[:, kb, :],
                                                                    op=ALU.add),
                           reads=[("l1m", kb - 1), ("l1m", kb)], writes=[("l1m2", kb // 2)])
                first_po = [True]
                items = []
                for kb in range(nkb):
                    st = {}

                    def s1(kb=kb, st=st, qg=qg, nkb=nkb):
                        j = kb - 2 * qg
                        c0 = 0 if j < 0 else j * 128
                        sbk = b2_i[0] % 4
                        b2_i[0] += 1
                        pss, psreg = ps[BBANK[sbk]], PS[BBANK[sbk]]
                        later = []
                        k_ = kb + 1
                        nod = 2 * qg
                        while k_ < nkb:
                            if k_ % 2 == 0 and k_ + 1 < nod:
                                later.append(("pair", k_ // 2))
                                k_ += 2
                            else:
                                later.append(("blk", k_))
                                k_ += 1
                        mm(pss[:, c0:QG], triL, l1m[:, kb, c0:QG], True, len(later) == 0,
                           reads=[("l1m", kb), "triL"], writes=[psreg])
                        for n_, (kind, ix) in enumerate(later):
                            if kind == "pair":
                                mm(pss[:, c0:QG], ones_b, l1m2[:, ix, c0:QG], False, n_ == len(later) - 1,
                                   reads=[("l1m2", ix), "cb"], writes=[psreg])
                                continue
                            kb2 = ix
                            j2 = kb2 - 2 * qg
                            c2 = 0 if j2 < 0 else j2 * 128
                            cc = max(c0, c2)
                            mm(pss[:, cc:QG], ones_b, l1m[:, kb2, cc:QG], False, n_ == len(later) - 1,
                               reads=[("l1m", kb2), "cb"], writes=[psreg])
                        st.update(sbk=sbk, c0=c0, j=j)

                    def s2(kb=kb, st=st, qg=qg, h=h, po=po, poreg=poreg, first_po=first_po, last=(kb == nkb - 1)):
                        sbk, c0, j = st["sbk"], st["c0"], st["j"]
                        pss, psreg = ps[BBANK[sbk]], PS[BBANK[sbk]]
                        ab_ = argb[sbk]
                        op("dve", lambda e, ab_=ab_, pss=pss, kb=kb, c0=c0: e.tensor_tensor(
                            out=ab_[:, c0:QG], in0=pss[:, c0:QG], in1=lbuf[:, kb, c0:QG], op=ALU.subtract),
                           reads=[psreg, ("lbuf", kb)], writes=[("argb", sbk)])
                        wt_ = wTb[sbk]
                        op("act", lambda e, ab_=ab_, wt_=wt_, c0=c0: e.activation(out=wt_[:, c0:QG], in_=ab_[:, c0:QG], func=AF.Exp),
                           reads=[("argb", sbk)], writes=[("wTb", sbk)])
                        if j >= 0:
                            op("dve", lambda e, wt_=wt_, c0=c0: e.tensor_tensor(out=wt_[:, c0:c0 + 128], in0=wt_[:, c0:c0 + 128],
                                                                                 in1=tri_mul, op=ALU.mult),
                               reads=[("wTb", sbk), "cb"], writes=[("wTb", sbk)])
                        for i in range(c0 // 128, 2):
                            mm(po[:, i * 65:i * 65 + 64], wt_[:, i * 128:(i + 1) * 128], Vaug[:, kb, h, 0:64], first_po[0], False,
                               reads=[("wTb", sbk), "V"], writes=[poreg])
                            first_po[0] = False
                        if not last:
                            return
                        for i in range(2):
                            t = qg * 2 + i
                            op("act", lambda e, i=i, t=t, po=po, h=h: e.activation(out=mix_tok[:, t, h * 64:(h + 1) * 64],
                                                                                   in_=po[:, i * 65:i * 65 + 64], func=AF.Copy),
                               reads=[poreg], writes=[("mix_tok", t)])

                    items.append((s1, s2))
                run_pipelined(items, depth=3)

    triL = sb("triL", [128, 128], BF16)
    op("pe", lambda e: e.transpose(ps[5].bitcast(BF16)[:, 0:128], tri_mul, ident_b), reads=["cb"], writes=[PS[5]])
    op("dve", lambda e: e.tensor_copy(out=triL, in_=ps[5].bitcast(BF16)[:, 0:128]), reads=[PS[5]], writes=["triL"])

    def mixer_C(l):
        lam_init = 0.8 - 0.6 * math.exp(-0.3 * l)
        set_v_ones()
        lamrow = rowp[:, RP_LAM - 2048:RP_LAM - 2048 + 128]
        lt = small[:, 64:128]
        ls = small[:, 24:28]
        op("dve", lambda e: e.tensor_tensor(out=lt[:, 0:32], in0=lamrow[:, 0:32], in1=lamrow[:, 32:64], op=ALU.mult),
           reads=["rowp"], writes=["lt"])
        op("dve", lambda e: e.tensor_tensor(out=lt[:, 32:64], in0=lamrow[:, 64:96], in1=lamrow[:, 96:128], op=ALU.mult),
           reads=["rowp"], writes=["lt"])
        op("dve", lambda e: e.reduce_sum(out=ls[:, 0:1], in_=lt[:, 0:32], axis=mybir.AxisListType.X), reads=["lt"], writes=["ls"])
        op("dve", lambda e: e.reduce_sum(out=ls[:, 1:2], in_=lt[:, 32:64], axis=mybir.AxisListType.X), reads=["lt"], writes=["ls"])
        op("act", lambda e: e.activation(out=ls[:, 0:2], in_=ls[:, 0:2], func=AF.Exp), reads=["ls"], writes=["ls"])
        op("dve", lambda e: e.tensor_tensor(out=ls[:, 2:3], in0=ls[:, 1:2], in1=ls[:, 0:1], op=ALU.subtract),
           reads=["ls"], writes=["ls"])
        op("dve", lambda e: e.tensor_scalar(out=ls[:, 2:3], in0=ls[:, 2:3], scalar1=-lam_init, scalar2=None, op0=ALU.add),
           reads=["ls"], writes=["ls"])
        nlam = ls[:, 2:3]
        if stopC <= 1:
            return
        bufK1 = carve(A_SCR + 4096, [128, 2, S], BF16)
        for (c0, dst, gc, lns, dreg) in ((1540, bufQ, CP_QNC, 2.0, "bufQ"), (1796, bufK, CP_KNC, 0.0, "bufK")):
            for ch in range(2):
                wt, wreg = load_w(l, c0 + ch * 128, 128)
                for tg in range(4):
                    pb, preg = projT(wt, wreg, 128, tg)
                    if dst is bufQ:
                        o2_ = (colp[:, CP_QNC1:CP_QNC1 + 1], bufIQ[:, ch, tg * 512:(tg + 1) * 512], "bufIQ")
                    else:
                        o2_ = (colp[:, gc:gc + 1], bufK1[:, ch, tg * 512:(tg + 1) * 512], "bufK1")
                    qk_norm_store(pb, preg, 128, bones32, 1.0 / 32, colp[:, gc:gc + 1], lns,
                                  dst[:, ch, tg * 512:(tg + 1) * 512], dreg, out2=o2_)
        if stopC <= 2:
            return
        for h in range(4):
            hp, ch = (h % 2) * 64, h // 2
            for c, (qc, qreg, kc, kreg) in enumerate(((bufQ, "bufQ", bufK, "bufK"), (bufIQ, "bufIQ", bufK1, "bufK1"))):
                r0 = hp + 32 * (1 - c)
                dma("sp", f"alq{c}", lambda e, qc=qc, r0=r0, ch=ch, h=h: e.dma_start(out=qc[r0:r0 + 2, ch, :], in_=alc_d[h]),
                    writes=[qreg])
                dma("sp", f"alk{c}", lambda e, kc=kc, r0=r0, ch=ch: e.dma_start(out=kc[r0:r0 + 2, ch, :], in_=onesr_d),
                    writes=[kreg])
        proj_v(l, 2052, 4, Vaug, "V")
        if stopC <= 3:
            return
        o1 = sb("o1c", [128, 64], F32)
        o2 = sb("o2c", [128, 64], F32)
        keep = {}
        specs = []
        for h in range(4):
            for c in range(2):
                hp, ch = (h % 2) * 64, h // 2
                qsrc = bufQ if c == 0 else bufIQ
                ksrc = bufK if c == 0 else bufK1
                specs.append(dict(
                    kT=lambda kb, hp=hp, ch=ch, ksrc=ksrc: ksrc[hp:hp + 64, ch, kb * 128:(kb + 1) * 128],
                    qT=lambda q0, q1, hp=hp, ch=ch, qsrc=qsrc: qsrc[hp:hp + 64, ch, q0:q1],
                    kq_regs=["bufQ", "bufIQ", "bufK", "bufK1"], extra=None, diag_mask=("nodiag" not in cdbg),
                    bias=lambda kb, qg, h=h: cf[:, CF_ALIBI + h * AL_N + (kb - 4 * qg) + AL_O:CF_ALIBI + h * AL_N + (kb - 4 * qg) + AL_O + 1],
                    bias_regs=["cf"],
                    V=lambda kb, h=h: Vaug[:, kb, h, :], v_regs=["V"]))

        stash = carve(A_SCR, [128, NT, 64], F32)

        def fin(si, qg, po, poreg):
            h, c = si // 2, si % 2
            if "nofin" in cdbg:
                return
            for i in range(4):
                t = qg * 4 + i
                op("dve", lambda e, i=i: e.reciprocal(out=recs[:, i:i + 1], in_=po[:, i * 65 + 64:i * 65 + 65]),
                   reads=[poreg], writes=["recs"])
                if c == 0:
                    op("dve", lambda e, i=i, t=t: e.tensor_scalar(out=stash[:, t, :], in0=po[:, i * 65:i * 65 + 64],
                                                                  scalar1=recs[:, i:i + 1], scalar2=None, op0=ALU.mult),
                       reads=[poreg, "recs"], writes=[("stash", t)])
                else:
                    op("dve", lambda e, i=i: e.tensor_scalar(out=o1, in0=po[:, i * 65:i * 65 + 64], scalar1=recs[:, i:i + 1],
                                                             scalar2=nlam, op0=ALU.mult, op1=ALU.mult),
                       reads=[poreg, "recs", "ls"], writes=["o1"])
                    op("dve", lambda e, t=t: e.tensor_tensor(out=o1, in0=o1, in1=stash[:, t, :], op=ALU.add),
                       reads=["o1", ("stash", t)], writes=["o1"])
                    op("act", lambda e: e.activation(out=o2, in_=o1, func=AF.Square, accum_out=small[:, 28:29]),
                       reads=["o1"], writes=["o2", "cs"])
                    op("act", lambda e: e.activation(out=small[:, 28:29], in_=small[:, 28:29], func=AF.Ln, bias=eps_t, scale=1.0 / 64),
                       reads=["cs", "eps"], writes=["cs"])
                    op("act", lambda e: e.activation(out=small[:, 28:29], in_=small[:, 28:29], func=AF.Exp,
                                                     bias=small[:, 21:22], scale=-0.5),
                       reads=["cs", "lnb2"], writes=["cs"])
                    op("dve", lambda e, t=t, h=h: e.scalar_tensor_tensor(out=mix_tok[:, t, h * 64:(h + 1) * 64], in0=o1,
                                                                         scalar=small[:, 28:29],
                                                                         in1=rowp[:, RP_SUBLN - 2048:RP_SUBLN - 2048 + 64],
                                                                         op0=ALU.mult, op1=ALU.mult),
                       reads=["o1", "cs", "rowp"], writes=[("mix_tok", t)])

        op("dve", lambda e: e.memset(small[:, 21:22], math.log(1.0 - lam_init)), writes=["lnb2"])
        attn_core(specs, fin)

    def mixer_D(l):
        set_v_ones()
        rtmp = sb("mixscr", [128, 512], F32)
        mx8 = sb("mx8", [128, 8], F32)
        thr = small[:, 128:144]
        iw = sb("iw", [128, NT, 8], F32)
        iwa = sb("iwa", [128, NT, 8], F32)
        iws = sb("iws", [128, NT, 8], F32)
        iqm = [sb(f"iqm{k_}", [128, 128], BF16) for k_ in range(8)]
        for ch in range(2):
            wt, wreg = load_w(l, 2308 + ch * 128, 128)
            for tg in range(4):
                pb, preg = projT(wt, wreg, 128, tg)
                qk_norm_store(pb, preg, 128, bones64, 1.0 / 64, colp[:, CP_QND:CP_QND + 1], 1.0,
                              bufQ[:, ch, tg * 512:(tg + 1) * 512], "bufQ")
        wt, wreg = load_w(l, 2564, 64)
        ik2 = wst_i[0] % 3
        wst_i[0] += 1
        wk2 = wst[ik2]
        for r in range(2):
            op("dve", lambda e, r=r, wt=wt: e.tensor_copy(out=wk2[:, :, r * 64:(r + 1) * 64], in_=wt), reads=[wreg], writes=[("wst", ik2)])
        for tg in range(4):
            pb, preg = projT(wk2, ("wst", ik2), 128, tg)
            qk_norm_store(pb, preg, 128, bones64, 1.0 / 64, colp[:, CP_KND:CP_KND + 1], 0.0,
                          bufK[:, 0, tg * 512:(tg + 1) * 512], "bufK")
        if stopD <= 1:
            return
        for ch in range(2):
            wt, wreg = load_w(l, 2692 + ch * 128, 128)
            for tg in range(4):
                pb, preg = projT(wt, wreg, 128, tg)
                op("dve", lambda e, pb=pb, ch=ch, tg=tg: e.tensor_scalar(out=bufIQ[:, ch, tg * 512:(tg + 1) * 512], in0=pb,
                                                                         scalar1=32 ** -0.5, scalar2=None, op0=ALU.mult),
                   reads=[preg], writes=["bufIQ"])
        wt, wreg = load_w(l, 2948, 32)
        i4 = wst_i[0] % 3
        wst_i[0] += 1
        w4 = wst[i4]
        for r in range(4):
            op("dve", lambda e, r=r, wt=wt: e.tensor_copy(out=w4[:, :, r * 32:(r + 1) * 32], in_=wt), reads=[wreg], writes=[("wst", i4)])
        for tg in range(4):
            pb, preg = projT(w4, ("wst", i4), 128, tg)
            op("act", lambda e, pb=pb, tg=tg: e.activation(out=bufIK[:, tg * 512:(tg + 1) * 512], in_=pb, func=AF.Copy),
               reads=[preg], writes=["bufIK"])
        wt, wreg = load_w(l, 2980, 8)
        for t in range(NT):
            b = pj_i[0] % 2
            pj_i[0] += 1
            for c in range(8):
                mm(ps[b][:, 0:8], hT[:, c, t * 128:(t + 1) * 128], wt[:, c, :], c == 0, c == 7,
                   reads=[wreg, ("hT", t // 4)], writes=[PS[b]])
            op("dve", lambda e, t=t, b=b: e.tensor_scalar(out=iw[:, t, :], in0=ps[b][:, 0:8], scalar1=8 ** -0.5, scalar2=None, op0=ALU.mult),
               reads=[PS[b]], writes=["iw"])
        op("act", lambda e: e.activation(out=iwa, in_=iw, func=AF.Abs), reads=["iw"], writes=["iwa"])
        op("act", lambda e: e.activation(out=iws, in_=iw, func=AF.Sign), reads=["iw"], writes=["iws"])
        proj_v(l, 2628, 1, Vaug, "V")

        if stopD <= 2:
            return
        QG = 256
        scoreb = [carve(A_MIXT, [128, S], F32), carve(A_MIXT + 8192, [128, S], F32)]
        mbvb = [carve(A_MIXT + 16384, [128, 2, S], BF16), carve(A_MIXT + 24576, [128, 2, S], BF16)]
        rt = [rtmp, carve(A_BUFK + 4096, [128, 512], F32)]
        posb = carve(A_W, [128, 2, 132], F32)
        den = small[:, 144:148]
        rt_i = [0]

        def index_topk(qg):
            for i in range(2):
                t = qg * 2 + i
                nk = (t + 1) * 128
                score, sreg = scoreb[t % 2], ("score", t % 2)
                mbv = mbvb[qg % 2]
                for hh in range(8):
                    g, ch = hh % 4, hh // 4
                    op("pool", lambda e, hh=hh, g=g, ch=ch, t=t: e.tensor_scalar(
                        out=iqm[hh], in0=bufIQ[:, ch, t * 128:(t + 1) * 128], scalar1=cf[:, CF_GMASK + g:CF_GMASK + g + 1],
                        scalar2=None, op0=ALU.mult), reads=["bufIQ", "cf"], writes=[("iqm", hh)])
                for k0 in range(0, nk, 512):
                    kn = min(512, nk - k0)
                    for hh in range(8):
                        b = pj_i[0] % 2
                        pj_i[0] += 1
                        r = rt_i[0] % 2
                        rt_i[0] += 1
                        mm(ps[b][:, 0:kn], iqm[hh], bufIK[:, k0:k0 + kn], True, True,
                           reads=[("iqm", hh), "bufIK"], writes=[PS[b]])
                        op("act", lambda e, b=b, kn=kn, t=t, hh=hh, r=r: e.activation(out=rt[r][:, 0:kn], in_=ps[b][:, 0:kn], func=AF.Relu,
                                                                                      scale=iwa[:, t, hh:hh + 1]),
                           reads=[PS[b], "iwa"], writes=[("rtmp", r)])
                        if hh == 0:
                            op("act", lambda e, k0=k0, kn=kn, t=t, r=r, score=score: e.activation(
                                out=score[:, k0:k0 + kn], in_=rt[r][:, 0:kn], func=AF.Copy, scale=iws[:, t, 0:1]),
                               reads=[("rtmp", r), "iws"], writes=[sreg])
                        else:
                            op("act", lambda e, kn=kn, t=t, hh=hh, r=r: e.activation(
                                out=rt[r][:, 0:kn], in_=rt[r][:, 0:kn], func=AF.Copy, scale=iws[:, t, hh:hh + 1]),
                               reads=[("rtmp", r), "iws"], writes=[("rtmp", r)])
                            op("pool", lambda e, k0=k0, kn=kn, r=r, score=score: e.tensor_tensor(
                                out=score[:, k0:k0 + kn], in0=score[:, k0:k0 + kn], in1=rt[r][:, 0:kn], op=ALU.add),
                               reads=[("rtmp", r), sreg], writes=[sreg])
                op("pool", lambda e, t=t, score=score: e.tensor_tensor(out=score[:, t * 128:(t + 1) * 128], in0=score[:, t * 128:(t + 1) * 128],
                                                                       in1=cf[:, CF_NEGTRI:CF_NEGTRI + 128], op=ALU.add),
                   reads=[sreg, "cf"], writes=[sreg])
                if t < 2:
                    op("dve", lambda e, i=i, nk=nk, score=score, mbv=mbv: e.tensor_scalar(
                        out=mbv[:, i, 0:nk], in0=score[:, 0:nk], scalar1=-1e29, scalar2=NEG, op0=ALU.is_lt, op1=ALU.mult),
                       reads=[sreg], writes=[("mbv", qg % 2, i)])
                else:
                    for r_ in range(32):
                        op("dve", lambda e, score=score, nk=nk: e.max(out=mx8, in_=score[:, 0:nk]), reads=[sreg], writes=["mx8"])
                        op("dve", lambda e, score=score, nk=nk: e.match_replace(out=score[:, 0:nk], in_to_replace=mx8,
                                                                                in_values=score[:, 0:nk], imm_value=-3e38),
                           reads=[sreg, "mx8"], writes=[sreg])
                    op("dve", lambda e, i=i, nk=nk, score=score, mbv=mbv: e.tensor_scalar(
                        out=mbv[:, i, 0:nk], in0=score[:, 0:nk], scalar1=-1e35, scalar2=NEG, op0=ALU.is_ge, op1=ALU.mult),
                       reads=[sreg], writes=[("mbv", qg % 2, i)])

        def attention(qg):
            nkb = 2 * (qg + 1)
            mbv = mbvb[qg % 2]
            items = []
            for h in range(4):
                hp, ch = (h % 2) * 64, h // 2
                pb_ = 2 + (po_i[0] % 2)
                po_i[0] += 1
                po, poreg = ps[pb_], PS[pb_]
                first_po = [True]
                for kb in range(nkb):
                    st = {}

                    def s1(h=h, hp=hp, ch=ch, kb=kb, st=st):
                        j = kb - 2 * qg
                        c0 = 0 if j < 0 else j * 128
                        sbk = pj_i[0] % 2
                        pj_i[0] += 1
                        pss, psreg = ps[sbk], PS[sbk]
                        mm(pss[:, c0:QG], bufK[hp:hp + 64, 0, kb * 128:(kb + 1) * 128], bufQ[hp:hp + 64, ch, qg * QG + c0:(qg + 1) * QG],
                           True, False, reads=["bufQ", "bufK"], writes=[psreg])
                        mm(pss[:, c0:QG], cb[0:2, CB_NSLOPE + (4 + h) * 128:CB_NSLOPE + (5 + h) * 128],
                           cb[0:2, CB_POS + c0:CB_POS + QG], False, False, reads=["cb"], writes=[psreg])
                        for i in range(c0 // 128, 2):
                            mm(pss[:, i * 128:(i + 1) * 128], mbv[:, i, kb * 128:(kb + 1) * 128], ident_b, False, i == 1,
                               reads=[("mbv", qg % 2, i), "cb"], writes=[psreg])
                        st.update(sbk=sbk, c0=c0)

                    def s2(h=h, kb=kb, st=st, po=po, poreg=poreg, first_po=first_po, pi=pb_ - 2, last=(kb == nkb - 1)):
                        sbk, c0 = st["sbk"], st["c0"]
                        pss, psreg = ps[sbk], PS[sbk]
                        pt = pT[sbk]
                        dlt = kb - 2 * qg
                        bias_ap = cf[:, CF_ALIBI + (4 + h) * AL_N + dlt + AL_O:CF_ALIBI + (4 + h) * AL_N + dlt + AL_O + 1]
                        op("act", lambda e, pt=pt, pss=pss, c0=c0, bias_ap=bias_ap: e.activation(
                            out=pt[:, c0:QG], in_=pss[:, c0:QG], func=AF.Exp, bias=bias_ap),
                           reads=[psreg, "cf"], writes=[("pT", sbk)])
                        for i in range(c0 // 128, 2):
                            mm(po[:, i * 65:(i + 1) * 65], pt[:, i * 128:(i + 1) * 128], Vaug[:, kb, 0, :], first_po[0], False,
                               reads=[("pT", sbk), "V"], writes=[poreg])
                            first_po[0] = False
                        if not last:
                            return
                        dn = den[:, pi * 2:pi * 2 + 2]
                        dsrc = po[:, 0:130].rearrange("p (i c) -> p i c", c=65)[:, :, 64]
                        op("act", lambda e, dn=dn, dsrc=dsrc: e.activation(out=dn, in_=dsrc, func=AF.Ln, bias=1e-30),
                           reads=[poreg], writes=[("den", pi)])
                        op("act", lambda e, dn=dn: e.activation(out=dn, in_=dn, func=AF.Exp, scale=-1.0),
                           reads=[("den", pi)], writes=[("den", pi)])
                        for i in range(2):
                            t = qg * 2 + i
                            op("act", lambda e, i=i, t=t, h=h, po=po, dn=dn: e.activation(
                                out=mix_tok[:, t, h * 64:(h + 1) * 64], in_=po[:, i * 65:i * 65 + 64], func=AF.Copy, scale=dn[:, i:i + 1]),
                               reads=[poreg, ("den", pi)], writes=[("mix_tok", t)])

                    items.append((s1, s2))
            run_pipelined(items)

        nqg = S // QG
        for qg in range(nqg):
            index_topk(qg)
            if stopD > 3 and qg >= 1:
                attention(qg - 1)
        if stopD > 3:
            attention(nqg - 1)

    def phase_moe(l):
        NE = 2
        M_W = 0
        wb = [[dict(w1=carve(M_W + (s_ * NE + j) * 12288, [128, 8, 256], BF16),
                    w3=carve(M_W + (s_ * NE + j) * 12288 + 4096, [128, 8, 256], BF16),
                    w2f=carve(M_W + (s_ * NE + j) * 12288 + 8192, [128, 2, D], BF16)) for j in range(NE)] for s_ in range(2)]
        M_O = 2 * NE * 12288
        w2stage = [carve(M_O, [128, 2, D], F32)]
        hid = carve(M_O + 8192, [128, NE * 2, 512], BF16)
        t1 = [carve(M_O + 12288, [128, 512], F32), carve(M_O + 14336, [128, 512], F32)]
        t2 = [carve(M_O + 16384, [128, 512], F32), carve(M_O + 18432, [128, 512], F32)]
        chi_ = carve(M_O + 20480, [32, S], BF16)
        clo_ = carve(M_O + 24576, [32, S], BF16)
        comb = carve(M_O + 28672, [128, NT, 32], F32)
        lg = carve(M_O + 30720, [128, 64], F32)[:, 0:36]
        wr = carve(M_O + 30976, [128, 8, 36], BF16)
        rs = small[:, 160:200]
        ohb = [sb(f"ohb_{i_}", [32, 128], BF16) for i_ in range(2)]
        dma("sp", "gate", lambda e: e.dma_start(out=gate, in_=modb_d[l][:, 5 * D:6 * D]), reads=[("modb", l, 5)], writes=["gate"])
        dma("pool", "wr", lambda e: e.dma_start(out=wr, in_=w_r_d[l].rearrange("(c p) n -> p c n", p=128)), writes=["wr"])
        brow = rowp[:, RP_BR - 2048:RP_BR - 2048 + 36]
        for t in range(NT):
            b = pj_i[0] % 2
            pj_i[0] += 1
            for c in range(8):
                mm(ps[b][:, 0:36], hT[:, c, t * 128:(t + 1) * 128], wr[:, c, :], c == 0, c == 7,
                   reads=["wr", ("hT", t // 4)], writes=[PS[b]])
            op("dve", lambda e, b=b: e.tensor_tensor(out=lg, in0=ps[b][:, 0:36], in1=brow, op=ALU.add),
               reads=[PS[b], "rowp"], writes=["lg"])
            R_ = ["lg", "rs"]
            gmax, gsum, v1, v2, w1c, w2c = (rs[:, k:k + 1] for k in range(6))
            goh = rs[:, 8:12]
            gex = rs[:, 12:16]
            op("dve", lambda e: e.reduce_max(out=gmax, in_=lg[:, 0:4], axis=mybir.AxisListType.X), reads=R_, writes=["rs"])
            op("dve", lambda e: e.tensor_scalar(out=goh, in0=lg[:, 0:4], scalar1=gmax, scalar2=None, op0=ALU.is_ge), reads=R_, writes=["rs"])
            op("dve", lambda e: e.tensor_scalar(out=gex, in0=lg[:, 0:4], scalar1=gmax, scalar2=None, op0=ALU.subtract), reads=R_, writes=["rs"])
            op("act", lambda e: e.activation(out=gex, in_=gex, func=AF.Exp, accum_out=gsum), reads=R_, writes=["rs"])
            op("dve", lambda e: e.reciprocal(out=gsum, in_=gsum), reads=R_, writes=["rs"])
            op("dve", lambda e: e.tensor_scalar(out=gex, in0=goh, scalar1=-1.0, scalar2=1e9, op0=ALU.add, op1=ALU.mult), reads=R_, writes=["rs"])
            for g in range(4):
                op("dve", lambda e, g=g: e.tensor_scalar(out=lg[:, 4 + g * 8:12 + g * 8], in0=lg[:, 4 + g * 8:12 + g * 8],
                                                         scalar1=gex[:, g:g + 1], scalar2=None, op0=ALU.add), reads=R_, writes=["lg"])
            el = lg[:, 4:36]
            eq1 = lnv[:, 0:32]
            eq2 = lnv[:, 32:64]
            el2 = lnv[:, 64:96]
            R2 = ["lg", "rs", "lnv"]
            op("dve", lambda e: e.reduce_max(out=v1, in_=el, axis=mybir.AxisListType.X), reads=R2, writes=["rs"])
            op("dve", lambda e: e.tensor_scalar(out=eq1, in0=el, scalar1=v1, scalar2=None, op0=ALU.is_ge), reads=R2, writes=["lnv"])
            op("dve", lambda e: e.scalar_tensor_tensor(out=el2, in0=eq1, scalar=-1e9, in1=el, op0=ALU.mult, op1=ALU.add), reads=R2, writes=["lnv"])
            op("dve", lambda e: e.reduce_max(out=v2, in_=el2, axis=mybir.AxisListType.X), reads=R2, writes=["rs"])
            op("dve", lambda e: e.tensor_scalar(out=eq2, in0=el2, scalar1=v2, scalar2=None, op0=ALU.is_ge), reads=R2, writes=["lnv"])
            op("dve", lambda e: e.tensor_tensor(out=w2c, in0=v1, in1=v2, op=ALU.subtract), reads=R2, writes=["rs"])
            op("act", lambda e: e.activation(out=w2c, in_=w2c, func=AF.Exp), reads=R2, writes=["rs"])
            op("dve", lambda e: e.tensor_scalar(out=w2c, in0=w2c, scalar1=1.0, scalar2=None, op0=ALU.add), reads=R2, writes=["rs"])
            op("dve", lambda e: e.reciprocal(out=w2c, in_=w2c), reads=R2, writes=["rs"])
            op("dve", lambda e: e.tensor_scalar(out=w1c, in0=w2c, scalar1=-1.0, scalar2=1.0, op0=ALU.mult, op1=ALU.add), reads=R2, writes=["rs"])
            op("dve", lambda e: e.tensor_tensor(out=w1c, in0=w1c, in1=gsum, op=ALU.mult), reads=R2, writes=["rs"])
            op("dve", lambda e: e.tensor_tensor(out=w2c, in0=w2c, in1=gsum, op=ALU.mult), reads=R2, writes=["rs"])
            op("dve", lambda e: e.tensor_scalar(out=eq1, in0=eq1, scalar1=w1c, scalar2=None, op0=ALU.mult), reads=R2, writes=["lnv"])
            op("dve", lambda e, t=t: e.scalar_tensor_tensor(out=comb[:, t, :], in0=eq2, scalar=w2c, in1=eq1, op0=ALU.mult, op1=ALU.add),
               reads=R2, writes=["comb"])
        chl = carve(M_O + 12288, [128, NT, 64], BF16)
        ctmp = carve(M_O + 16384, [128, NT, 32], F32)
        op("dve", lambda e: e.tensor_copy(out=chl[:, :, 0:32], in_=comb), reads=["comb"], writes=["chl"])
        op("dve", lambda e: e.tensor_tensor(out=ctmp, in0=comb, in1=chl[:, :, 0:32], op=ALU.subtract), reads=["comb", "chl"], writes=["ctmp"])
        op("dve", lambda e: e.tensor_copy(out=chl[:, :, 32:64], in_=ctmp), reads=["ctmp"], writes=["chl"])
        pstb = ps[5].bitcast(BF16)
        for t in range(NT):
            op("pe", lambda e, t=t: e.transpose(pstb[0:64, (t % 8) * 128:(t % 8 + 1) * 128], chl[:, t, :], ident_b), reads=["chl", "cb"], writes=[PS[5]])
            if t % 8 == 7:
                t0_ = (t // 8) * 1024
                op("dve", lambda e, t0_=t0_: e.tensor_copy(out=chi_[:, t0_:t0_ + 1024], in_=pstb[0:32, :]), reads=[PS[5]], writes=["chi_"])
                op("dve", lambda e, t0_=t0_: e.tensor_copy(out=clo_[:, t0_:t0_ + 1024], in_=pstb[32:64, :]), reads=[PS[5]], writes=["clo_"])
        em.barrier()

        nsets = 32 // NE

        def load_set(s_):
            sl = s_ % 2
            for j in range(NE):
                e_ = s_ * NE + j
                w = wb[sl][j]
                dma("pool", f"mw{sl}{j}a", lambda e, w=w, e_=e_: e.dma_start(out=w["w1"], in_=w1_d[l][e_].rearrange("(c p) n -> p c n", p=128)),
                    writes=[("w13", sl, j)])
                dma("pool", f"mw{sl}{j}b", lambda e, w=w, e_=e_: e.dma_start(out=w["w3"], in_=w3_d[l][e_].rearrange("(c p) n -> p c n", p=128)),
                    writes=[("w13", sl, j)])
                st = w2stage[0]
                dma("sp", "mw2s", lambda e, st=st, e_=e_: e.dma_start(out=st, in_=w2_d[l][e_].rearrange("(c p) n -> p c n", p=128)),
                    writes=[("w2st", 0)])
                for c in range(2):
                    op("pool", lambda e, w=w, st=st, c=c: e.tensor_tensor(out=w["w2f"][:, c, :], in0=st[:, c, :], in1=gate, op=ALU.mult),
                       reads=[("w2st", 0), "gate"], writes=[("w2", sl, j)])

        hidb = [hid, carve(M_O + 28672, [128, NE * 2, 512], BF16)]
        steps = [(s_, tg) for s_ in range(nsets) for tg in range(4)]

        def stepA(i):
            s_, tg = steps[i]
            sl = s_ % 2
            hb = hidb[i % 2]
            for j in range(NE):
                e_ = s_ * NE + j
                w = wb[sl][j]
                n_ = i * NE + j
                cbk = 4 + n_ % 2
                oh = ohb[n_ % 2]
                op("dve", lambda e, oh=oh, e_=e_: e.tensor_scalar(out=oh, in0=ones_b[0:32, :], scalar1=ident_f[0:32, e_:e_ + 1],
                                                                  scalar2=None, op0=ALU.mult),
                   reads=["cb", "cf"], writes=[("ohb", n_ % 2)])
                mm(ps[cbk], oh, chi_[:, tg * 512:(tg + 1) * 512], True, False, reads=[("ohb", n_ % 2), "chi_"], writes=[PS[cbk]])
                mm(ps[cbk], oh, clo_[:, tg * 512:(tg + 1) * 512], False, True, reads=[("ohb", n_ % 2), "clo_"], writes=[PS[cbk]])
                for fc in range(2):
                    k = pj_i[0] % 2
                    pj_i[0] += 1
                    p1, p3 = ps[k], ps[2 + k]
                    for c in range(8):
                        mm(p1, w["w1"][:, c, fc * 128:(fc + 1) * 128], hT[:, c, tg * 512:(tg + 1) * 512], c == 0, c == 7,
                           reads=[("w13", sl, j), ("hT", tg)], writes=[PS[k]], inc=(c == 7))
                    for c in range(8):
                        mm(p3, w["w3"][:, c, fc * 128:(fc + 1) * 128], hT[:, c, tg * 512:(tg + 1) * 512], c == 0, c == 7,
                           reads=[("w13", sl, j), ("hT", tg)], writes=[PS[2 + k]], inc=(c == 7))
                    op("act", lambda e, k=k, p1=p1: e.activation(out=t1[k], in_=p1, func=AF.Silu), reads=[PS[k]], writes=[("t1", k)])
                    op("dve", lambda e, k=k, p3=p3: e.tensor_tensor(out=t2[k], in0=t1[k], in1=p3, op=ALU.mult),
                       reads=[("t1", k), PS[2 + k]], writes=[("t2", k)])
                    op("dve", lambda e, k=k, j=j, fc=fc, hb=hb, cbk=cbk: e.tensor_tensor(out=hb[:, j * 2 + fc, :], in0=t2[k], in1=ps[cbk], op=ALU.mult),
                       reads=[("t2", k), PS[cbk]], writes=[("hid", i % 2, j * 2 + fc)])

        def stepB(i):
            s_, tg = steps[i]
            sl = s_ % 2
            hb = hidb[i % 2]
            for ti in range(4):
                t = tg * 4 + ti
                for hf in range(2):
                    b = 6 + (ti * 2 + hf) % 2
                    n = 0
                    for j in range(NE):
                        for fc in range(2):
                            mm(ps[b], hb[:, j * 2 + fc, ti * 128:(ti + 1) * 128], wb[sl][j]["w2f"][:, fc, hf * 512:(hf + 1) * 512],
                               n == 0, n == NE * 2 - 1, reads=[("hid", i % 2, j * 2 + fc), ("w2", sl, j)], writes=[PS[b]],
                               inc=(n == NE * 2 - 1))
                            n += 1
                    op("dve", lambda e, t=t, hf=hf, b=b: e.tensor_tensor(out=x[:, t, hf * 512:(hf + 1) * 512],
                                                                         in0=x[:, t, hf * 512:(hf + 1) * 512], in1=ps[b], op=ALU.add),
                       reads=[PS[b], ("x", t)], writes=[("x", t)])

        load_set(0)
        load_set(1)
        stepA(0)
        for i, (s_, tg) in enumerate(steps):
            if tg == 0 and s_ >= 1 and s_ + 1 < nsets and "moe_nodma" not in cdbg:
                load_set(s_ + 1)
            if i + 1 < len(steps):
                stepA(i + 1)
            stepB(i)

    set_lnb()

    for l in range(nl):
        phase_mod(l)
        em.barrier()
        phase_norm(l, 0)
        dma("sp", "gate", lambda e, l=l: e.dma_start(out=gate, in_=modb_d[l][:, 2 * D:3 * D]), reads=[("modb", l, 2)], writes=["gate"])
        em.barrier()
        for m, name in enumerate("ABCD"):
            if name in mixers:
                {"A": mixer_A, "B": mixer_B, "C": mixer_C, "D": mixer_D}[name](l)
                em.barrier()
                mixer_out(l, m)
                em.barrier()
        phase_norm(l, 1)
        em.barrier()
        if do_moe:
            phase_moe(l)
            em.barrier()
    for t in range(NT):
        dma("sp", f"xo{t % 4}", lambda e, t=t: e.dma_start(out=out_d[t * 128:(t + 1) * 128, :], in_=x[:, t, :]),
            reads=[("x", t)], writes=[("out", t)])
    em.wait_all("sp", [("out", t) for t in range(NT)])
    em.finish()
    return nc


def prep_inputs(inp, n_cores=8):
    f = lambda a: np.ascontiguousarray(np.asarray(a, dtype=np.float32))
    cbv, cfv = make_consts()
    rowp = np.zeros((L, 128, NRP), np.float32)
    colp = np.zeros((L, 128, NCP), np.float32)
    for l in range(L):
        row = np.concatenate([f(inp["norm1_g"])[l], f(inp["norm2_g"])[l], f(inp["subln_g"])[l],
                              f(inp["b_group"])[l], f(inp["b_expert"])[l],
                              f(inp["lam_q1"])[l], f(inp["lam_k1"])[l], f(inp["lam_q2"])[l], f(inp["lam_k2"])[l]])
        rowp[l] = np.broadcast_to(row[None, :], (128, NRP))
        colp[l, :, CP_QNA] = np.tile(f(inp["qn_a"])[l], 2)
        colp[l, :, CP_KNA] = np.tile(f(inp["kn_a"])[l], 2)
        qc = np.tile(f(inp["qn_c"])[l], 4)
        m0 = (np.arange(128) % 64) // 32 == 0
        colp[l, m0, CP_QNC] = qc[m0]
        colp[l, ~m0, CP_QNC1] = qc[~m0]
        colp[l, :, CP_KNC] = np.tile(f(inp["kn_c"])[l], 4)
        colp[l, :, CP_QND] = np.tile(f(inp["qn_d"])[l], 2)
        colp[l, :, CP_KND] = np.tile(f(inp["kn_d"])[l], 2)
        colp[l, :, CP_BF] = np.tile(f(inp["b_f"])[l], 32)
        colp[l, :, CP_BETA:CP_BETA + 8] = f(inp["mix_beta"])[l].reshape(8, 128).T
    shared = dict(
        ada_w=f(inp["ada_w"]),
        ada_b=np.ascontiguousarray(np.broadcast_to(f(inp["ada_b"])[:, None, :], (L, 128, 6 * D))),
        w_in=f(inp["w_in"]), w_out=f(inp["w_out"]),
        w_r=np.ascontiguousarray(np.concatenate([f(inp["w_group"]), f(inp["w_expert"])], axis=-1)),
        w1=f(inp["w1"]).reshape(L, 32, D, 256), w3=f(inp["w3"]).reshape(L, 32, D, 256),
        w2=f(inp["w2"]).reshape(L, 32, 256, D),
        rowp=rowp, colp=colp, cb=cbv, cf=cfv, alc=make_alc(), onesr=np.ones((2, S), ml_dtypes.bfloat16))
    xs, cs = f(inp["x"]), f(inp["c"])
    maps = []
    for b in range(n_cores):
        m = dict(shared)
        m["x"] = xs[b]
        m["cT"] = np.ascontiguousarray(cs[b].reshape(8, 128).T)
        maps.append(m)
    return maps


_NC_CACHE = {}


def kernel(**inputs):
    if "nc" not in _NC_CACHE:
        nc = bass.Bass("TRN2", target_bir_lowering=False)
        _NC_CACHE["nc"] = build(nc, {})
    nc = _NC_CACHE["nc"]
    maps = prep_inputs(inputs, 8)
    res = run_bass_kernel_spmd(nc, maps, core_ids=list(range(8)))
    return np.stack([np.asarray(r["out"], dtype=np.float32) for r in res.results], axis=0)
```
